# Optimizing a Trainium2 kernel written in Bass

```python
import math
import jax, jax.numpy as jnp
from jax import lax
import numpy as np

D_MODEL = 2048
BATCH = 2
SEQ = 4096
DEPTH = 1

GRID_W = 64
HEAD_DIM = 128
NA_HEADS = 8
NA_WIDTH = NA_HEADS * HEAD_DIM
NA_WIN_ROWS_MAX = 8
NA_WIN_COLS = 16
DA_HEADS = 4
DA_VDIM = 2 * HEAD_DIM
DA_WIDTH = DA_HEADS * DA_VDIM
DA_QK = DA_HEADS * HEAD_DIM
DA_Q_BLOCK = 128
DA_LAYER_LAMBDA_BASE = 0.8
MIX_WIDTH = NA_WIDTH + DA_WIDTH
IN_COLS = 3 * NA_WIDTH + 4 * DA_QK + DA_WIDTH
N_EXPERTS = 16
EC_CAPACITY_FACTOR = 2
EXPERT_FF = 2816
RMS_EPS = 1e-6

kernel_name = "hybrid_natten_diffattn_ec_moe"


def rms_norm(x, g):
    xf = x.astype(jnp.float32)
    y = xf * lax.rsqrt(jnp.mean(xf * xf, axis=-1, keepdims=True) + RMS_EPS)
    return (y * g.astype(jnp.float32)).astype(x.dtype)


def lambda_init(layer_idx):
    return DA_LAYER_LAMBDA_BASE - 0.6 * math.exp(-0.3 * layer_idx)


def neighborhood_attention(q, k, v, rpb):
    B, S, H, d = q.shape
    rows = S // GRID_W
    kh = min(NA_WIN_ROWS_MAX, rows)
    kw = NA_WIN_COLS
    qg = q.reshape(B, rows, GRID_W, H, d)
    kg = k.reshape(B, rows, GRID_W, H, d)
    vg = v.reshape(B, rows, GRID_W, H, d)
    r = jnp.arange(rows)
    rs = jnp.clip(r - kh // 2, 0, rows - kh)
    row_idx = rs[:, None] + jnp.arange(kh)[None, :]
    k_rows = kg[:, row_idx]
    v_rows = vg[:, row_idx]
    c = jnp.arange(GRID_W)
    cs = jnp.clip(c - kw // 2, 0, GRID_W - kw)
    col_valid = (c[None, :] >= cs[:, None]) & (c[None, :] < cs[:, None] + kw)
    row_off = row_idx - r[:, None] + (NA_WIN_ROWS_MAX - 1)
    col_off = jnp.clip(c[None, :] - c[:, None], -(kw - 1), kw - 1) + (kw - 1)
    bias = rpb[:, row_off[:, None, :, None], col_off[None, :, None, :]]
    s = jnp.einsum('brqhd,brkchd->bhrqkc', qg, k_rows).astype(jnp.float32) * (d ** -0.5)
    s = s + bias.astype(jnp.float32)[None]
    s = jnp.where(col_valid[:, None, :], s, -jnp.inf)
    p = jax.nn.softmax(s.reshape(B, H, rows, GRID_W, kh * GRID_W), axis=-1)
    p = p.reshape(s.shape).astype(v.dtype)
    o = jnp.einsum('bhrqkc,brkchd->brqhd', p, v_rows)
    return o.reshape(B, S, H * d)


def differential_attention(q1, q2, k1, k2, v, lam, slopes):
    B, S, H, d = q1.shape
    nb = S // DA_Q_BLOCK
    scale = d ** -0.5
    pos = jnp.arange(S, dtype=jnp.float32)

    def to_blocks(t):
        return t.reshape(B, nb, DA_Q_BLOCK, H, d).transpose(1, 0, 2, 3, 4)

    def block(args):
        q1b, q2b, tb = args
        dist = jnp.abs(tb[:, None] - pos[None, :])
        alibi = -slopes[:, None, None] * dist[None]
        s1 = jnp.einsum('bqhd,bkhd->bhqk', q1b, k1).astype(jnp.float32) * scale + alibi
        s2 = jnp.einsum('bqhd,bkhd->bhqk', q2b, k2).astype(jnp.float32) * scale + alibi
        w = jax.nn.softmax(s1, axis=-1) - lam * jax.nn.softmax(s2, axis=-1)
        return jnp.einsum('bhqk,bkhe->bqhe', w.astype(v.dtype), v)

    o = lax.map(block, (to_blocks(q1), to_blocks(q2), pos.reshape(nb, DA_Q_BLOCK)))
    return o.transpose(1, 0, 2, 3, 4).reshape(B, S, H, v.shape[-1])


def expert_choice_ffn(h, w_router, w_gate, w_up, w_down):
    B, S, D = h.shape
    n_exp = w_router.shape[-1]
    cap = EC_CAPACITY_FACTOR * S // n_exp
    aff = jax.nn.softmax((h @ w_router).astype(jnp.float32), axis=-1)
    gate, idx = lax.top_k(aff.transpose(0, 2, 1), cap)
    bidx = jnp.arange(B)[:, None, None]
    xs = h[bidx, idx]
    g = jnp.einsum('becd,edf->becf', xs, w_gate)
    u = jnp.einsum('becd,edf->becf', xs, w_up)
    y = jnp.einsum('becf,efd->becd', jax.nn.silu(g) * u, w_down)
    y = y * gate[..., None].astype(h.dtype)
    return jnp.zeros_like(h).at[bidx, idx].add(y)


def setup_inputs(seed: int = 0) -> dict:
    key = jax.random.key(seed)
    ks = jax.random.split(key, 20)
    f32 = jnp.float32
    L = DEPTH

    def nrm(k, shape, scale):
        return jax.random.normal(k, shape, f32) * scale

    def gain(k, shape):
        return 1.0 + 0.02 * jax.random.normal(k, shape, f32)

    return {
        "x": jax.random.normal(ks[0], (BATCH, SEQ, D_MODEL), f32),
        "ln1_g": gain(ks[1], (L, D_MODEL)),
        "w_in": nrm(ks[2], (L, D_MODEL, IN_COLS), D_MODEL ** -0.5),
        "qn_a": gain(ks[3], (L, HEAD_DIM)),
        "kn_a": gain(ks[4], (L, HEAD_DIM)),
        "rpb_a": nrm(ks[5], (L, NA_HEADS, 2 * NA_WIN_ROWS_MAX - 1, 2 * NA_WIN_COLS - 1), 0.5),
        "on_a": gain(ks[6], (L, NA_WIDTH)),
        "qn_b": gain(ks[7], (L, HEAD_DIM)),
        "kn_b": gain(ks[8], (L, HEAD_DIM)),
        "lam_q1": nrm(ks[9], (L, HEAD_DIM), 0.1),
        "lam_k1": nrm(ks[10], (L, HEAD_DIM), 0.1),
        "lam_q2": nrm(ks[11], (L, HEAD_DIM), 0.1),
        "lam_k2": nrm(ks[12], (L, HEAD_DIM), 0.1),
        "subln_b": gain(ks[13], (L, DA_VDIM)),
        "w_out": nrm(ks[14], (L, MIX_WIDTH, D_MODEL), MIX_WIDTH ** -0.5),
        "ln2_g": gain(ks[15], (L, D_MODEL)),
        "w_router": nrm(ks[16], (L, D_MODEL, N_EXPERTS), D_MODEL ** -0.5),
        "w_gate": nrm(ks[17], (L, N_EXPERTS, D_MODEL, EXPERT_FF), D_MODEL ** -0.5),
        "w_up": nrm(ks[18], (L, N_EXPERTS, D_MODEL, EXPERT_FF), D_MODEL ** -0.5),
        "w_down": nrm(ks[19], (L, N_EXPERTS, EXPERT_FF, D_MODEL), EXPERT_FF ** -0.5),
    }


def reference(x, ln1_g, w_in, qn_a, kn_a, rpb_a, on_a, qn_b, kn_b, lam_q1, lam_k1,
              lam_q2, lam_k2, subln_b, w_out, ln2_g, w_router, w_gate, w_up, w_down):
    B, S, _ = x.shape
    splits = [NA_WIDTH, 2 * NA_WIDTH, 3 * NA_WIDTH,
              3 * NA_WIDTH + DA_QK, 3 * NA_WIDTH + 2 * DA_QK,
              3 * NA_WIDTH + 3 * DA_QK, 3 * NA_WIDTH + 4 * DA_QK]
    slopes = 2.0 ** (-8.0 * jnp.arange(1, DA_HEADS + 1, dtype=jnp.float32) / DA_HEADS)
    for l in range(DEPTH):
        h = rms_norm(x, ln1_g[l])
        proj = h @ w_in[l]
        qa, ka, va, q1, q2, k1, k2, vb = jnp.split(proj, splits, axis=-1)
        qa = rms_norm(qa.reshape(B, S, NA_HEADS, HEAD_DIM), qn_a[l])
        ka = rms_norm(ka.reshape(B, S, NA_HEADS, HEAD_DIM), kn_a[l])
        va = va.reshape(B, S, NA_HEADS, HEAD_DIM)
        oa = rms_norm(neighborhood_attention(qa, ka, va, rpb_a[l]), on_a[l])
        lam_i = lambda_init(l)
        lam = (jnp.exp(jnp.sum(lam_q1[l].astype(jnp.float32) * lam_k1[l].astype(jnp.float32)))
               - jnp.exp(jnp.sum(lam_q2[l].astype(jnp.float32) * lam_k2[l].astype(jnp.float32)))
               + lam_i)
        q1 = rms_norm(q1.reshape(B, S, DA_HEADS, HEAD_DIM), qn_b[l])
        q2 = rms_norm(q2.reshape(B, S, DA_HEADS, HEAD_DIM), qn_b[l])
        k1 = rms_norm(k1.reshape(B, S, DA_HEADS, HEAD_DIM), kn_b[l])
        k2 = rms_norm(k2.reshape(B, S, DA_HEADS, HEAD_DIM), kn_b[l])
        vb = vb.reshape(B, S, DA_HEADS, DA_VDIM)
        ob = differential_attention(q1, q2, k1, k2, vb, lam, slopes)
        ob = (rms_norm(ob, subln_b[l]) * (1.0 - lam_i)).reshape(B, S, DA_WIDTH)
        x = x + jnp.concatenate([oa, ob], axis=-1) @ w_out[l]
        h2 = rms_norm(x, ln2_g[l])
        x = x + expert_choice_ffn(h2, w_router[l], w_gate[l], w_up[l], w_down[l])
    return x
```

```python
import numpy as np
from contextlib import ExitStack
import concourse.bass as bass
import concourse.mybir as mybir
from concourse.bass_utils import run_bass_kernel_spmd

F32 = mybir.dt.float32
BF16 = mybir.dt.bfloat16
I32 = mybir.dt.int32
U8 = mybir.dt.uint8
AF = mybir.ActivationFunctionType
ALU = mybir.AluOpType
AX = mybir.AxisListType

D = 2048
S = 4096
NT = 32
GRID_W = 64
EPS = 1e-6
N_EXP = 16
CAP = 512
FF = 2816
NFC = FF // 128
GROUPS = [[0, 1, 2, 3], [4, 5, 6, 7]]
SCALE = 128.0 ** -0.5
LAM_INIT = 0.8 - 0.6
NEG = -30000.0
NPAT = 25
BISECT_ITERS = 24


class Sched:
    COMPUTE = ("act", "dve", "pool", "pe")

    def __init__(self, nc, es, rings):
        self.nc = nc
        self.ops = []
        self.lastw = {}
        self.readers = {}
        self.sems = []
        self.csem = {}
        for e in self.COMPUTE:
            self.csem[e] = self._sem(es, "c_" + e)
        self.rings = {e: [self._sem(es, f"d_{e}_{i}") for i in range(k)] for e, k in rings.items()}
        self.dma_count = {e: 0 for e in rings}
        self.es = es
        self.barrier_deps = []
        self.recent = {e: None for e in self.COMPUTE}
        self.recent_dma = {e: [] for e in rings}
        self.cc_ops = []
        self.cc_pool = [self._sem(es, f"cc_{i}") for i in range(12)]

    def _sem(self, es, name):
        self.sems.append(es.enter_context(self.nc.semaphore(name)))
        return len(self.sems) - 1

    def add(self, eng, fn, reads=(), writes=(), kind="c", extra=()):
        oid = len(self.ops)
        deps = {}
        for r in reads:
            w = self.lastw.get(r)
            if w is not None:
                deps.setdefault(w, set()).add("raw")
        for w_ in writes:
            w = self.lastw.get(w_)
            if w is not None:
                deps.setdefault(w, set()).add("waw")
            for rd in self.readers.get(w_, ()):
                deps.setdefault(rd, set()).add("war")
        for d in self.barrier_deps:
            deps.setdefault(d, set()).add("raw")
        for d in extra:
            deps.setdefault(d, set()).add("raw")
        fdeps = []
        for d, types in deps.items():
            p = self.ops[d]
            if p["kind"] == "c" and p["eng"] == eng and kind == "c":
                if eng == "pe" or "raw" not in types:
                    continue
            fdeps.append(d)
        op = dict(id=oid, eng=eng, fn=fn, kind=kind, deps=fdeps, has_dep=False, sem=None, val=0, prev=0)
        if kind == "dma":
            j = self.dma_count[eng]
            self.dma_count[eng] += 1
            K = len(self.rings[eng])
            op["sem"] = self.rings[eng][j % K]
            op["val"] = 16 * (j // K + 1)
            op["prev"] = 16 * (j // K)
            self.recent_dma[eng].append(oid)
            self.recent_dma[eng] = self.recent_dma[eng][-K:]
        elif kind == "cc":
            op["sem"] = self.cc_pool[len(self.cc_ops)]
            op["val"] = 1
            self.cc_ops.append(oid)
        else:
            self.recent[eng] = oid
        for r in reads:
            self.readers.setdefault(r, []).append(oid)
        for w_ in writes:
            self.lastw[w_] = oid
            self.readers[w_] = []
        self.ops.append(op)
        return oid

    def barrier(self):
        deps = [v for v in self.recent.values() if v is not None]
        for lst in self.recent_dma.values():
            deps += lst
        deps += self.cc_ops
        self.barrier_deps = deps

    def emit(self, block):
        ops = self.ops
        for op in ops:
            for d in op["deps"]:
                ops[d]["has_dep"] = True
        cnt = {e: 0 for e in self.COMPUTE}
        for op in ops:
            if op["kind"] == "c" and op["has_dep"]:
                cnt[op["eng"]] += 1
                op["sem"] = self.csem[op["eng"]]
                op["val"] = cnt[op["eng"]]
        for e, c in cnt.items():
            assert c < 60000, (e, c)
        lists = {}
        for op in ops:
            lists.setdefault(op["eng"], []).append(op)
        final = {}
        for op in ops:
            if op["sem"] is not None:
                final[op["sem"]] = max(final.get(op["sem"], 0), op["val"])
        sems = self.sems

        def run(name, eng):
            waited = {}
            for op in lists.get(name, []):
                waits = {}
                for d in op["deps"]:
                    p = ops[d]
                    waits[p["sem"]] = max(waits.get(p["sem"], 0), p["val"])
                if op["kind"] == "dma" and op["prev"] > 0:
                    waits[op["sem"]] = max(waits.get(op["sem"], 0), op["prev"])
                for s_, v in waits.items():
                    if waited.get(s_, 0) < v:
                        eng.wait_ge(sems[s_], v)
                        waited[s_] = v
                ins = op["fn"](eng)
                if op["kind"] == "dma":
                    ins.then_inc(sems[op["sem"]], 16)
                elif op["kind"] == "cc":
                    ins.then_inc(sems[op["sem"]], 1)
                elif op["has_dep"]:
                    ins.then_inc(sems[op["sem"]], 1)
            if name == "sync":
                for s_, v in final.items():
                    if waited.get(s_, 0) < v:
                        eng.wait_ge(sems[s_], v)

        @block.sync
        def _(e):
            run("sync", e)

        @block.scalar
        def _(e):
            run("act", e)

        @block.vector
        def _(e):
            run("dve", e)

        @block.gpsimd
        def _(e):
            run("pool", e)

        @block.tensor
        def _(e):
            run("pe", e)


class Arena:
    def __init__(self, ar, size):
        self.ar = ar
        self.size = size

    def view(self, off, shape, dt):
        esz = {F32: 4, BF16: 2, I32: 4}[dt]
        n = int(np.prod(shape[1:]))
        assert off % 4 == 0 and off + n * esz <= self.size, (off, shape, self.size)
        v = self.ar[:, off:off + n * esz].bitcast(dt)
        if len(shape) == 3:
            v = v.rearrange("p (a b) -> p a b", a=shape[1])
        elif len(shape) == 4:
            v = v.rearrange("p (a b c) -> p a b c", a=shape[1], b=shape[2])
        return v


def bc(ap, shape):
    return ap.unsqueeze(len(ap.shape)).to_broadcast(list(shape))


def build_nc(debug=False, stop_after=None):
    nc = bass.Bass("TRN2", target_bir_lowering=False)

    def din(name, shape, dt=F32):
        return nc.dram_tensor(name, list(shape), dt, kind="ExternalInput").ap()

    x_b = din("x_b", [S, D])
    x_own = din("x_own", [1024, D])
    w_in_g = din("w_in_g", [D, 1536])
    ln1T = din("ln1T", [128, 16])
    qkg = din("qkg", [128, 8])
    nab = din("nab", [2, 128, NPAT, 128])
    alib = din("alib", [128, 4, 256])
    cbias = din("cbias", [128, 64])
    lamv = din("lamv", [4, 128])
    w_out_p = din("w_out_p", [D, D])
    wog = din("wog", [128, 16])
    ln2 = din("ln2", [1, D])
    w_router = din("w_router", [D, N_EXP])
    sel = din("sel", [128, 4, N_EXP])
    fcv = din("fcv", [128, NT])
    own_tok = din("own_tok", [128, 8, 4], I32)
    mix_idx = din("mix_idx", [128, 8, 4], I32)
    if stop_after is None:
        wg_e = din("wg_e", [4, D, FF])
        wu_e = din("wu_e", [4, D, FF])
        wd_e = din("wd_e", [4, FF, D])
    out = nc.dram_tensor("out", [S, 512], F32, kind="ExternalOutput").ap()

    ag1_in = nc.dram_tensor("ag1_in", [S, 512], BF16).ap()
    ag1_out = nc.dram_tensor("ag1_out", [4 * S, 512], BF16).ap()
    h2_in = nc.dram_tensor("h2_in", [1024, D], BF16).ap()
    h2_all = nc.dram_tensor("h2_all", [S, D], BF16).ap()
    aff_in = nc.dram_tensor("aff_in", [1024, N_EXP], F32).ap()
    aff_all = nc.dram_tensor("aff_all", [S, N_EXP], F32).ap()
    part = nc.dram_tensor("part", [4 * S, 512], F32).ap()
    rs_out = nc.dram_tensor("rs_out", [S, 512], F32).ap()
    dbg = {}
    if debug:
        dbg["mix"] = nc.dram_tensor("dbg_mix", [S, 512], BF16, kind="ExternalOutput").ap()
        dbg["x1"] = nc.dram_tensor("dbg_x1", [1024, D], F32, kind="ExternalOutput").ap()
        dbg["aff"] = nc.dram_tensor("dbg_aff", [S, N_EXP], F32, kind="ExternalOutput").ap()
        dbg["idx"] = nc.dram_tensor("dbg_idx", [128, 16], I32, kind="ExternalOutput").ap()
        dbg["gate"] = nc.dram_tensor("dbg_gate", [128, 16], F32, kind="ExternalOutput").ap()

    ARENA = 196 * 1024
    with ExitStack() as es:
        ar_t = es.enter_context(nc.sbuf_tensor("arena", [128, ARENA], U8))
        A = Arena(ar_t, ARENA)
        small = es.enter_context(nc.sbuf_tensor("small", [128, 1024], F32))
        ident_b = es.enter_context(nc.sbuf_tensor("ident_b", [128, 128], BF16))
        ident_f = es.enter_context(nc.sbuf_tensor("ident_f", [128, 128], F32))
        iota_f = es.enter_context(nc.sbuf_tensor("iota_f", [128, 512], F32))
        iota_p = es.enter_context(nc.sbuf_tensor("iota_p", [128, 1], F32))
        zero_b = es.enter_context(nc.sbuf_tensor("zero_b", [128, 32], BF16))
        pbanks = [es.enter_context(nc.psum_tensor(f"pb{i}", [128, 512], F32)) for i in range(8)]
        sc = Sched(nc, es, rings={"sync": 16, "pool": 24})
        block = es.enter_context(nc.Block())

        so = [0]

        def sm(n):
            v = small[:, so[0]:so[0] + n]
            so[0] += n
            assert so[0] <= 1024
            return v

        eps_c = sm(1)
        ones_b = A.view(ARENA - 256, [128, 128], BF16)
        ARENA_USE = ARENA - 256

        def pb(i):
            return pbanks[i][:]

        def pbb(i):
            return pbanks[i][:].bitcast(BF16)

        sc.add("pool", lambda e: e.memset(eps_c, EPS), writes=["eps"])
        sc.add("pool", lambda e: e.iota(iota_f[:], pattern=[[1, 512]], base=0, channel_multiplier=0,
                                        allow_small_or_imprecise_dtypes=True), writes=["iota_f"])
        sc.add("pool", lambda e: e.iota(iota_p[:], pattern=[[0, 1]], base=0, channel_multiplier=1,
                                        allow_small_or_imprecise_dtypes=True), writes=["iota_p"])
        sc.add("dve", lambda e: e.tensor_scalar(out=ident_f[:], in0=iota_f[:, 0:128], scalar1=iota_p[:, 0:1],
                                                scalar2=None, op0=ALU.is_equal),
               reads=["iota_f", "iota_p"], writes=["ident_f"])
        sc.add("dve", lambda e: e.tensor_copy(out=ident_b[:], in_=ident_f[:]), reads=["ident_f"], writes=["ident_b"])
        sc.add("pool", lambda e: e.memset(ones_b, 1.0), writes=["ones_b"])
        sc.add("pool", lambda e: e.memset(zero_b[:], 0.0), writes=["zero_b"])

        QKT = A.view(0, [128, 8, S], BF16)
        VA = A.view(65536, [128, NT, 2, 130], BF16)
        VB = A.view(82176, [128, NT, 258], BF16)
        R1 = 98688
        WP = A.view(R1, [128, 16, 1536], BF16)
        XIN = [A.view(R1 + 49152 + i * 8192, [128, D], F32) for i in range(2)]
        R2 = R1 + 65536
        XS = [A.view(R2 + i * 4096, [128, D], BF16) for i in range(2)]
        XT = [A.view(R2 + 8192 + i * 4096, [128, 16, 128], BF16) for i in range(2)]
        SQ = A.view(R2 + 16384, [128, 8, 128], F32)
        QKB = [A.view(R2 + 20480 + i * 2048, [128, 8, 128], BF16) for i in range(2)]
        JUNK = A.view(R2 + 24576, [128, D], BF16)
        assert R2 + 28672 <= ARENA_USE

        ln1T_s = sm(16)
        qkg_s = sm(8)
        ssx = sm(NT)
        rsx = sm(NT)
        ssqk = sm(8 * 2)
        rsqk = sm(8 * 2)

        sc.add("sync", lambda e: e.dma_start(out=ln1T_s, in_=ln1T), writes=["ln1T"], kind="dma")
        sc.add("sync", lambda e: e.dma_start(out=qkg_s, in_=qkg), writes=["qkg"], kind="dma")
        sc.add("pool", lambda e: e.memset(VA[:, :, :, 128:130], 1.0), writes=["VAones"])
        sc.add("pool", lambda e: e.memset(VB[:, :, 256:258], 1.0), writes=["VBones"])
        w_in_v = w_in_g.rearrange("(kc p) n -> p kc n", p=128)
        for kc in range(16):
            sc.add("pool", lambda e, kc=kc: e.dma_start(out=WP[:, kc, :], in_=w_in_v[:, kc, :]),
                   writes=[("WPraw", kc)], kind="dma")
        for kc in range(16):
            eng = "dve" if kc % 2 == 0 else "pool"
            sc.add(eng, lambda e, kc=kc: e.tensor_scalar(out=WP[:, kc, :], in0=WP[:, kc, :],
                                                         scalar1=ln1T_s[:, kc:kc + 1], scalar2=None, op0=ALU.mult),
                   reads=[("WPraw", kc), "ln1T"], writes=[("WP", kc)])

        def a_load(tt):
            sc.add("sync", lambda e: e.dma_start(out=XIN[tt % 2], in_=x_b[tt * 128:(tt + 1) * 128, :]),
                   writes=[("xin", tt % 2)], kind="dma")

        def a_norm(tt):
            s = tt % 2
            sc.add("act", lambda e: e.activation(out=JUNK, in_=XIN[s], func=AF.Square, accum_out=ssx[:, tt:tt + 1]),
                   reads=[("xin", s)], writes=["junk", ("ssx", tt)])
            sc.add("act", lambda e: e.activation(out=rsx[:, tt:tt + 1], in_=ssx[:, tt:tt + 1], func=AF.Sqrt,
                                                 scale=1.0 / D, bias=eps_c),
                   reads=[("ssx", tt), "eps"], writes=[("rsx0", tt)])
            sc.add("dve", lambda e: e.reciprocal(out=rsx[:, tt:tt + 1], in_=rsx[:, tt:tt + 1]),
                   reads=[("rsx0", tt)], writes=[("rsx", tt)])
            sc.add("pool", lambda e: e.tensor_scalar(out=XS[s], in0=XIN[s], scalar1=rsx[:, tt:tt + 1], scalar2=None,
                                                     op0=ALU.mult),
                   reads=[("xin", s), ("rsx", tt)], writes=[("xs", s)])

        def a_transpose(tt):
            s = tt % 2
            for half in range(2):
                def f(e, half=half):
                    r = None
                    for j in range(8):
                        kc = half * 8 + j
                        r = e.transpose(out=pbb(0)[:, j * 128:(j + 1) * 128], in_=XS[s][:, kc * 128:(kc + 1) * 128],
                                        identity=ident_b[:])
                    return r
                sc.add("pe", f, reads=[("xs", s), "ident_b"], writes=[("pb", 0)])
                sc.add("dve", lambda e, half=half: e.tensor_copy(
                    out=XT[s][:, half * 8:(half + 1) * 8, :],
                    in_=pbb(0).rearrange("p (a b) -> p a b", a=8)),
                    reads=[("pb", 0)], writes=[("xT", s, half)])

        def a_proj(tt):
            s = tt % 2
            base = 2 + 3 * (tt % 2)

            def f(e):
                r = None
                for kc in range(16):
                    for nb in range(3):
                        r = e.matmul(pb(base + nb), lhsT=XT[s][:, kc, :], rhs=WP[:, kc, nb * 512:(nb + 1) * 512],
                                     start=(kc == 0), stop=(kc == 15))
                return r
            sc.add("pe", f, reads=[("xT", s, 0), ("xT", s, 1)] + [("WP", kc) for kc in range(16)],
                   writes=[("pb", base + q_) for q_ in range(3)])

        def a_evac(tt):
            s = tt % 2
            base = 2 + 3 * (tt % 2)
            qk_ps = [pb(base), pb(base + 1)]
            for h2_ in range(2):
                sc.add("act", lambda e, h2_=h2_: e.activation(
                    out=SQ[:, h2_ * 4:(h2_ + 1) * 4, :], in_=qk_ps[h2_].rearrange("p (a b) -> p a b", a=4),
                    func=AF.Square), reads=[("pb", base + h2_)], writes=[("sq", h2_)])
            sc.add("dve", lambda e: e.tensor_reduce(out=ssqk[:, s * 8:(s + 1) * 8], in_=SQ, axis=AX.X, op=ALU.add),
                   reads=[("sq", 0), ("sq", 1)], writes=[("ssqk", s)])
            sc.add("act", lambda e: e.activation(out=rsqk[:, s * 8:(s + 1) * 8], in_=ssqk[:, s * 8:(s + 1) * 8],
                                                 func=AF.Sqrt, scale=1.0 / 128, bias=eps_c),
                   reads=[("ssqk", s), "eps"], writes=[("rsqk0", s)])
            sc.add("dve", lambda e: e.reciprocal(out=rsqk[:, s * 8:(s + 1) * 8], in_=rsqk[:, s * 8:(s + 1) * 8]),
                   reads=[("rsqk0", s)], writes=[("rsqk", s)])
            for h2_ in range(2):
                sc.add("dve", lambda e, h2_=h2_: e.tensor_tensor(
                    out=QKB[s][:, h2_ * 4:(h2_ + 1) * 4, :], in0=qk_ps[h2_].rearrange("p (a b) -> p a b", a=4),
                    in1=bc(rsqk[:, s * 8 + h2_ * 4:s * 8 + (h2_ + 1) * 4], [128, 4, 128]), op=ALU.mult),
                    reads=[("pb", base + h2_), ("rsqk", s)], writes=[("qkb", s, h2_)])
            sc.add("act", lambda e: e.copy(out=VA[:, tt, :, 0:128],
                                           in_=pb(base + 2)[:, 0:256].rearrange("p (a b) -> p a b", a=2)),
                   reads=[("pb", base + 2)], writes=[("VA", tt)])
            sc.add("act", lambda e: e.copy(out=VB[:, tt, 0:256], in_=pb(base + 2)[:, 256:512]),
                   reads=[("pb", base + 2)], writes=[("VB", tt)])

        def a_qkT(tt):
            s = tt % 2

            def f(e):
                r = None
                for j in range(8):
                    r = e.transpose(out=pbb(1)[:, j * 128:(j + 1) * 128], in_=QKB[s][:, j, :], identity=ident_b[:])
                return r
            sc.add("pe", f, reads=[("qkb", s, 0), ("qkb", s, 1), "ident_b"], writes=[("pb", 1)])
            sc.add("dve", lambda e: e.tensor_tensor(out=QKT[:, :, tt * 128:(tt + 1) * 128],
                                                    in0=pbb(1).rearrange("p (a b) -> p a b", a=8),
                                                    in1=bc(qkg_s, [128, 8, 128]), op=ALU.mult),
                   reads=[("pb", 1), "qkg"], writes=[("QKT", tt)])

        a_load(0)
        a_load(1)
        a_norm(0)
        a_transpose(0)
        for tt in range(NT):
            a_proj(tt)
            if tt + 1 < NT:
                a_norm(tt + 1)
                a_transpose(tt + 1)
            if tt + 2 < NT:
                a_load(tt + 2)
            if tt >= 1:
                a_qkT(tt - 1)
            a_evac(tt)
        a_qkT(NT - 1)
        sc.barrier()
        if stop_after == "A":
            sc.add("sync", lambda e: e.dma_start(out=out[0:128, :], in_=XIN[0]), writes=["outdummy"], kind="dma")
            sc.emit(block)
            return nc

        WO = A.view(R1, [128, 16, D], BF16)
        TS = [A.view(R2 + i * 2048, [128, 2, 256], F32) for i in range(3)]
        PT = [A.view(R2 + 6144 + i * 1024, [128, 2, 256], BF16) for i in range(3)]
        ALB = A.view(R2 + 9216, [128, 4, 256], F32)
        OB1 = A.view(R2 + 13312, [128, 256], F32)
        OBN = [A.view(R2 + 14336 + i * 1024, [128, 512], BF16) for i in range(2)]
        NAB = A.view(R2 + 16384, [128, NPAT, 128], F32)
        TNA = A.view(R2 + 29184, [128, 5, 128], F32)
        PNA = A.view(R2 + 31744, [128, 5, 128], BF16)
        LAMT = A.view(R2 + 33024, [128, 4, 128], F32)
        JB = A.view(R2 + 35072, [128, 256], F32)
        assert R2 + 36096 <= ARENA_USE
        cb_s = sm(64)
        wog_s = sm(16)
        lam_s = sm(8)
        dst = sm(64)
        nst = sm(8)

        sc.add("sync", lambda e: e.dma_start(out=ALB, in_=alib), writes=["alb"], kind="dma")
        sc.add("sync", lambda e: e.dma_start(out=cb_s, in_=cbias), writes=["cb"], kind="dma")
        sc.add("sync", lambda e: e.dma_start(out=wog_s, in_=wog), writes=["wog"], kind="dma")
        for i in range(4):
            sc.add("sync", lambda e, i=i: e.dma_start(out=LAMT[:, i, :], in_=lamv[i:i + 1, :].partition_broadcast(128)),
                   writes=[("lamt", i)], kind="dma")
        sc.add("dve", lambda e: e.tensor_tensor(out=LAMT[:, 0, :], in0=LAMT[:, 0, :], in1=LAMT[:, 1, :], op=ALU.mult),
               reads=[("lamt", 0), ("lamt", 1)], writes=["lp1"])
        sc.add("dve", lambda e: e.tensor_tensor(out=LAMT[:, 2, :], in0=LAMT[:, 2, :], in1=LAMT[:, 3, :], op=ALU.mult),
               reads=[("lamt", 2), ("lamt", 3)], writes=["lp2"])
        sc.add("dve", lambda e: e.tensor_reduce(out=lam_s[:, 0:1], in_=LAMT[:, 0, :], axis=AX.X, op=ALU.add),
               reads=["lp1"], writes=["ls1"])
        sc.add("dve", lambda e: e.tensor_reduce(out=lam_s[:, 1:2], in_=LAMT[:, 2, :], axis=AX.X, op=ALU.add),
               reads=["lp2"], writes=["ls2"])
        sc.add("act", lambda e: e.activation(out=lam_s[:, 2:4], in_=lam_s[:, 0:2], func=AF.Exp),
               reads=["ls1", "ls2"], writes=["lexp"])
        sc.add("dve", lambda e: e.tensor_tensor(out=lam_s[:, 4:5], in0=lam_s[:, 3:4], in1=lam_s[:, 2:3], op=ALU.subtract),
               reads=["lexp"], writes=["ldiff"])
        sc.add("dve", lambda e: e.tensor_scalar(out=lam_s[:, 5:6], in0=lam_s[:, 4:5], scalar1=-LAM_INIT, scalar2=None,
                                                op0=ALU.add),
               reads=["ldiff"], writes=["neglam"])

        w_out_v = w_out_p.rearrange("(kc p) n -> p kc n", p=128)
        for kc in range(16):
            sc.add("pool", lambda e, kc=kc: e.dma_start(out=WO[:, kc, :], in_=w_out_v[:, kc, :]),
                   writes=[("WOraw", kc)], kind="dma")
        for kc in range(16):
            mul2 = 1.0 if (kc % 4) < 2 else (1.0 - LAM_INIT)
            sc.add("pool", lambda e, kc=kc, mul2=mul2: e.tensor_scalar(
                out=WO[:, kc, :], in0=WO[:, kc, :], scalar1=wog_s[:, kc:kc + 1], scalar2=mul2, op0=ALU.mult, op1=ALU.mult),
                reads=[("WOraw", kc), "wog"], writes=[("WO", kc)])

        if stop_after == "B0":
            sc.add("sync", lambda e: e.dma_start(out=out[0:128, :], in_=XIN[0]), writes=["outdummy"], kind="dma")
            sc.emit(block)
            return nc
        SB = [5, 6, 7]
        ACC = [0, 1, 2, 3]
        Q1, Q2, K1, K2 = 4, 5, 6, 7

        def b_score(qb, kt, n):
            bk = SB[n % 3]

            def f(e):
                e.matmul(pb(bk)[:, 0:256], lhsT=QKT[:, K1, kt * 128:(kt + 1) * 128],
                         rhs=QKT[:, Q1, qb * 256:(qb + 1) * 256], start=True, stop=True)
                return e.matmul(pb(bk)[:, 256:512], lhsT=QKT[:, K2, kt * 128:(kt + 1) * 128],
                                rhs=QKT[:, Q2, qb * 256:(qb + 1) * 256], start=True, stop=True)
            sc.add("pe", f, reads=[("QKT", kt), ("QKT", 2 * qb), ("QKT", 2 * qb + 1)], writes=[("pb", bk)])

        def b_soft(qb, kt, n):
            bk = SB[n % 3]
            delta = 2 * qb - kt
            if delta >= 1:
                ti = 0
            elif delta <= -2:
                ti = 1
            elif delta == 0:
                ti = 2
            else:
                ti = 3
            ci = delta + 32
            sc.add("dve", lambda e: e.scalar_tensor_tensor(
                out=TS[n % 3], in0=pb(bk).rearrange("p (a b) -> p a b", a=2), scalar=SCALE,
                in1=ALB[:, ti:ti + 1, :].to_broadcast([128, 2, 256]), op0=ALU.mult, op1=ALU.add),
                reads=[("pb", bk), "alb"], writes=[("ts", n % 3)])
            sc.add("act", lambda e: e.activation(out=PT[n % 3], in_=TS[n % 3], func=AF.Exp, bias=cb_s[:, ci:ci + 1]),
                   reads=[("ts", n % 3), "cb"], writes=[("pt", n % 3)])

        def b_pv(qb, kt, n):
            def f(e):
                r = None
                for i in range(2):
                    for j in range(2):
                        r = e.matmul(pb(ACC[i * 2 + j])[:, 0:258], lhsT=PT[n % 3][:, i, j * 128:(j + 1) * 128],
                                     rhs=VB[:, kt, :], start=(kt == 0), stop=(kt == NT - 1))
                return r
            sc.add("pe", f, reads=[("pt", n % 3), ("VB", kt), "VBones"], writes=[("pb", q_) for q_ in range(4)])

        def b_epilogue(qb):
            for j in range(2):
                tq = 2 * qb + j
                a1 = pb(ACC[j])
                a2 = pb(ACC[2 + j])
                st = dst[:, (tq % 4) * 8:(tq % 4) * 8 + 8]
                k = ("dst", tq % 4)
                ka1, ka2 = ("pb", ACC[j]), ("pb", ACC[2 + j])
                sc.add("dve", lambda e, a1=a1, st=st: e.reciprocal(out=st[:, 0:1], in_=a1[:, 256:257]),
                       reads=[ka1], writes=[k + (0,)])
                sc.add("dve", lambda e, a2=a2, st=st: e.reciprocal(out=st[:, 1:2], in_=a2[:, 256:257]),
                       reads=[ka2], writes=[k + (1,)])
                sc.add("dve", lambda e, st=st: e.tensor_tensor(out=st[:, 2:3], in0=st[:, 1:2], in1=lam_s[:, 5:6], op=ALU.mult),
                       reads=[k + (1,), "neglam"], writes=[k + (2,)])
                sc.add("dve", lambda e, a1=a1, st=st: e.tensor_scalar(out=OB1, in0=a1[:, 0:256], scalar1=st[:, 0:1],
                                                                      scalar2=None, op0=ALU.mult),
                       reads=[ka1, k + (0,)], writes=["ob1a"])
                sc.add("dve", lambda e, a2=a2, st=st: e.scalar_tensor_tensor(out=OB1, in0=a2[:, 0:256], scalar=st[:, 2:3],
                                                                             in1=OB1, op0=ALU.mult, op1=ALU.add),
                       reads=[ka2, "ob1a", k + (2,)], writes=["ob1"])
                sc.add("dve", lambda e: e.tensor_tensor(out=JB, in0=OB1, in1=OB1, op=ALU.mult), reads=["ob1"], writes=["jb"])
                sc.add("dve", lambda e, st=st: e.tensor_reduce(out=st[:, 3:4], in_=JB, axis=AX.X, op=ALU.add),
                       reads=["jb"], writes=[k + (3,)])
                sc.add("act", lambda e, st=st: e.activation(out=st[:, 4:5], in_=st[:, 3:4], func=AF.Ln, scale=1.0 / 256,
                                                            bias=eps_c),
                       reads=[k + (3,), "eps"], writes=[k + (4,)])
                sc.add("act", lambda e, st=st: e.activation(out=st[:, 5:6], in_=st[:, 4:5], func=AF.Exp, scale=-0.5),
                       reads=[k + (4,)], writes=[k + (5,)])
                sc.add("dve", lambda e, st=st, tq=tq: e.tensor_scalar(out=OBN[tq % 2][:, 256:512], in0=OB1, scalar1=st[:, 5:6],
                                                                      scalar2=None, op0=ALU.mult),
                       reads=["ob1", k + (5,)], writes=[("obn_b", tq % 2)])
                sc.add("sync", lambda e, tq=tq: e.dma_start(out=ag1_in[tq * 128:(tq + 1) * 128, 256:512],
                                                            in_=OBN[tq % 2][:, 256:512]),
                       reads=[("obn_b", tq % 2)], writes=[("ag1_in_b", tq)], kind="dma")

        import os
        SKIP_BC = os.environ.get("SKIP_BC") == "1"
        seq = [(qb, kt) for qb in range(16) for kt in range(NT)]
        if SKIP_BC:
            seq = []
            ag_reads_skip = True
        LOOK = 2
        for i in range(min(LOOK, len(seq))):
            b_score(seq[i][0], seq[i][1], i)
        for i, (qb, kt) in enumerate(seq):
            if i + LOOK < len(seq):
                b_score(seq[i + LOOK][0], seq[i + LOOK][1], i + LOOK)
            b_soft(qb, kt, i)
            b_pv(qb, kt, i)
            if kt == NT - 1:
                b_epilogue(qb)

        if stop_after == "B":
            sc.add("sync", lambda e: e.dma_start(out=out[0:128, :], in_=XIN[0]), writes=["outdummy"], kind="dma")
            sc.emit(block)
            return nc
        NSA, NSB, NO = 5, 6, 4

        def na_tile(hh, m):
            qch, kch = hh, 2 + hh
            if m in (0, 1, 30, 31):
                sp = {0: 0, 1: 1, 30: 2, 31: 3}[m]
                pats = [5 + sp * 5 + i for i in range(5)]
            else:
                pats = list(range(5))
            if m in (0, 1):
                kts = [0, 1, 2, 3, 3]
            elif m in (30, 31):
                kts = [28, 29, 30, 31, 31]
            else:
                kts = [m - 2 + i for i in range(5)]

            def f(e):
                r = None
                for i in range(5):
                    o = pb(NSA)[:, i * 128:(i + 1) * 128] if i < 4 else pb(NSB)[:, 0:128]
                    r = e.matmul(o, lhsT=QKT[:, kch, kts[i] * 128:(kts[i] + 1) * 128],
                                 rhs=QKT[:, qch, m * 128:(m + 1) * 128], start=True, stop=True)
                return r
            sc.add("pe", f, reads=[("QKT", k_) for k_ in set(kts + [m])], writes=[("pb", NSA), ("pb", NSB)])
            contiguous = pats == list(range(pats[0], pats[0] + 5))
            assert contiguous
            p0 = pats[0]
            sc.add("dve", lambda e: e.scalar_tensor_tensor(
                out=TNA[:, 0:4, :], in0=pb(NSA).rearrange("p (a b) -> p a b", a=4), scalar=SCALE,
                in1=NAB[:, p0:p0 + 4, :], op0=ALU.mult, op1=ALU.add),
                reads=[("pb", NSA), ("nab", hh)], writes=["tna0"])
            sc.add("dve", lambda e: e.scalar_tensor_tensor(
                out=TNA[:, 4, :], in0=pb(NSB)[:, 0:128], scalar=SCALE,
                in1=NAB[:, p0 + 4, :], op0=ALU.mult, op1=ALU.add),
                reads=[("pb", NSB), ("nab", hh)], writes=["tna1"])
            sc.add("act", lambda e: e.activation(out=PNA, in_=TNA, func=AF.Exp), reads=["tna0", "tna1"], writes=["pna"])

            def f2(e):
                r = None
                for i in range(5):
                    r = e.matmul(pb(NO)[:, 0:130], lhsT=PNA[:, i, :], rhs=VA[:, kts[i], hh, :],
                                 start=(i == 0), stop=(i == 4))
                return r
            sc.add("pe", f2, reads=["pna", "VAones"] + [("VA", k_) for k_ in set(kts)], writes=[("pb", NO)])
            sc.add("dve", lambda e: e.reciprocal(out=nst[:, hh:hh + 1], in_=pb(NO)[:, 128:129]), reads=[("pb", NO)],
                   writes=[("nst", hh)])
            ob = OBN[m % 2]
            sc.add("dve", lambda e: e.tensor_scalar(out=ob[:, hh * 128:(hh + 1) * 128], in0=pb(NO)[:, 0:128],
                                                    scalar1=nst[:, hh:hh + 1], scalar2=None, op0=ALU.mult),
                   reads=[("pb", NO), ("nst", hh)], writes=[("obn_a", m % 2)])
            sc.add("sync", lambda e: e.dma_start(out=ag1_in[m * 128:(m + 1) * 128, hh * 128:(hh + 1) * 128],
                                                 in_=ob[:, hh * 128:(hh + 1) * 128]),
                   reads=[("obn_a", m % 2)], writes=[("ag1_in_a", hh, m)], kind="dma")

        for hh in range(0 if SKIP_BC else 2):
            sc.add("sync", lambda e, hh=hh: e.dma_start(out=NAB, in_=nab[hh]), writes=[("nab", hh)], kind="dma")
            for m in range(NT):
                na_tile(hh, m)

        if stop_after == "C":
            sc.add("sync", lambda e: e.dma_start(out=out[0:128, :], in_=XIN[0]), writes=["outdummy"], kind="dma")
            sc.emit(block)
            return nc
        ag_reads = [("ag1_in_b", t) for t in range(NT)] + [("ag1_in_a", h_, t) for h_ in range(2) for t in range(NT)]
        for k_ in range(4):
            sc.add("pool", lambda e, k_=k_: e.collective_compute(
                "AllGather", ALU.bypass, replica_groups=GROUPS,
                ins=[ag1_in[1024 * k_:1024 * (k_ + 1), :].opt()], outs=[ag1_out[4096 * k_:4096 * (k_ + 1), :].opt()]),
                reads=ag_reads, writes=[("ag1_out", k_)], kind="cc")
        if debug:
            for t_ in range(NT):
                sc.add("sync", lambda e, t_=t_: e.dma_start(out=dbg["mix"][t_ * 128:(t_ + 1) * 128, :],
                                                            in_=ag1_in[t_ * 128:(t_ + 1) * 128, :]),
                       reads=ag_reads, writes=[("dbg_mix", t_)], kind="dma")
        sc.barrier()
        if stop_after == "attn":
            sc.add("sync", lambda e: e.dma_start(out=out[0:128, :], in_=XIN[0]), writes=["outdummy"], kind="dma")
            sc.emit(block)
            return nc

        P0 = 0
        MIXT = [A.view(P0 + i * 4096, [128, 4, 512], BF16) for i in range(2)]
        MIXN = [A.view(P0 + 8192 + i * 4096, [128, 4, 512], BF16) for i in range(2)]
        MXT = [A.view(P0 + 16384 + i * 4096, [128, 16, 128], BF16) for i in range(2)]
        XO = [A.view(P0 + 24576 + i * 8192, [128, D], F32) for i in range(2)]
        X1 = [A.view(P0 + 40960 + i * 8192, [128, D], F32) for i in range(2)]
        H2F = A.view(P0 + 57344, [128, D], F32)
        H2T = A.view(P0 + 65536, [128, 16, 128], F32)
        H2B = [A.view(P0 + 73728 + i * 4096, [128, D], BF16) for i in range(2)]
        LN2R = A.view(P0 + 81920, [128, D], F32)
        WR = A.view(P0 + 90112, [128, 16, N_EXP], F32)
        ZERO = A.view(P0 + 91136, [128, 1024], F32)
        JD = A.view(P0 + 95232, [128, D], BF16)
        assert P0 + 95232 + 4096 <= R1 + 65536
        JD = A.view(R2, [128, D], BF16)
        own_s = es.enter_context(nc.sbuf_tensor("own_s", [128, 8, 4], I32))
        mixi_s = es.enter_context(nc.sbuf_tensor("mixi_s", [128, 8, 4], I32))
        dstat = sm(8 * 8)
        logit = sm(8 * 16)
        affs = sm(8 * 16)

        sc.add("sync", lambda e: e.dma_start(out=own_s[:], in_=own_tok), writes=["own"], kind="dma")
        sc.add("sync", lambda e: e.dma_start(out=mixi_s[:], in_=mix_idx), writes=["mixi"], kind="dma")
        sc.add("sync", lambda e: e.dma_start(out=LN2R, in_=ln2[0:1, :].partition_broadcast(128)), writes=["ln2r"], kind="dma")
        sc.add("sync", lambda e: e.dma_start(out=WR, in_=w_router.rearrange("(kc p) n -> p kc n", p=128)),
               writes=["wr"], kind="dma")
        sc.add("pool", lambda e: e.memset(ZERO, 0.0), writes=["zero"])
        part_v = part.rearrange("(a b p) n -> p a b n", p=128, b=2)
        for a_ in range(64):
            sc.add("sync", lambda e, a_=a_: e.dma_start(out=part_v[:, a_, :, :], in_=ZERO.rearrange("p (b n) -> p b n", b=2)),
                   reads=["zero"], writes=[("partz", a_)], kind="dma")
        partz_all = [("partz", a_) for a_ in range(64)]

        def d_tile(i):
            s = i % 2
            st = dstat[:, i * 8:(i + 1) * 8]
            for r in range(4):
                sc.add("pool", lambda e, r=r: e.indirect_dma_start(
                    out=MIXT[s][:, r, :], out_offset=None, in_=ag1_out,
                    in_offset=bass.IndirectOffsetOnAxis(ap=mixi_s[:, i, r:r + 1], axis=0)),
                    reads=[("ag1_out", k_) for k_ in range(4)] + ["mixi"], writes=[("mixt", s, r)], kind="dma")
            sc.add("sync", lambda e: e.dma_start(out=XO[s], in_=x_own[i * 128:(i + 1) * 128, :]),
                   writes=[("xo", s)], kind="dma")
            mr = [("mixt", s, r) for r in range(4)]
            sc.add("act", lambda e: e.activation(out=JD[:, 0:1024].rearrange("p (a b) -> p a b", a=4),
                                                 in_=MIXT[s][:, :, 0:256], func=AF.Square, accum_out=st[:, 0:1]),
                   reads=mr, writes=["jd", ("dst0", i)])
            sc.add("act", lambda e: e.activation(out=st[:, 1:2], in_=st[:, 0:1], func=AF.Sqrt, scale=1.0 / 1024, bias=eps_c),
                   reads=[("dst0", i), "eps"], writes=[("dst1", i)])
            sc.add("dve", lambda e: e.reciprocal(out=st[:, 2:3], in_=st[:, 1:2]), reads=[("dst1", i)], writes=[("dst2", i)])
            sc.add("dve", lambda e: e.tensor_scalar(out=MIXN[s][:, :, 0:256], in0=MIXT[s][:, :, 0:256], scalar1=st[:, 2:3],
                                                    scalar2=None, op0=ALU.mult),
                   reads=mr + [("dst2", i)], writes=[("mixn_a", s)])
            sc.add("pool", lambda e: e.tensor_copy(out=MIXN[s][:, :, 256:512], in_=MIXT[s][:, :, 256:512]),
                   reads=mr, writes=[("mixn_b", s)])
            for half in range(2):
                def f(e, half=half):
                    r_ = None
                    for j in range(8):
                        kc = half * 8 + j
                        r_ = e.transpose(out=pbb(0)[:, j * 128:(j + 1) * 128],
                                         in_=MIXN[s][:, kc // 4, (kc % 4) * 128:(kc % 4 + 1) * 128], identity=ident_b[:])
                    return r_
                sc.add("pe", f, reads=[("mixn_a", s), ("mixn_b", s), "ident_b"], writes=[("pb", 0)])
                sc.add("act", lambda e, half=half: e.copy(out=MXT[s][:, half * 8:(half + 1) * 8, :],
                                                          in_=pbb(0).rearrange("p (a b) -> p a b", a=8)),
                       reads=[("pb", 0)], writes=[("mxt", s, half)])
            for nb in range(4):
                bk = 1 + (nb % 2)

                def f(e, nb=nb, bk=bk):
                    r_ = None
                    for kc in range(16):
                        r_ = e.matmul(pb(bk), lhsT=MXT[s][:, kc, :], rhs=WO[:, kc, nb * 512:(nb + 1) * 512],
                                      start=(kc == 0), stop=(kc == 15))
                    return r_
                sc.add("pe", f, reads=[("mxt", s, 0), ("mxt", s, 1)] + [("WO", kc) for kc in range(16)],
                       writes=[("pb", bk)])
                sc.add("dve", lambda e, nb=nb, bk=bk: e.tensor_tensor(out=X1[s][:, nb * 512:(nb + 1) * 512], in0=pb(bk),
                                                                      in1=XO[s][:, nb * 512:(nb + 1) * 512], op=ALU.add),
                       reads=[("pb", bk), ("xo", s)], writes=[("x1", s, nb)])
            x1r = [("x1", s, nb) for nb in range(4)]
            for db in range(4):
                sc.add("pool", lambda e, db=db: e.indirect_dma_start(
                    out=part, out_offset=bass.IndirectOffsetOnAxis(ap=own_s[:, i, db:db + 1], axis=0),
                    in_=X1[s][:, db * 512:(db + 1) * 512], in_offset=None),
                    reads=x1r + ["own"] + partz_all, writes=[("part_x1", i, db)], kind="dma")
            if debug:
                sc.add("sync", lambda e: e.dma_start(out=dbg["x1"][i * 128:(i + 1) * 128, :], in_=X1[s]),
                       reads=x1r, writes=[("dbgx1", i)], kind="dma")
            sc.add("act", lambda e: e.activation(out=JD, in_=X1[s], func=AF.Square, accum_out=st[:, 3:4]),
                   reads=x1r, writes=["jd", ("dst3", i)])
            sc.add("act", lambda e: e.activation(out=st[:, 4:5], in_=st[:, 3:4], func=AF.Sqrt, scale=1.0 / D, bias=eps_c),
                   reads=[("dst3", i), "eps"], writes=[("dst4", i)])
            sc.add("dve", lambda e: e.reciprocal(out=st[:, 5:6], in_=st[:, 4:5]), reads=[("dst4", i)], writes=[("dst5", i)])
            sc.add("dve", lambda e: e.scalar_tensor_tensor(out=H2F, in0=X1[s], scalar=st[:, 5:6], in1=LN2R,
                                                           op0=ALU.mult, op1=ALU.mult),
                   reads=x1r + [("dst5", i), "ln2r"], writes=["h2f"])
            sc.add("pool", lambda e: e.tensor_copy(out=H2B[s], in_=H2F), reads=["h2f"], writes=[("h2b", s)])
            sc.add("sync", lambda e: e.dma_start(out=h2_in[i * 128:(i + 1) * 128, :], in_=H2B[s]),
                   reads=[("h2b", s)], writes=[("h2_in", i)], kind="dma")
            for q4 in range(4):
                def f(e, q4=q4):
                    r_ = None
                    for j in range(4):
                        kc = q4 * 4 + j
                        r_ = e.transpose(out=pb(3 + (q4 % 2))[:, j * 128:(j + 1) * 128], in_=H2F[:, kc * 128:(kc + 1) * 128],
                                         identity=ident_f[:])
                    return r_
                sc.add("pe", f, reads=["h2f", "ident_f"], writes=[("pb", 3 + (q4 % 2))])
                sc.add("act", lambda e, q4=q4: e.copy(out=H2T[:, q4 * 4:(q4 + 1) * 4, :],
                                                      in_=pb(3 + (q4 % 2)).rearrange("p (a b) -> p a b", a=4)),
                       reads=[("pb", 3 + (q4 % 2))], writes=[("h2t", q4)])

            def fl(e):
                r_ = None
                for kc in range(16):
                    r_ = e.matmul(pb(5)[:, 0:N_EXP], lhsT=H2T[:, kc, :], rhs=WR[:, kc, :], start=(kc == 0), stop=(kc == 15))
                return r_
            sc.add("pe", fl, reads=[("h2t", q4) for q4 in range(4)] + ["wr"], writes=[("pb", 5)])
            lg = logit[:, i * 16:(i + 1) * 16]
            af = affs[:, i * 16:(i + 1) * 16]
            sc.add("dve", lambda e: e.tensor_reduce(out=st[:, 6:7], in_=pb(5)[:, 0:N_EXP], axis=AX.X, op=ALU.max),
                   reads=[("pb", 5)], writes=[("dst6", i)])
            sc.add("dve", lambda e: e.tensor_scalar(out=lg, in0=pb(5)[:, 0:N_EXP], scalar1=st[:, 6:7], scalar2=None,
                                                    op0=ALU.subtract),
                   reads=[("pb", 5), ("dst6", i)], writes=[("lg", i)])
            sc.add("act", lambda e: e.activation(out=lg, in_=lg, func=AF.Exp, accum_out=st[:, 7:8]),
                   reads=[("lg", i)], writes=[("lge", i), ("dst7", i)])
            sc.add("dve", lambda e: e.reciprocal(out=st[:, 7:8], in_=st[:, 7:8]), reads=[("dst7", i)], writes=[("dst7r", i)])
            sc.add("dve", lambda e: e.tensor_scalar(out=af, in0=lg, scalar1=st[:, 7:8], scalar2=None, op0=ALU.mult),
                   reads=[("lge", i), ("dst7r", i)], writes=[("aff", i)])
            sc.add("sync", lambda e: e.dma_start(out=aff_in[i * 128:(i + 1) * 128, :], in_=af),
                   reads=[("aff", i)], writes=[("aff_in", i)], kind="dma")

        for i in range(8):
            d_tile(i)
        sc.add("pool", lambda e: e.collective_compute("AllGather", ALU.bypass, replica_groups=GROUPS,
                                                      ins=[aff_in.opt()], outs=[aff_all.opt()]),
               reads=[("aff_in", i) for i in range(8)], writes=["aff_all"], kind="cc")
        for j_ in range(4):
            sc.add("pool", lambda e, j_=j_: e.collective_compute(
                "AllGather", ALU.bypass, replica_groups=GROUPS,
                ins=[h2_in[256 * j_:256 * (j_ + 1), :].opt()], outs=[h2_all[1024 * j_:1024 * (j_ + 1), :].opt()]),
                reads=[("h2_in", i) for i in range(8)], writes=[("h2_all", j_)], kind="cc")
        if debug:
            sc.add("sync", lambda e: e.dma_start(out=dbg["aff"], in_=aff_all), reads=["aff_all"], writes=["dbg_aff"], kind="dma")
        sc.barrier()
        if stop_after == "router":
            sc.add("sync", lambda e: e.dma_start(out=out[0:128, :], in_=X1[0]), writes=["outdummy"], kind="dma")
            sc.emit(block)
            return nc

        E0 = 0
        XSG = A.view(E0, [128, 4, D], BF16)
        XST = A.view(E0 + 16384, [128, 16, 512], BF16)
        HT = A.view(E0 + 32768, [128, NFC, 512], BF16)
        YT = [A.view(E0 + 55296 + i * 2048, [128, 512], F32) for i in range(4)]
        ST_ = [A.view(E0 + 63488 + i * 1024, [128, 512], BF16) for i in range(4)]
        SG = A.view(E0 + 67584, [128, 512], F32)
        SG2 = A.view(E0 + 69632, [128, 512], F32)
        NGU = 5
        WG = [A.view(E0 + 71680 + i * 8192, [128, 16, 128], BF16) for i in range(NGU)]
        WU = [A.view(E0 + 71680 + i * 8192 + 4096, [128, 16, 128], BF16) for i in range(NGU)]
        WDO = E0 + 71680 + NGU * 8192
        WD = [A.view(WDO + i * 22528, [128, NFC, 512], BF16) for i in range(2)]
        A16 = A.view(WDO + 45056, [128, NT, N_EXP], F32)
        SELT = A.view(WDO + 47104, [128, 4, N_EXP], F32)
        PRD = A.view(WDO + 47360, [128, NT, N_EXP], F32)
        A4 = A.view(WDO + 49408, [128, 4, NT], F32)
        CMP = A.view(WDO + 49920, [128, 4, NT], F32)
        AP3 = A.view(WDO + 50432, [128, 4, NT, 6], BF16)
        RES = A.view(WDO + 54400, [128, 4, NT], F32)
        UTRI = A.view(WDO + 52224, [128, 128], F32)
        LT32 = A.view(WDO + 52736, [128, NT], F32)
        MSK = A.view(WDO + 53376, [128, 4, NT], F32)
        POS = A.view(WDO + 53888, [128, 4, NT], F32)
        FCV = A.view(WDO + 54912, [128, NT], F32)
        ONESF = A.view(WDO + 55040, [128, 4], F32)
        CSB = A.view(WDO + 55296, [128, 4, 128], F32)
        assert WDO + 57344 <= ARENA_USE, WDO + 57344
        thr = sm(4)
        cand = sm(4)
        cntp = es.enter_context(nc.sbuf_tensor("cntp", [128, 4], BF16))
        ge = sm(4)
        idxf = sm(80)
        pselc = sm(128)
        gts = sm(16)
        idx_i = es.enter_context(nc.sbuf_tensor("idx_i", [128, 4, 20], I32))
        pidx = es.enter_context(nc.sbuf_tensor("pidx", [128, NT], F32))

        gu_n = [0]

        def load_gu(e_, fc):
            k = gu_n[0] % NGU
            gu_n[0] += 1
            gv = wg_e[e_].rearrange("(kc p) n -> p kc n", p=128)
            uv = wu_e[e_].rearrange("(kc p) n -> p kc n", p=128)
            sc.add("pool", lambda e: e.dma_start(out=WG[k], in_=gv[:, :, fc * 128:(fc + 1) * 128]),
                   writes=[("wg", k)], kind="dma")
            sc.add("pool", lambda e: e.dma_start(out=WU[k], in_=uv[:, :, fc * 128:(fc + 1) * 128]),
                   writes=[("wu", k)], kind="dma")
            return k

        wd_n = [0]

        def load_wd(e_, db):
            k = wd_n[0] % 2
            wd_n[0] += 1
            dv = wd_e[e_].rearrange("(fc p) n -> p fc n", p=128)
            for h_ in range(2):
                sc.add("pool", lambda e, h_=h_: e.dma_start(out=WD[k][:, h_ * 11:(h_ + 1) * 11, :],
                                                            in_=dv[:, h_ * 11:(h_ + 1) * 11, db * 512:(db + 1) * 512]),
                       writes=[("wd", k, h_)], kind="dma")
            return k

        sc.add("sync", lambda e: e.dma_start(out=A16, in_=aff_all.rearrange("(c p) j -> p c j", p=128)),
               reads=["aff_all"], writes=["a16"], kind="dma")
        sc.add("sync", lambda e: e.dma_start(out=SELT, in_=sel), writes=["selt"], kind="dma")
        for e_ in range(4):
            sc.add("dve", lambda e, e_=e_: e.tensor_tensor(out=PRD, in0=A16, in1=SELT[:, e_:e_ + 1, :].to_broadcast([128, NT, N_EXP]),
                                                           op=ALU.mult),
                   reads=["a16", "selt"], writes=["prd"])
            sc.add("dve", lambda e, e_=e_: e.tensor_reduce(out=A4[:, e_, :], in_=PRD, axis=AX.X, op=ALU.add),
                   reads=["prd"], writes=[("a4", e_)])
        a4r = [("a4", e_) for e_ in range(4)]
        sc.add("dve", lambda e: e.tensor_scalar(out=UTRI, in0=iota_f[:, 0:128], scalar1=iota_p[:, 0:1], scalar2=None,
                                                op0=ALU.is_gt), reads=["iota_f", "iota_p"], writes=["utri"])
        sc.add("dve", lambda e: e.tensor_scalar(out=LT32, in0=iota_f[:, 0:NT], scalar1=iota_p[:, 0:1], scalar2=None,
                                                op0=ALU.is_gt), reads=["iota_f", "iota_p"], writes=["lt32"])
        sc.add("dve", lambda e: e.tensor_copy(out=pidx[:], in_=iota_p[:, 0:1].to_broadcast([128, NT])),
               reads=["iota_p"], writes=["pidx"])
        sc.add("dve", lambda e: e.tensor_copy(out=AP3[:, :, :, 0], in_=iota_f[:, 0:NT].unsqueeze(1).to_broadcast([128, 4, NT])),
               reads=["iota_f"], writes=["ap3_0"])
        sc.add("sync", lambda e: e.dma_start(out=FCV, in_=fcv), writes=["fcv"], kind="dma")
        sc.add("pool", lambda e: e.memset(ONESF, 1.0), writes=["ones_f"])
        sc.add("dve", lambda e: e.tensor_copy(out=AP3[:, :, :, 1], in_=FCV.unsqueeze(1).to_broadcast([128, 4, NT])),
               reads=["fcv"], writes=["ap3_1"])
        sc.add("dve", lambda e: e.tensor_copy(out=AP3[:, :, :, 2], in_=pidx[:].unsqueeze(1).to_broadcast([128, 4, NT])),
               reads=["pidx"], writes=["ap3_2"])
        sc.add("dve", lambda e: e.tensor_copy(out=AP3[:, :, :, 3], in_=A4), reads=a4r, writes=["ap3_3"])
        sc.add("dve", lambda e: e.tensor_tensor(out=RES, in0=A4, in1=AP3[:, :, :, 3], op=ALU.subtract),
               reads=a4r + ["ap3_3"], writes=["res1"])
        sc.add("dve", lambda e: e.tensor_copy(out=AP3[:, :, :, 4], in_=RES), reads=["res1"], writes=["ap3_4"])
        sc.add("dve", lambda e: e.tensor_tensor(out=RES, in0=RES, in1=AP3[:, :, :, 4], op=ALU.subtract),
               reads=["res1", "ap3_4"], writes=["res2"])
        sc.add("dve", lambda e: e.tensor_copy(out=AP3[:, :, :, 5], in_=RES), reads=["res2"], writes=["ap3_5"])
        ap3r = ["ap3_0", "ap3_1", "ap3_2", "ap3_3", "ap3_4", "ap3_5"]

        sc.add("dve", lambda e: e.memset(thr, 0.0), writes=["thr"])
        for it in range(1, BISECT_ITERS + 1):
            step = 2.0 ** (-it)
            sc.add("dve", lambda e, step=step: e.tensor_scalar(out=cand, in0=thr, scalar1=step, scalar2=None, op0=ALU.add),
                   reads=["thr"], writes=["cand"])
            sc.add("dve", lambda e: e.tensor_tensor(out=CMP, in0=A4, in1=bc(cand, [128, 4, NT]), op=ALU.is_gt),
                   reads=a4r + ["cand"], writes=["cmp"])
            def fcnt(e):
                with nc.allow_low_precision(reason="per-partition counts <= 32 are exact in bf16"):
                    return e.tensor_reduce(out=cntp[:], in_=CMP, axis=AX.X, op=ALU.add)
            sc.add("dve", fcnt, reads=["cmp"], writes=["cntp"])
            sc.add("pe", lambda e: e.matmul(pb(7)[:, 0:4], lhsT=ones_b, rhs=cntp[:], start=True, stop=True),
                   reads=["cntp", "ones_b"], writes=[("pb", 7)])
            sc.add("dve", lambda e: e.tensor_scalar(out=ge, in0=pb(7)[:, 0:4], scalar1=float(CAP) - 0.5, scalar2=None,
                                                    op0=ALU.is_gt), reads=[("pb", 7)], writes=["ge"])
            sc.add("dve", lambda e, step=step: e.scalar_tensor_tensor(out=thr, in0=ge, scalar=step, in1=thr,
                                                                      op0=ALU.mult, op1=ALU.add),
                   reads=["ge", "thr"], writes=["thr"])
        sc.add("dve", lambda e: e.tensor_tensor(out=MSK, in0=A4, in1=bc(thr, [128, 4, NT]), op=ALU.is_gt),
               reads=a4r + ["thr"], writes=["msk"])

        for e_ in range(4):
            sc.add("pe", lambda e, e_=e_: e.matmul(pb(6)[0:NT, e_:e_ + 1], lhsT=MSK[:, e_, :], rhs=ONESF[:, 0:1],
                                                   start=True, stop=True),
                   reads=["msk", "ones_f"], writes=[("pb", 6)])
        for e_ in range(4):
            sc.add("dve", lambda e, e_=e_: e.tensor_copy(out=CSB[0:NT, e_, :], in_=pb(6)[0:NT, e_:e_ + 1].to_broadcast([NT, 128])),
                   reads=[("pb", 6)], writes=[("csb", e_)])
        for e_ in range(4):
            def fpos(e, e_=e_):
                e.matmul(pb(7)[:, e_ * NT:(e_ + 1) * NT], lhsT=UTRI, rhs=MSK[:, e_, :], start=True, stop=False)
                return e.matmul(pb(7)[:, e_ * NT:(e_ + 1) * NT], lhsT=CSB[0:NT, e_, :], rhs=LT32[0:NT, :], start=False, stop=True)
            sc.add("pe", fpos, reads=["msk", "utri", "lt32", ("csb", e_)], writes=[("pb", 7)])
        sc.add("dve", lambda e: e.scalar_tensor_tensor(out=POS, in0=pb(7)[:, 0:4 * NT].rearrange("p (a b) -> p a b", a=4),
                                                       scalar=1.0, in1=MSK, op0=ALU.add, op1=ALU.mult),
               reads=[("pb", 7), "msk"], writes=["pos0"])
        sc.add("dve", lambda e: e.tensor_scalar(out=POS, in0=POS, scalar1=-1.0, scalar2=None, op0=ALU.add),
               reads=["pos0"], writes=["pos"])

        gu_list = [(e_, fc) for e_ in range(4) for fc in range(NFC)]
        wd_list = [(e_, db) for e_ in range(4) for db in range(4)]
        gu_loaded, wd_loaded = [], []

        def gu_prefetch(upto):
            while len(gu_loaded) < min(upto, len(gu_list)):
                gu_loaded.append(load_gu(*gu_list[len(gu_loaded)]))

        def wd_prefetch(upto):
            while len(wd_loaded) < min(upto, len(wd_list)):
                wd_loaded.append(load_wd(*wd_list[len(wd_loaded)]))

        gu_prefetch(NGU - 1)
        wd_prefetch(1)
        prev_scatter = []
        h2r = [("h2_all", j_) for j_ in range(4)]
        def expert(e_, prev_scatter):
            for c in range(NT):
                stile = ST_[c % 4]
                sc.add("dve", lambda e, c=c, stile=stile: e.tensor_scalar(out=stile, in0=iota_f[:], scalar1=POS[:, e_, c:c + 1],
                                                                          scalar2=None, op0=ALU.is_equal),
                       reads=["pos", "iota_f"], writes=[("stile", c % 4)])

                def fsel(e, c=c, stile=stile):
                    r_ = None
                    if c == 0:
                        e.matmul(pb(6)[:, 0:32], lhsT=ident_b[:], rhs=zero_b[:], start=True, stop=False)
                    for sg in range(4):
                        r_ = e.matmul(pb(6)[:, sg * 8:sg * 8 + 6], lhsT=stile[:, sg * 128:(sg + 1) * 128],
                                      rhs=AP3[:, e_, c, :], start=False, stop=(c == NT - 1))
                    return r_
                sc.add("pe", fsel, reads=[("stile", c % 4), "zero_b", "ident_b"] + ap3r, writes=[("pb", 6)])
            ik = ("idx", e_)
            psc = pselc[:, e_ * 32:(e_ + 1) * 32]
            sc.add("dve", lambda e, psc=psc: e.tensor_copy(out=psc, in_=pb(6)[:, 0:32]), reads=[("pb", 6)], writes=[("psc", e_)])
            psv = psc.rearrange("p (a b) -> p a b", a=4)
            fb = e_ * 20
            sc.add("dve", lambda e, psv=psv, fb=fb: e.scalar_tensor_tensor(out=idxf[:, fb:fb + 4], in0=psv[:, :, 0], scalar=128.0,
                                                                           in1=psv[:, :, 2], op0=ALU.mult, op1=ALU.add),
                   reads=[("psc", e_)], writes=[ik + (0,)])
            for db in range(1, 4):
                sc.add("dve", lambda e, fb=fb, db=db: e.tensor_scalar(out=idxf[:, fb + 4 * db:fb + 4 * db + 4], in0=idxf[:, fb:fb + 4],
                                                                      scalar1=float(S * db), scalar2=None, op0=ALU.add),
                       reads=[ik + (0,)], writes=[ik + (0, db)])
            sc.add("dve", lambda e, psv=psv, fb=fb: e.scalar_tensor_tensor(out=idxf[:, fb + 16:fb + 20], in0=psv[:, :, 1], scalar=128.0,
                                                                           in1=psv[:, :, 2], op0=ALU.mult, op1=ALU.add),
                   reads=[("psc", e_)], writes=[ik + (1,)])
            sc.add("dve", lambda e, fb=fb: e.tensor_copy(out=idx_i[:, e_, :], in_=idxf[:, fb:fb + 20]),
                   reads=[ik + (0,), ik + (1,)] + [ik + (0, db) for db in range(1, 4)], writes=[ik])
            sc.add("dve", lambda e, psv=psv: e.tensor_reduce(out=gts[:, e_ * 4:(e_ + 1) * 4], in_=psv[:, :, 3:6], axis=AX.X, op=ALU.add),
                   reads=[("psc", e_)], writes=[("gate", e_)])
            if debug:
                sc.add("sync", lambda e: e.dma_start(out=dbg["idx"][:, e_ * 4:(e_ + 1) * 4], in_=idx_i[:, e_, 0:4]),
                       reads=[ik], writes=[("dbgidx", e_)], kind="dma")
                sc.add("sync", lambda e: e.dma_start(out=dbg["gate"][:, e_ * 4:(e_ + 1) * 4], in_=gts[:, e_ * 4:(e_ + 1) * 4]),
                       reads=[("gate", e_)], writes=[("dbggate", e_)], kind="dma")
            for sg in range(4):
                sc.add("pool", lambda e, sg=sg: e.indirect_dma_start(
                    out=XSG[:, sg, :], out_offset=None, in_=h2_all,
                    in_offset=bass.IndirectOffsetOnAxis(ap=idx_i[:, e_, 16 + sg:17 + sg], axis=0)),
                    reads=h2r + [ik], writes=[("xsg", sg)], kind="dma")
            for sg in range(4):
                for half in range(2):
                    bk = (sg * 2 + half) % 2

                    def ftr(e, sg=sg, half=half, bk=bk):
                        r_ = None
                        for j in range(8):
                            kc = half * 8 + j
                            r_ = e.transpose(out=pbb(bk)[:, j * 128:(j + 1) * 128], in_=XSG[:, sg, kc * 128:(kc + 1) * 128],
                                             identity=ident_b[:])
                        return r_
                    sc.add("pe", ftr, reads=[("xsg", sg), "ident_b"], writes=[("pb", bk)])
                    ce = "act" if half == 0 else "dve"
                    if ce == "act":
                        sc.add("act", lambda e, sg=sg, half=half, bk=bk: e.copy(
                            out=XST[:, half * 8:(half + 1) * 8, sg * 128:(sg + 1) * 128],
                            in_=pbb(bk).rearrange("p (a b) -> p a b", a=8)),
                            reads=[("pb", bk)], writes=[("xst", sg, half)])
                    else:
                        sc.add("dve", lambda e, sg=sg, half=half, bk=bk: e.tensor_copy(
                            out=XST[:, half * 8:(half + 1) * 8, sg * 128:(sg + 1) * 128],
                            in_=pbb(bk).rearrange("p (a b) -> p a b", a=8)),
                            reads=[("pb", bk)], writes=[("xst", sg, half)])
            xstr = [("xst", sg, half) for sg in range(4) for half in range(2)]
            for fc in range(NFC):
                n_ = e_ * NFC + fc
                gu_prefetch(n_ + NGU)
                k = gu_loaded[n_]
                pg, pu = 2 + 2 * (fc % 2), 3 + 2 * (fc % 2)

                def fgu(e, k=k, pg=pg, pu=pu):
                    r_ = None
                    for kc in range(16):
                        r_ = e.matmul(pb(pg), lhsT=WG[k][:, kc, :], rhs=XST[:, kc, :], start=(kc == 0), stop=(kc == 15))
                    for kc in range(16):
                        r_ = e.matmul(pb(pu), lhsT=WU[k][:, kc, :], rhs=XST[:, kc, :], start=(kc == 0), stop=(kc == 15))
                    return r_
                sc.add("pe", fgu, reads=xstr + [("wg", k), ("wu", k)], writes=[("pb", pg), ("pb", pu)])
                sgt = SG if fc % 2 == 0 else SG2
                sc.add("act", lambda e, pg=pg, sgt=sgt: e.activation(out=sgt, in_=pb(pg), func=AF.Silu),
                       reads=[("pb", pg)], writes=[("sg", fc % 2)])
                sc.add("dve", lambda e, pu=pu, sgt=sgt, fc=fc: e.tensor_tensor(out=HT[:, fc, :], in0=sgt, in1=pb(pu), op=ALU.mult),
                       reads=[("sg", fc % 2), ("pb", pu)], writes=[("ht", fc)])
            htr = [("ht", fc) for fc in range(NFC)]
            scat = []
            for db in range(4):
                nd = e_ * 4 + db
                wd_prefetch(nd + 2)
                kd = wd_loaded[nd]
                for sg in range(4):
                    m_ = db * 4 + sg
                    bk = m_ % 2

                    def fdn(e, sg=sg, kd=kd, bk=bk):
                        r_ = None
                        for fc in range(NFC):
                            r_ = e.matmul(pb(bk), lhsT=HT[:, fc, sg * 128:(sg + 1) * 128], rhs=WD[kd][:, fc, :],
                                          start=(fc == 0), stop=(fc == NFC - 1))
                        return r_
                    sc.add("pe", fdn, reads=htr + [("wd", kd, 0), ("wd", kd, 1)], writes=[("pb", bk)])
                    yt = YT[m_ % 4]
                    if m_ % 2 == 0:
                        sc.add("act", lambda e, bk=bk, yt=yt, sg=sg: e.activation(out=yt, in_=pb(bk), func=AF.Copy,
                                                                                  scale=gts[:, e_ * 4 + sg:e_ * 4 + sg + 1]),
                               reads=[("pb", bk), ("gate", e_)], writes=[("yt", m_ % 4)])
                    else:
                        sc.add("dve", lambda e, bk=bk, yt=yt, sg=sg: e.tensor_scalar(out=yt, in0=pb(bk),
                                                                                     scalar1=gts[:, e_ * 4 + sg:e_ * 4 + sg + 1],
                                                                                     scalar2=None, op0=ALU.mult),
                               reads=[("pb", bk), ("gate", e_)], writes=[("yt", m_ % 4)])
                    scat.append(sc.add("pool", lambda e, db=db, sg=sg, yt=yt: e.indirect_dma_start(
                        out=part,
                        out_offset=bass.IndirectOffsetOnAxis(ap=idx_i[:, e_, db * 4 + sg:db * 4 + sg + 1], axis=0),
                        in_=yt, in_offset=None, compute_op=ALU.add),
                        reads=[("yt", m_ % 4), ik] + partz_all + [("part_x1", i, db_) for i in range(8) for db_ in range(4)],
                        writes=[("part_sc", e_, db, sg)], kind="dma", extra=prev_scatter))
            return scat

        for e_ in range(4):
            prev_scatter = expert(e_, prev_scatter)
        all_sc = [("part_sc", e_, db, sg) for e_ in range(4) for db in range(4) for sg in range(4)]
        sc.add("pool", lambda e: e.collective_compute("ReduceScatter", ALU.add, replica_groups=GROUPS,
                                                      ins=[part.opt()], outs=[rs_out.opt()]),
               reads=all_sc + partz_all + [("part_x1", i, db_) for i in range(8) for db_ in range(4)], writes=["rs_out"], kind="cc")
        for i in range(8):
            sc.add("sync", lambda e, i=i: e.dma_start(out=out[i * 512:(i + 1) * 512, :], in_=rs_out[i * 512:(i + 1) * 512, :]),
                   reads=["rs_out"], writes=[("out", i)], kind="dma")
        sc.emit(block)
    return nc


def _na_bias_tables(rpb_h):
    ki = np.arange(128)[:, None]
    qi = np.arange(128)[None, :]

    def pat(m, kt):
        qr = 2 * m + qi // GRID_W
        qc = qi % GRID_W
        kr = 2 * kt + ki // GRID_W
        kc = ki % GRID_W
        rs = np.clip(qr - 4, 0, 56)
        cs = np.clip(qc - 8, 0, GRID_W - 16)
        valid = (kr >= rs) & (kr < rs + 8) & (kc >= cs) & (kc < cs + 16)
        ro = np.clip(kr - qr + 7, 0, 14)
        co = np.clip(kc - qc, -15, 15) + 15
        return np.where(valid, rpb_h[ro, co], np.float32(NEG)).astype(np.float32)

    full_mask = np.full((128, 128), NEG, np.float32)
    pats = [pat(10, 10 + d) for d in (-2, -1, 0, 1, 2)]
    for m in (0, 1):
        pats += [pat(m, kt) for kt in (0, 1, 2, 3)] + [full_mask]
    for m in (30, 31):
        pats += [pat(m, kt) for kt in (28, 29, 30, 31)] + [full_mask]
    return np.ascontiguousarray(np.stack(pats, axis=1))


def _alibi_tables(g):
    slope = np.float32(2.0 ** (-8.0 * (g + 1) / 4))
    ki = np.arange(128, dtype=np.float32)[:, None]
    qi = np.arange(256, dtype=np.float32)[None, :]
    t = np.stack([-slope * (qi - ki), slope * (qi - ki), -slope * np.abs(qi - ki), -slope * np.abs(qi - ki - 128.0)],
                 axis=1).astype(np.float32)
    cb = np.zeros((128, 64), np.float32)
    for delta in range(-31, 31):
        if delta >= 1:
            cb[:, delta + 32] = -slope * 128.0 * delta
        elif delta <= -2:
            cb[:, delta + 32] = slope * 128.0 * delta
    return np.ascontiguousarray(t), cb


def _prep_inputs(inp):
    f = lambda a: np.ascontiguousarray(np.asarray(a, dtype=np.float32))
    x = f(inp["x"])
    w_in = f(inp["w_in"])[0]
    w_out = f(inp["w_out"])[0]
    on_a = f(inp["on_a"])[0]
    subln = f(inp["subln_b"])[0]
    rpb = f(inp["rpb_a"])[0]
    wg, wu, wd = np.asarray(inp["w_gate"])[0], np.asarray(inp["w_up"])[0], np.asarray(inp["w_down"])[0]
    ln1T = np.ascontiguousarray(f(inp["ln1_g"])[0].reshape(16, 128).T)
    qkg = np.ascontiguousarray(np.stack([f(inp["qn_a"])[0]] * 2 + [f(inp["kn_a"])[0]] * 2 + [f(inp["qn_b"])[0]] * 2
                                        + [f(inp["kn_b"])[0]] * 2, axis=1))
    lamv = np.ascontiguousarray(np.stack([f(inp["lam_q1"])[0], f(inp["lam_k1"])[0], f(inp["lam_q2"])[0], f(inp["lam_k2"])[0]]))
    ln2 = f(inp["ln2_g"])
    w_router = f(inp["w_router"])[0]
    maps = []
    p = np.arange(128)
    cc_ = np.arange(NT)
    fcv = np.ascontiguousarray(np.broadcast_to((8 * ((cc_ % 8) // 2) + 2 * (cc_ // 8) + (cc_ % 2)).astype(np.float32), (128, NT)))
    for c in range(8):
        b, g = c // 4, c % 4
        cols = np.concatenate([
            np.arange(256 * g, 256 * g + 256), 1024 + np.arange(256 * g, 256 * g + 256),
            3072 + np.arange(128 * g, 128 * g + 128), 3584 + np.arange(128 * g, 128 * g + 128),
            4096 + np.arange(128 * g, 128 * g + 128), 4608 + np.arange(128 * g, 128 * g + 128),
            2048 + np.arange(256 * g, 256 * g + 256), 5120 + np.arange(256 * g, 256 * g + 256)])
        rows, wog_cols = [], []
        for r in range(4):
            rows += [np.arange(256 * r, 256 * r + 256), 1024 + np.arange(256 * r, 256 * r + 256)]
            wog_cols += [on_a[256 * r:256 * r + 128], on_a[256 * r + 128:256 * r + 256], subln[0:128], subln[128:256]]
        rows = np.concatenate(rows)
        alib, cb = _alibi_tables(g)
        sel = np.zeros((128, 4, N_EXP), np.float32)
        for e_ in range(4):
            sel[:, e_, 4 * g + e_] = 1.0
        own = (1024 * g + np.arange(8)[None, :] * 128 + p[:, None]).astype(np.int32)
        mixi = (4096 * g + np.arange(4)[None, None, :] * 1024 + (np.arange(8)[None, :] * 128 + p[:, None])[:, :, None]).astype(np.int32)
        maps.append({
            "x_b": x[b], "x_own": np.ascontiguousarray(x[b, 1024 * g:1024 * (g + 1)]),
            "w_in_g": np.ascontiguousarray(w_in[:, cols]), "ln1T": ln1T, "qkg": qkg,
            "nab": np.ascontiguousarray(np.stack([_na_bias_tables(rpb[2 * g]), _na_bias_tables(rpb[2 * g + 1])])),
            "alib": alib, "cbias": cb, "lamv": lamv,
            "w_out_p": np.ascontiguousarray(w_out[rows]), "wog": np.ascontiguousarray(np.stack(wog_cols, axis=1)),
            "ln2": ln2, "w_router": w_router, "sel": sel, "own_tok": np.ascontiguousarray((own[:, :, None] + S * np.arange(4)[None, None, :]).astype(np.int32)),
            "mix_idx": np.ascontiguousarray(mixi), "fcv": fcv,
            "wg_e": np.ascontiguousarray(wg[4 * g:4 * g + 4], dtype=np.float32),
            "wu_e": np.ascontiguousarray(wu[4 * g:4 * g + 4], dtype=np.float32),
            "wd_e": np.ascontiguousarray(wd[4 * g:4 * g + 4], dtype=np.float32),
        })
    return maps


def kernel(**inputs):
    maps = _prep_inputs(inputs)
    nc = build_nc()
    res = run_bass_kernel_spmd(nc, maps, core_ids=list(range(8)))
    out = np.empty((2, S, D), np.float32)
    for c in range(8):
        b, g = c // 4, c % 4
        out[b, :, 512 * g:512 * (g + 1)] = np.asarray(res.results[c]["out"], dtype=np.float32)
    return out
```

```python
import numpy as np
from contextlib import ExitStack
import concourse.bass as bass
import concourse.mybir as mybir
from concourse.bass_utils import run_bass_kernel_spmd

F32 = mybir.dt.float32
BF16 = mybir.dt.bfloat16
I32 = mybir.dt.int32
U8 = mybir.dt.uint8
AF = mybir.ActivationFunctionType
ALU = mybir.AluOpType
AX = mybir.AxisListType

D = 2048
S = 4096
NT = 32
GRID_W = 64
EPS = 1e-6
N_EXP = 16
CAP = 512
FF = 2816
NFC = FF // 128
GROUPS = [[0, 1, 2, 3], [4, 5, 6, 7]]
SCALE = 128.0 ** -0.5
LAM_INIT = 0.8 - 0.6
NEG = -30000.0
NPAT = 25
BISECT_ITERS = 24


class Sched:
    COMPUTE = ("act", "dve", "pool", "pe")

    def __init__(self, nc, es, rings):
        self.nc = nc
        self.ops = []
        self.lastw = {}
        self.readers = {}
        self.sems = []
        self.csem = {}
        for e in self.COMPUTE:
            self.csem[e] = self._sem(es, "c_" + e)
        self.rings = {e: [self._sem(es, f"d_{e}_{i}") for i in range(k)] for e, k in rings.items()}
        self.dma_count = {e: 0 for e in rings}
        self.es = es
        self.barrier_deps = []
        self.recent = {e: None for e in self.COMPUTE}
        self.recent_dma = {e: [] for e in rings}
        self.cc_ops = []
        self.cc_pool = [self._sem(es, f"cc_{i}") for i in range(12)]

    def _sem(self, es, name):
        self.sems.append(es.enter_context(self.nc.semaphore(name)))
        return len(self.sems) - 1

    def add(self, eng, fn, reads=(), writes=(), kind="c", extra=()):
        oid = len(self.ops)
        deps = {}
        for r in reads:
            w = self.lastw.get(r)
            if w is not None:
                deps.setdefault(w, set()).add("raw")
        for w_ in writes:
            w = self.lastw.get(w_)
            if w is not None:
                deps.setdefault(w, set()).add("waw")
            for rd in self.readers.get(w_, ()):
                deps.setdefault(rd, set()).add("war")
        for d in self.barrier_deps:
            deps.setdefault(d, set()).add("raw")
        for d in extra:
            deps.setdefault(d, set()).add("raw")
        fdeps = []
        for d, types in deps.items():
            p = self.ops[d]
            if p["kind"] == "c" and p["eng"] == eng and kind == "c":
                if eng == "pe" or "raw" not in types:
                    continue
            fdeps.append(d)
        op = dict(id=oid, eng=eng, fn=fn, kind=kind, deps=fdeps, has_dep=False, sem=None, val=0, prev=0)
        if kind == "dma":
            j = self.dma_count[eng]
            self.dma_count[eng] += 1
            K = len(self.rings[eng])
            op["sem"] = self.rings[eng][j % K]
            op["val"] = 16 * (j // K + 1)
            op["prev"] = 16 * (j // K)
            self.recent_dma[eng].append(oid)
            self.recent_dma[eng] = self.recent_dma[eng][-K:]
        elif kind == "cc":
            op["sem"] = self.cc_pool[len(self.cc_ops)]
            op["val"] = 1
            self.cc_ops.append(oid)
        else:
            self.recent[eng] = oid
        for r in reads:
            self.readers.setdefault(r, []).append(oid)
        for w_ in writes:
            self.lastw[w_] = oid
            self.readers[w_] = []
        self.ops.append(op)
        return oid

    def barrier(self, include_cc=True):
        deps = [v for v in self.recent.values() if v is not None]
        for lst in self.recent_dma.values():
            deps += lst
        if include_cc:
            deps += self.cc_ops
        self.barrier_deps = deps

    def emit(self, block):
        ops = self.ops
        for op in ops:
            for d in op["deps"]:
                ops[d]["has_dep"] = True
        cnt = {e: 0 for e in self.COMPUTE}
        for op in ops:
            if op["kind"] == "c" and op["has_dep"]:
                cnt[op["eng"]] += 1
                op["sem"] = self.csem[op["eng"]]
                op["val"] = cnt[op["eng"]]
        for e, c in cnt.items():
            assert c < 60000, (e, c)
        lists = {}
        for op in ops:
            lists.setdefault(op["eng"], []).append(op)
        final = {}
        for op in ops:
            if op["sem"] is not None:
                final[op["sem"]] = max(final.get(op["sem"], 0), op["val"])
        sems = self.sems

        def run(name, eng):
            waited = {}
            for op in lists.get(name, []):
                waits = {}
                for d in op["deps"]:
                    p = ops[d]
                    waits[p["sem"]] = max(waits.get(p["sem"], 0), p["val"])
                if op["kind"] == "dma" and op["prev"] > 0:
                    waits[op["sem"]] = max(waits.get(op["sem"], 0), op["prev"])
                for s_, v in waits.items():
                    if waited.get(s_, 0) < v:
                        eng.wait_ge(sems[s_], v)
                        waited[s_] = v
                ins = op["fn"](eng)
                if op["kind"] == "dma":
                    ins.then_inc(sems[op["sem"]], 16)
                elif op["kind"] == "cc":
                    ins.then_inc(sems[op["sem"]], 1)
                elif op["has_dep"]:
                    ins.then_inc(sems[op["sem"]], 1)
            if name == "sync":
                for s_, v in final.items():
                    if waited.get(s_, 0) < v:
                        eng.wait_ge(sems[s_], v)

        @block.sync
        def _(e):
            run("sync", e)

        @block.scalar
        def _(e):
            run("act", e)

        @block.vector
        def _(e):
            run("dve", e)

        @block.gpsimd
        def _(e):
            run("pool", e)

        @block.tensor
        def _(e):
            run("pe", e)


class Arena:
    def __init__(self, ar, size):
        self.ar = ar
        self.size = size

    def view(self, off, shape, dt):
        esz = {F32: 4, BF16: 2, I32: 4}[dt]
        n = int(np.prod(shape[1:]))
        assert off % 4 == 0 and off + n * esz <= self.size, (off, shape, self.size)
        v = self.ar[:, off:off + n * esz].bitcast(dt)
        if len(shape) == 3:
            v = v.rearrange("p (a b) -> p a b", a=shape[1])
        elif len(shape) == 4:
            v = v.rearrange("p (a b c) -> p a b c", a=shape[1], b=shape[2])
        return v


def bc(ap, shape):
    return ap.unsqueeze(len(ap.shape)).to_broadcast(list(shape))


def build_nc(debug=False, stop_after=None):
    nc = bass.Bass("TRN2", target_bir_lowering=False)

    def din(name, shape, dt=F32):
        return nc.dram_tensor(name, list(shape), dt, kind="ExternalInput").ap()

    x_b = din("x_b", [S, D])
    x_own = din("x_own", [1024, D])
    w_in_g = din("w_in_g", [D, 1536])
    ln1T = din("ln1T", [128, 16])
    qkg = din("qkg", [128, 8])
    nab = din("nab", [2, 128, NPAT, 128])
    alib = din("alib", [128, 4, 256])
    cbias = din("cbias", [128, 64])
    lamv = din("lamv", [4, 128])
    w_out_p = din("w_out_p", [D, D])
    wog = din("wog", [128, 16])
    ln2 = din("ln2", [1, D])
    w_router = din("w_router", [D, N_EXP])
    sel = din("sel", [128, 4, N_EXP])
    fcv = din("fcv", [128, NT])
    own_tok = din("own_tok", [128, 8, 4], I32)
    mix_idx = din("mix_idx", [128, 8, 4], I32)
    if stop_after is None:
        wg_e = din("wg_e", [4, D, FF])
        wu_e = din("wu_e", [4, D, FF])
        wd_e = din("wd_e", [4, FF, D])
    out = nc.dram_tensor("out", [S, 512], F32, kind="ExternalOutput").ap()

    ag1_in = nc.dram_tensor("ag1_in", [S, 512], BF16).ap()
    ag1_out = nc.dram_tensor("ag1_out", [4 * S, 512], BF16).ap()
    h2_in = nc.dram_tensor("h2_in", [1024, D], BF16).ap()
    h2_all = nc.dram_tensor("h2_all", [S, D], BF16).ap()
    aff_in = nc.dram_tensor("aff_in", [1024, N_EXP], F32).ap()
    aff_all = nc.dram_tensor("aff_all", [S, N_EXP], F32).ap()
    part = nc.dram_tensor("part", [4 * S, 512], F32).ap()
    rs_out = nc.dram_tensor("rs_out", [S, 512], F32).ap()
    dbg = {}
    if debug:
        dbg["mix"] = nc.dram_tensor("dbg_mix", [S, 512], BF16, kind="ExternalOutput").ap()
        dbg["x1"] = nc.dram_tensor("dbg_x1", [1024, D], F32, kind="ExternalOutput").ap()
        dbg["aff"] = nc.dram_tensor("dbg_aff", [S, N_EXP], F32, kind="ExternalOutput").ap()
        dbg["idx"] = nc.dram_tensor("dbg_idx", [128, 16], I32, kind="ExternalOutput").ap()
        dbg["gate"] = nc.dram_tensor("dbg_gate", [128, 16], F32, kind="ExternalOutput").ap()

    ARENA = 196 * 1024
    with ExitStack() as es:
        ar_t = es.enter_context(nc.sbuf_tensor("arena", [128, ARENA], U8))
        A = Arena(ar_t, ARENA)
        small = es.enter_context(nc.sbuf_tensor("small", [128, 1024], F32))
        ident_b = es.enter_context(nc.sbuf_tensor("ident_b", [128, 128], BF16))
        ident_f = es.enter_context(nc.sbuf_tensor("ident_f", [128, 128], F32))
        iota_f = es.enter_context(nc.sbuf_tensor("iota_f", [128, 512], F32))
        iota_p = es.enter_context(nc.sbuf_tensor("iota_p", [128, 1], F32))
        zero_b = es.enter_context(nc.sbuf_tensor("zero_b", [128, 32], BF16))
        pbanks = [es.enter_context(nc.psum_tensor(f"pb{i}", [128, 512], F32)) for i in range(8)]
        sc = Sched(nc, es, rings={"sync": 16, "pool": 24})
        block = es.enter_context(nc.Block())

        so = [0]

        def sm(n):
            v = small[:, so[0]:so[0] + n]
            so[0] += n
            assert so[0] <= 1024
            return v

        eps_c = sm(1)
        ones_b = A.view(ARENA - 256, [128, 128], BF16)
        ARENA_USE = ARENA - 256

        def pb(i):
            return pbanks[i][:]

        def pbb(i):
            return pbanks[i][:].bitcast(BF16)

        sc.add("pool", lambda e: e.memset(eps_c, EPS), writes=["eps"])
        sc.add("pool", lambda e: e.iota(iota_f[:], pattern=[[1, 512]], base=0, channel_multiplier=0,
                                        allow_small_or_imprecise_dtypes=True), writes=["iota_f"])
        sc.add("pool", lambda e: e.iota(iota_p[:], pattern=[[0, 1]], base=0, channel_multiplier=1,
                                        allow_small_or_imprecise_dtypes=True), writes=["iota_p"])
        sc.add("dve", lambda e: e.tensor_scalar(out=ident_f[:], in0=iota_f[:, 0:128], scalar1=iota_p[:, 0:1],
                                                scalar2=None, op0=ALU.is_equal),
               reads=["iota_f", "iota_p"], writes=["ident_f"])
        sc.add("dve", lambda e: e.tensor_copy(out=ident_b[:], in_=ident_f[:]), reads=["ident_f"], writes=["ident_b"])
        sc.add("pool", lambda e: e.memset(ones_b, 1.0), writes=["ones_b"])
        sc.add("pool", lambda e: e.memset(zero_b[:], 0.0), writes=["zero_b"])

        QKT = A.view(0, [128, 8, S], BF16)
        VA = A.view(65536, [128, NT, 2, 130], BF16)
        VB = A.view(82176, [128, NT, 258], BF16)
        R1 = 98688
        WP = A.view(R1, [128, 16, 1536], BF16)
        XIN = [A.view(R1 + 49152 + i * 8192, [128, D], F32) for i in range(2)]
        R2 = R1 + 65536
        XS = [A.view(R2 + i * 4096, [128, D], BF16) for i in range(2)]
        XT = [A.view(R2 + 8192 + i * 4096, [128, 16, 128], BF16) for i in range(2)]
        SQ = A.view(R2 + 16384, [128, 8, 128], F32)
        QKB = [A.view(R2 + 20480 + i * 2048, [128, 8, 128], BF16) for i in range(2)]
        JUNK = A.view(R2 + 24576, [128, D], BF16)
        assert R2 + 28672 <= ARENA_USE

        ln1T_s = sm(16)
        qkg_s = sm(8)
        ssx = sm(NT)
        rsx = sm(NT)
        ssqk = sm(8 * 2)
        rsqk = sm(8 * 2)

        sc.add("sync", lambda e: e.dma_start(out=ln1T_s, in_=ln1T), writes=["ln1T"], kind="dma")
        sc.add("sync", lambda e: e.dma_start(out=qkg_s, in_=qkg), writes=["qkg"], kind="dma")
        sc.add("pool", lambda e: e.memset(VA[:, :, :, 128:130], 1.0), writes=["VAones"])
        sc.add("pool", lambda e: e.memset(VB[:, :, 256:258], 1.0), writes=["VBones"])
        w_in_v = w_in_g.rearrange("(kc p) n -> p kc n", p=128)
        for kc in range(16):
            sc.add("pool", lambda e, kc=kc: e.dma_start(out=WP[:, kc, :], in_=w_in_v[:, kc, :]),
                   writes=[("WPraw", kc)], kind="dma")
        for kc in range(16):
            if kc % 2 == 0:
                sc.add("dve", lambda e, kc=kc: e.tensor_scalar(out=WP[:, kc, :], in0=WP[:, kc, :],
                                                               scalar1=ln1T_s[:, kc:kc + 1], scalar2=None, op0=ALU.mult),
                       reads=[("WPraw", kc), "ln1T"], writes=[("WP", kc)])
            else:
                sc.add("act", lambda e, kc=kc: e.activation(out=WP[:, kc, :], in_=WP[:, kc, :], func=AF.Copy,
                                                            scale=ln1T_s[:, kc:kc + 1]),
                       reads=[("WPraw", kc), "ln1T"], writes=[("WP", kc)])

        def a_load(tt):
            sc.add("sync", lambda e: e.dma_start(out=XIN[tt % 2], in_=x_b[tt * 128:(tt + 1) * 128, :]),
                   writes=[("xin", tt % 2)], kind="dma")

        def a_norm(tt):
            s = tt % 2
            sc.add("act", lambda e: e.activation(out=JUNK, in_=XIN[s], func=AF.Square, accum_out=ssx[:, tt:tt + 1]),
                   reads=[("xin", s)], writes=["junk", ("ssx", tt)])
            sc.add("act", lambda e: e.activation(out=rsx[:, tt:tt + 1], in_=ssx[:, tt:tt + 1], func=AF.Sqrt,
                                                 scale=1.0 / D, bias=eps_c),
                   reads=[("ssx", tt), "eps"], writes=[("rsx0", tt)])
            sc.add("dve", lambda e: e.reciprocal(out=rsx[:, tt:tt + 1], in_=rsx[:, tt:tt + 1]),
                   reads=[("rsx0", tt)], writes=[("rsx", tt)])
            sc.add("act", lambda e: e.activation(out=XS[s], in_=XIN[s], func=AF.Copy, scale=rsx[:, tt:tt + 1]),
                   reads=[("xin", s), ("rsx", tt)], writes=[("xs", s)])

        def a_transpose(tt):
            s = tt % 2
            for half in range(2):
                def f(e, half=half):
                    r = None
                    for j in range(8):
                        kc = half * 8 + j
                        r = e.transpose(out=pbb(0)[:, j * 128:(j + 1) * 128], in_=XS[s][:, kc * 128:(kc + 1) * 128],
                                        identity=ident_b[:])
                    return r
                sc.add("pe", f, reads=[("xs", s), "ident_b"], writes=[("pb", 0)])
                sc.add("dve", lambda e, half=half: e.tensor_copy(
                    out=XT[s][:, half * 8:(half + 1) * 8, :],
                    in_=pbb(0).rearrange("p (a b) -> p a b", a=8)),
                    reads=[("pb", 0)], writes=[("xT", s, half)])

        def a_proj(tt):
            s = tt % 2
            base = 2 + 3 * (tt % 2)

            def f(e):
                r = None
                for kc in range(16):
                    for nb in range(3):
                        r = e.matmul(pb(base + nb), lhsT=XT[s][:, kc, :], rhs=WP[:, kc, nb * 512:(nb + 1) * 512],
                                     start=(kc == 0), stop=(kc == 15))
                return r
            sc.add("pe", f, reads=[("xT", s, 0), ("xT", s, 1)] + [("WP", kc) for kc in range(16)],
                   writes=[("pb", base + q_) for q_ in range(3)])

        def a_evac(tt):
            s = tt % 2
            base = 2 + 3 * (tt % 2)
            qk_ps = [pb(base), pb(base + 1)]
            for h2_ in range(2):
                sc.add("act", lambda e, h2_=h2_: e.activation(
                    out=SQ[:, h2_ * 4:(h2_ + 1) * 4, :], in_=qk_ps[h2_].rearrange("p (a b) -> p a b", a=4),
                    func=AF.Square), reads=[("pb", base + h2_)], writes=[("sq", h2_)])
            sc.add("dve", lambda e: e.tensor_reduce(out=ssqk[:, s * 8:(s + 1) * 8], in_=SQ, axis=AX.X, op=ALU.add),
                   reads=[("sq", 0), ("sq", 1)], writes=[("ssqk", s)])
            sc.add("act", lambda e: e.activation(out=rsqk[:, s * 8:(s + 1) * 8], in_=ssqk[:, s * 8:(s + 1) * 8],
                                                 func=AF.Sqrt, scale=1.0 / 128, bias=eps_c),
                   reads=[("ssqk", s), "eps"], writes=[("rsqk0", s)])
            sc.add("dve", lambda e: e.reciprocal(out=rsqk[:, s * 8:(s + 1) * 8], in_=rsqk[:, s * 8:(s + 1) * 8]),
                   reads=[("rsqk0", s)], writes=[("rsqk", s)])
            for h2_ in range(2):
                sc.add("dve", lambda e, h2_=h2_: e.tensor_tensor(
                    out=QKB[s][:, h2_ * 4:(h2_ + 1) * 4, :], in0=qk_ps[h2_].rearrange("p (a b) -> p a b", a=4),
                    in1=bc(rsqk[:, s * 8 + h2_ * 4:s * 8 + (h2_ + 1) * 4], [128, 4, 128]), op=ALU.mult),
                    reads=[("pb", base + h2_), ("rsqk", s)], writes=[("qkb", s, h2_)])
            sc.add("act", lambda e: e.copy(out=VA[:, tt, :, 0:128],
                                           in_=pb(base + 2)[:, 0:256].rearrange("p (a b) -> p a b", a=2)),
                   reads=[("pb", base + 2)], writes=[("VA", tt)])
            sc.add("act", lambda e: e.copy(out=VB[:, tt, 0:256], in_=pb(base + 2)[:, 256:512]),
                   reads=[("pb", base + 2)], writes=[("VB", tt)])

        def a_qkT(tt):
            s = tt % 2

            def f(e):
                r = None
                for j in range(8):
                    r = e.transpose(out=pbb(1)[:, j * 128:(j + 1) * 128], in_=QKB[s][:, j, :], identity=ident_b[:])
                return r
            sc.add("pe", f, reads=[("qkb", s, 0), ("qkb", s, 1), "ident_b"], writes=[("pb", 1)])
            sc.add("dve", lambda e: e.tensor_tensor(out=QKT[:, :, tt * 128:(tt + 1) * 128],
                                                    in0=pbb(1).rearrange("p (a b) -> p a b", a=8),
                                                    in1=bc(qkg_s, [128, 8, 128]), op=ALU.mult),
                   reads=[("pb", 1), "qkg"], writes=[("QKT", tt)])

        ZERO = A.view(R2 + 28672, [128, 1024], F32)
        sc.add("pool", lambda e: e.memset(ZERO, 0.0), writes=["zero"])
        part_v = part.rearrange("(a b p) n -> p a b n", p=128, b=2)

        def zero_fill(a_):
            sc.add("sync", lambda e: e.dma_start(out=part_v[:, a_, :, :], in_=ZERO.rearrange("p (b n) -> p b n", b=2)),
                   reads=["zero"], writes=[("partz", a_)], kind="dma")
        partz_all = [("partz", a_) for a_ in range(64)]
        a_load(0)
        a_load(1)
        a_norm(0)
        a_transpose(0)
        for tt in range(NT):
            a_proj(tt)
            if tt + 1 < NT:
                a_norm(tt + 1)
                a_transpose(tt + 1)
            if tt + 2 < NT:
                a_load(tt + 2)
            zero_fill(2 * tt)
            zero_fill(2 * tt + 1)
            if tt >= 1:
                a_qkT(tt - 1)
            a_evac(tt)
        a_qkT(NT - 1)
        sc.barrier()
        if stop_after == "A":
            sc.add("sync", lambda e: e.dma_start(out=out[0:128, :], in_=XIN[0]), writes=["outdummy"], kind="dma")
            sc.emit(block)
            return nc

        WO = A.view(R1, [128, 16, D], BF16)
        TS = [A.view(R2 + i * 2048, [128, 2, 256], F32) for i in range(3)]
        PT = [A.view(R2 + 6144 + i * 1024, [128, 2, 256], BF16) for i in range(3)]
        ALB = A.view(R2 + 9216, [128, 4, 256], F32)
        OB1 = A.view(R2 + 13312, [128, 256], F32)
        OBN = [A.view(R2 + 14336 + i * 1024, [128, 512], BF16) for i in range(2)]
        NAB = A.view(R2 + 16384, [128, NPAT, 128], F32)
        TNA = A.view(R2 + 29184, [128, 5, 128], F32)
        PNA = A.view(R2 + 31744, [128, 5, 128], BF16)
        LAMT = A.view(R2 + 33024, [128, 4, 128], F32)
        JB = A.view(R2 + 35072, [128, 256], F32)
        assert R2 + 36096 <= ARENA_USE
        cb_s = sm(64)
        wog_s = sm(16)
        lam_s = sm(8)
        dst = sm(64)
        nst = sm(8)

        sc.add("sync", lambda e: e.dma_start(out=ALB, in_=alib), writes=["alb"], kind="dma")
        sc.add("sync", lambda e: e.dma_start(out=cb_s, in_=cbias), writes=["cb"], kind="dma")
        sc.add("sync", lambda e: e.dma_start(out=wog_s, in_=wog), writes=["wog"], kind="dma")
        for i in range(4):
            sc.add("sync", lambda e, i=i: e.dma_start(out=LAMT[:, i, :], in_=lamv[i:i + 1, :].partition_broadcast(128)),
                   writes=[("lamt", i)], kind="dma")
        sc.add("dve", lambda e: e.tensor_tensor(out=LAMT[:, 0, :], in0=LAMT[:, 0, :], in1=LAMT[:, 1, :], op=ALU.mult),
               reads=[("lamt", 0), ("lamt", 1)], writes=["lp1"])
        sc.add("dve", lambda e: e.tensor_tensor(out=LAMT[:, 2, :], in0=LAMT[:, 2, :], in1=LAMT[:, 3, :], op=ALU.mult),
               reads=[("lamt", 2), ("lamt", 3)], writes=["lp2"])
        sc.add("dve", lambda e: e.tensor_reduce(out=lam_s[:, 0:1], in_=LAMT[:, 0, :], axis=AX.X, op=ALU.add),
               reads=["lp1"], writes=["ls1"])
        sc.add("dve", lambda e: e.tensor_reduce(out=lam_s[:, 1:2], in_=LAMT[:, 2, :], axis=AX.X, op=ALU.add),
               reads=["lp2"], writes=["ls2"])
        sc.add("act", lambda e: e.activation(out=lam_s[:, 2:4], in_=lam_s[:, 0:2], func=AF.Exp),
               reads=["ls1", "ls2"], writes=["lexp"])
        sc.add("dve", lambda e: e.tensor_tensor(out=lam_s[:, 4:5], in0=lam_s[:, 3:4], in1=lam_s[:, 2:3], op=ALU.subtract),
               reads=["lexp"], writes=["ldiff"])
        sc.add("dve", lambda e: e.tensor_scalar(out=lam_s[:, 5:6], in0=lam_s[:, 4:5], scalar1=-LAM_INIT, scalar2=None,
                                                op0=ALU.add),
               reads=["ldiff"], writes=["neglam"])

        w_out_v = w_out_p.rearrange("(kc p) n -> p kc n", p=128)
        for kc in range(16):
            sc.add("pool", lambda e, kc=kc: e.dma_start(out=WO[:, kc, :], in_=w_out_v[:, kc, :]),
                   writes=[("WOraw", kc)], kind="dma")
        for kc in range(16):
            mul2 = 1.0 if (kc % 4) < 2 else (1.0 - LAM_INIT)
            sc.add("pool", lambda e, kc=kc, mul2=mul2: e.tensor_scalar(
                out=WO[:, kc, :], in0=WO[:, kc, :], scalar1=wog_s[:, kc:kc + 1], scalar2=mul2, op0=ALU.mult, op1=ALU.mult),
                reads=[("WOraw", kc), "wog"], writes=[("WO", kc)])

        if stop_after == "B0":
            sc.add("sync", lambda e: e.dma_start(out=out[0:128, :], in_=XIN[0]), writes=["outdummy"], kind="dma")
            sc.emit(block)
            return nc
        SB = [5, 6, 7]
        ACC = [0, 1, 2, 3]
        Q1, Q2, K1, K2 = 4, 5, 6, 7

        def b_score(qb, kt, n):
            bk = SB[n % 3]

            def f(e):
                e.matmul(pb(bk)[:, 0:256], lhsT=QKT[:, K1, kt * 128:(kt + 1) * 128],
                         rhs=QKT[:, Q1, qb * 256:(qb + 1) * 256], start=True, stop=True)
                return e.matmul(pb(bk)[:, 256:512], lhsT=QKT[:, K2, kt * 128:(kt + 1) * 128],
                                rhs=QKT[:, Q2, qb * 256:(qb + 1) * 256], start=True, stop=True)
            sc.add("pe", f, reads=[("QKT", kt), ("QKT", 2 * qb), ("QKT", 2 * qb + 1)], writes=[("pb", bk)])

        def b_soft(qb, kt, n):
            bk = SB[n % 3]
            delta = 2 * qb - kt
            if delta >= 1:
                ti = 0
            elif delta <= -2:
                ti = 1
            elif delta == 0:
                ti = 2
            else:
                ti = 3
            ci = delta + 32
            sc.add("dve", lambda e: e.scalar_tensor_tensor(
                out=TS[n % 3], in0=pb(bk).rearrange("p (a b) -> p a b", a=2), scalar=SCALE,
                in1=ALB[:, ti:ti + 1, :].to_broadcast([128, 2, 256]), op0=ALU.mult, op1=ALU.add),
                reads=[("pb", bk), "alb"], writes=[("ts", n % 3)])
            sc.add("act", lambda e: e.activation(out=PT[n % 3], in_=TS[n % 3], func=AF.Exp, bias=cb_s[:, ci:ci + 1]),
                   reads=[("ts", n % 3), "cb"], writes=[("pt", n % 3)])

        def b_pv(qb, kt, n):
            def f(e):
                r = None
                for i in range(2):
                    for j in range(2):
                        r = e.matmul(pb(ACC[i * 2 + j])[:, 0:258], lhsT=PT[n % 3][:, i, j * 128:(j + 1) * 128],
                                     rhs=VB[:, kt, :], start=(kt == 0), stop=(kt == NT - 1))
                return r
            sc.add("pe", f, reads=[("pt", n % 3), ("VB", kt), "VBones"], writes=[("pb", q_) for q_ in range(4)])

        def b_epilogue(qb):
            for j in range(2):
                tq = 2 * qb + j
                a1 = pb(ACC[j])
                a2 = pb(ACC[2 + j])
                st = dst[:, (tq % 4) * 8:(tq % 4) * 8 + 8]
                k = ("dst", tq % 4)
                ka1, ka2 = ("pb", ACC[j]), ("pb", ACC[2 + j])
                sc.add("dve", lambda e, a1=a1, st=st: e.reciprocal(out=st[:, 0:1], in_=a1[:, 256:257]),
                       reads=[ka1], writes=[k + (0,)])
                sc.add("dve", lambda e, a2=a2, st=st: e.reciprocal(out=st[:, 1:2], in_=a2[:, 256:257]),
                       reads=[ka2], writes=[k + (1,)])
                sc.add("dve", lambda e, st=st: e.tensor_tensor(out=st[:, 2:3], in0=st[:, 1:2], in1=lam_s[:, 5:6], op=ALU.mult),
                       reads=[k + (1,), "neglam"], writes=[k + (2,)])
                sc.add("dve", lambda e, a1=a1, st=st: e.tensor_scalar(out=OB1, in0=a1[:, 0:256], scalar1=st[:, 0:1],
                                                                      scalar2=None, op0=ALU.mult),
                       reads=[ka1, k + (0,)], writes=["ob1a"])
                sc.add("dve", lambda e, a2=a2, st=st: e.scalar_tensor_tensor(out=OB1, in0=a2[:, 0:256], scalar=st[:, 2:3],
                                                                             in1=OB1, op0=ALU.mult, op1=ALU.add),
                       reads=[ka2, "ob1a", k + (2,)], writes=["ob1"])
                sc.add("dve", lambda e: e.tensor_tensor(out=JB, in0=OB1, in1=OB1, op=ALU.mult), reads=["ob1"], writes=["jb"])
                sc.add("dve", lambda e, st=st: e.tensor_reduce(out=st[:, 3:4], in_=JB, axis=AX.X, op=ALU.add),
                       reads=["jb"], writes=[k + (3,)])
                sc.add("act", lambda e, st=st: e.activation(out=st[:, 4:5], in_=st[:, 3:4], func=AF.Ln, scale=1.0 / 256,
                                                            bias=eps_c),
                       reads=[k + (3,), "eps"], writes=[k + (4,)])
                sc.add("act", lambda e, st=st: e.activation(out=st[:, 5:6], in_=st[:, 4:5], func=AF.Exp, scale=-0.5),
                       reads=[k + (4,)], writes=[k + (5,)])
                sc.add("dve", lambda e, st=st, tq=tq: e.tensor_scalar(out=OBN[tq % 2][:, 256:512], in0=OB1, scalar1=st[:, 5:6],
                                                                      scalar2=None, op0=ALU.mult),
                       reads=["ob1", k + (5,)], writes=[("obn_b", tq % 2)])
                sc.add("sync", lambda e, tq=tq: e.dma_start(out=ag1_in[tq * 128:(tq + 1) * 128, 256:512],
                                                            in_=OBN[tq % 2][:, 256:512]),
                       reads=[("obn_b", tq % 2)], writes=[("ag1_in_b", tq)], kind="dma")

        if stop_after == "B":
            sc.add("sync", lambda e: e.dma_start(out=out[0:128, :], in_=XIN[0]), writes=["outdummy"], kind="dma")
            sc.emit(block)
            return nc
        NSA, NSB, NO = 5, 6, 4

        def na_tile(hh, m):
            qch, kch = hh, 2 + hh
            if m in (0, 1, 30, 31):
                sp = {0: 0, 1: 1, 30: 2, 31: 3}[m]
                pats = [5 + sp * 5 + i for i in range(5)]
            else:
                pats = list(range(5))
            if m in (0, 1):
                kts = [0, 1, 2, 3, 3]
            elif m in (30, 31):
                kts = [28, 29, 30, 31, 31]
            else:
                kts = [m - 2 + i for i in range(5)]

            def f(e):
                r = None
                for i in range(5):
                    o = pb(NSA)[:, i * 128:(i + 1) * 128] if i < 4 else pb(NSB)[:, 0:128]
                    r = e.matmul(o, lhsT=QKT[:, kch, kts[i] * 128:(kts[i] + 1) * 128],
                                 rhs=QKT[:, qch, m * 128:(m + 1) * 128], start=True, stop=True)
                return r
            sc.add("pe", f, reads=[("QKT", k_) for k_ in set(kts + [m])], writes=[("pb", NSA), ("pb", NSB)])
            contiguous = pats == list(range(pats[0], pats[0] + 5))
            assert contiguous
            p0 = pats[0]
            sc.add("dve", lambda e: e.scalar_tensor_tensor(
                out=TNA[:, 0:4, :], in0=pb(NSA).rearrange("p (a b) -> p a b", a=4), scalar=SCALE,
                in1=NAB[:, p0:p0 + 4, :], op0=ALU.mult, op1=ALU.add),
                reads=[("pb", NSA), ("nab", hh)], writes=["tna0"])
            sc.add("dve", lambda e: e.scalar_tensor_tensor(
                out=TNA[:, 4, :], in0=pb(NSB)[:, 0:128], scalar=SCALE,
                in1=NAB[:, p0 + 4, :], op0=ALU.mult, op1=ALU.add),
                reads=[("pb", NSB), ("nab", hh)], writes=["tna1"])
            sc.add("act", lambda e: e.activation(out=PNA, in_=TNA, func=AF.Exp), reads=["tna0", "tna1"], writes=["pna"])

            def f2(e):
                r = None
                for i in range(5):
                    r = e.matmul(pb(NO)[:, 0:130], lhsT=PNA[:, i, :], rhs=VA[:, kts[i], hh, :],
                                 start=(i == 0), stop=(i == 4))
                return r
            sc.add("pe", f2, reads=["pna", "VAones"] + [("VA", k_) for k_ in set(kts)], writes=[("pb", NO)])
            sc.add("dve", lambda e: e.reciprocal(out=nst[:, hh:hh + 1], in_=pb(NO)[:, 128:129]), reads=[("pb", NO)],
                   writes=[("nst", hh)])
            ob = OBN[m % 2]
            sc.add("dve", lambda e: e.tensor_scalar(out=ob[:, hh * 128:(hh + 1) * 128], in0=pb(NO)[:, 0:128],
                                                    scalar1=nst[:, hh:hh + 1], scalar2=None, op0=ALU.mult),
                   reads=[("pb", NO), ("nst", hh)], writes=[("obn_a", m % 2)])
            sc.add("sync", lambda e: e.dma_start(out=ag1_in[m * 128:(m + 1) * 128, hh * 128:(hh + 1) * 128],
                                                 in_=ob[:, hh * 128:(hh + 1) * 128]),
                   reads=[("obn_a", m % 2)], writes=[("ag1_in_a", hh, m)], kind="dma")

        for hh in range(2):
            sc.add("sync", lambda e, hh=hh: e.dma_start(out=NAB, in_=nab[hh]), writes=[("nab", hh)], kind="dma")
            for m in range(NT):
                na_tile(hh, m)

        def ag_slab(k_):
            rd = [("ag1_in_b", t) for t in range(8 * k_, 8 * k_ + 8)] + \
                 [("ag1_in_a", h_, t) for h_ in range(2) for t in range(8 * k_, 8 * k_ + 8)]
            sc.add("pool", lambda e: e.collective_compute(
                "AllGather", ALU.bypass, replica_groups=GROUPS,
                ins=[ag1_in[1024 * k_:1024 * (k_ + 1), :].opt()], outs=[ag1_out[4096 * k_:4096 * (k_ + 1), :].opt()]),
                reads=rd, writes=[("ag1_out", k_)], kind="cc")

        seq = [(qb, kt) for qb in range(16) for kt in range(NT)]
        LOOK = 2
        for i in range(min(LOOK, len(seq))):
            b_score(seq[i][0], seq[i][1], i)
        for i, (qb, kt) in enumerate(seq):
            if i + LOOK < len(seq):
                b_score(seq[i + LOOK][0], seq[i + LOOK][1], i + LOOK)
            b_soft(qb, kt, i)
            b_pv(qb, kt, i)
            if kt == NT - 1:
                b_epilogue(qb)
                if qb % 4 == 3:
                    ag_slab(qb // 4)

        if stop_after == "C":
            sc.add("sync", lambda e: e.dma_start(out=out[0:128, :], in_=XIN[0]), writes=["outdummy"], kind="dma")
            sc.emit(block)
            return nc
        ag_reads = [("ag1_in_b", t) for t in range(NT)] + [("ag1_in_a", h_, t) for h_ in range(2) for t in range(NT)]
        if debug:
            for t_ in range(NT):
                sc.add("sync", lambda e, t_=t_: e.dma_start(out=dbg["mix"][t_ * 128:(t_ + 1) * 128, :],
                                                            in_=ag1_in[t_ * 128:(t_ + 1) * 128, :]),
                       reads=ag_reads, writes=[("dbg_mix", t_)], kind="dma")
        sc.barrier()
        if stop_after == "attn":
            sc.add("sync", lambda e: e.dma_start(out=out[0:128, :], in_=XIN[0]), writes=["outdummy"], kind="dma")
            sc.emit(block)
            return nc

        P0 = 0
        MIXT = [A.view(P0 + i * 4096, [128, 4, 512], BF16) for i in range(2)]
        MIXN = [A.view(P0 + 8192 + i * 4096, [128, 4, 512], BF16) for i in range(2)]
        MXT = [A.view(P0 + 16384 + i * 4096, [128, 16, 128], BF16) for i in range(2)]
        XO = [A.view(P0 + 24576 + i * 8192, [128, D], F32) for i in range(2)]
        X1 = [A.view(P0 + 40960 + i * 8192, [128, D], F32) for i in range(2)]
        H2F = A.view(P0 + 57344, [128, D], F32)
        H2T = A.view(P0 + 65536, [128, 16, 128], F32)
        H2B = [A.view(P0 + 73728 + i * 4096, [128, D], BF16) for i in range(2)]
        LN2R = A.view(P0 + 81920, [128, D], F32)
        WR = A.view(P0 + 90112, [128, 16, N_EXP], F32)
        JD = A.view(P0 + 95232, [128, D], BF16)
        assert P0 + 95232 + 4096 <= R1 + 65536
        JD = A.view(R2, [128, D], BF16)
        own_s = es.enter_context(nc.sbuf_tensor("own_s", [128, 8, 4], I32))
        mixi_s = es.enter_context(nc.sbuf_tensor("mixi_s", [128, 8, 4], I32))
        dstat = sm(8 * 8)
        logit = sm(8 * 16)
        affs = sm(8 * 16)

        sc.add("sync", lambda e: e.dma_start(out=own_s[:], in_=own_tok), writes=["own"], kind="dma")
        sc.add("sync", lambda e: e.dma_start(out=mixi_s[:], in_=mix_idx), writes=["mixi"], kind="dma")
        sc.add("sync", lambda e: e.dma_start(out=LN2R, in_=ln2[0:1, :].partition_broadcast(128)), writes=["ln2r"], kind="dma")
        sc.add("sync", lambda e: e.dma_start(out=WR, in_=w_router.rearrange("(kc p) n -> p kc n", p=128)),
               writes=["wr"], kind="dma")

        def d_tile(i):
            s = i % 2
            st = dstat[:, i * 8:(i + 1) * 8]
            for r in range(4):
                sc.add("pool", lambda e, r=r: e.indirect_dma_start(
                    out=MIXT[s][:, r, :], out_offset=None, in_=ag1_out,
                    in_offset=bass.IndirectOffsetOnAxis(ap=mixi_s[:, i, r:r + 1], axis=0)),
                    reads=[("ag1_out", k_) for k_ in range(4)] + ["mixi"], writes=[("mixt", s, r)], kind="dma")
            sc.add("sync", lambda e: e.dma_start(out=XO[s], in_=x_own[i * 128:(i + 1) * 128, :]),
                   writes=[("xo", s)], kind="dma")
            mr = [("mixt", s, r) for r in range(4)]
            sc.add("act", lambda e: e.activation(out=JD[:, 0:1024].rearrange("p (a b) -> p a b", a=4),
                                                 in_=MIXT[s][:, :, 0:256], func=AF.Square, accum_out=st[:, 0:1]),
                   reads=mr, writes=["jd", ("dst0", i)])
            sc.add("act", lambda e: e.activation(out=st[:, 1:2], in_=st[:, 0:1], func=AF.Sqrt, scale=1.0 / 1024, bias=eps_c),
                   reads=[("dst0", i), "eps"], writes=[("dst1", i)])
            sc.add("dve", lambda e: e.reciprocal(out=st[:, 2:3], in_=st[:, 1:2]), reads=[("dst1", i)], writes=[("dst2", i)])
            sc.add("dve", lambda e: e.tensor_scalar(out=MIXN[s][:, :, 0:256], in0=MIXT[s][:, :, 0:256], scalar1=st[:, 2:3],
                                                    scalar2=None, op0=ALU.mult),
                   reads=mr + [("dst2", i)], writes=[("mixn_a", s)])
            sc.add("dve", lambda e: e.tensor_copy(out=MIXN[s][:, :, 256:512], in_=MIXT[s][:, :, 256:512]),
                   reads=mr, writes=[("mixn_b", s)])
            for half in range(2):
                def f(e, half=half):
                    r_ = None
                    for j in range(8):
                        kc = half * 8 + j
                        r_ = e.transpose(out=pbb(0)[:, j * 128:(j + 1) * 128],
                                         in_=MIXN[s][:, kc // 4, (kc % 4) * 128:(kc % 4 + 1) * 128], identity=ident_b[:])
                    return r_
                sc.add("pe", f, reads=[("mixn_a", s), ("mixn_b", s), "ident_b"], writes=[("pb", 0)])
                sc.add("act", lambda e, half=half: e.copy(out=MXT[s][:, half * 8:(half + 1) * 8, :],
                                                          in_=pbb(0).rearrange("p (a b) -> p a b", a=8)),
                       reads=[("pb", 0)], writes=[("mxt", s, half)])
            for nb in range(4):
                bk = 1 + (nb % 2)

                def f(e, nb=nb, bk=bk):
                    r_ = None
                    for kc in range(16):
                        r_ = e.matmul(pb(bk), lhsT=MXT[s][:, kc, :], rhs=WO[:, kc, nb * 512:(nb + 1) * 512],
                                      start=(kc == 0), stop=(kc == 15))
                    return r_
                sc.add("pe", f, reads=[("mxt", s, 0), ("mxt", s, 1)] + [("WO", kc) for kc in range(16)],
                       writes=[("pb", bk)])
                sc.add("dve", lambda e, nb=nb, bk=bk: e.tensor_tensor(out=X1[s][:, nb * 512:(nb + 1) * 512], in0=pb(bk),
                                                                      in1=XO[s][:, nb * 512:(nb + 1) * 512], op=ALU.add),
                       reads=[("pb", bk), ("xo", s)], writes=[("x1", s, nb)])
            x1r = [("x1", s, nb) for nb in range(4)]
            for db in range(4):
                sc.add("pool", lambda e, db=db: e.indirect_dma_start(
                    out=part, out_offset=bass.IndirectOffsetOnAxis(ap=own_s[:, i, db:db + 1], axis=0),
                    in_=X1[s][:, db * 512:(db + 1) * 512], in_offset=None),
                    reads=x1r + ["own"] + partz_all, writes=[("part_x1", i, db)], kind="dma")
            if debug:
                sc.add("sync", lambda e: e.dma_start(out=dbg["x1"][i * 128:(i + 1) * 128, :], in_=X1[s]),
                       reads=x1r, writes=[("dbgx1", i)], kind="dma")
            sc.add("act", lambda e: e.activation(out=JD, in_=X1[s], func=AF.Square, accum_out=st[:, 3:4]),
                   reads=x1r, writes=["jd", ("dst3", i)])
            sc.add("act", lambda e: e.activation(out=st[:, 4:5], in_=st[:, 3:4], func=AF.Sqrt, scale=1.0 / D, bias=eps_c),
                   reads=[("dst3", i), "eps"], writes=[("dst4", i)])
            sc.add("dve", lambda e: e.reciprocal(out=st[:, 5:6], in_=st[:, 4:5]), reads=[("dst4", i)], writes=[("dst5", i)])
            sc.add("dve", lambda e: e.scalar_tensor_tensor(out=H2F, in0=X1[s], scalar=st[:, 5:6], in1=LN2R,
                                                           op0=ALU.mult, op1=ALU.mult),
                   reads=x1r + [("dst5", i), "ln2r"], writes=["h2f"])
            sc.add("act", lambda e: e.copy(out=H2B[s], in_=H2F), reads=["h2f"], writes=[("h2b", s)])
            sc.add("sync", lambda e: e.dma_start(out=h2_in[i * 128:(i + 1) * 128, :], in_=H2B[s]),
                   reads=[("h2b", s)], writes=[("h2_in", i)], kind="dma")
            for q4 in range(4):
                def f(e, q4=q4):
                    r_ = None
                    for j in range(4):
                        kc = q4 * 4 + j
                        r_ = e.transpose(out=pb(3 + (q4 % 2))[:, j * 128:(j + 1) * 128], in_=H2F[:, kc * 128:(kc + 1) * 128],
                                         identity=ident_f[:])
                    return r_
                sc.add("pe", f, reads=["h2f", "ident_f"], writes=[("pb", 3 + (q4 % 2))])
                sc.add("act", lambda e, q4=q4: e.copy(out=H2T[:, q4 * 4:(q4 + 1) * 4, :],
                                                      in_=pb(3 + (q4 % 2)).rearrange("p (a b) -> p a b", a=4)),
                       reads=[("pb", 3 + (q4 % 2))], writes=[("h2t", q4)])

            def fl(e):
                r_ = None
                for kc in range(16):
                    r_ = e.matmul(pb(5)[:, 0:N_EXP], lhsT=H2T[:, kc, :], rhs=WR[:, kc, :], start=(kc == 0), stop=(kc == 15))
                return r_
            sc.add("pe", fl, reads=[("h2t", q4) for q4 in range(4)] + ["wr"], writes=[("pb", 5)])
            lg = logit[:, i * 16:(i + 1) * 16]
            af = affs[:, i * 16:(i + 1) * 16]
            sc.add("dve", lambda e: e.tensor_reduce(out=st[:, 6:7], in_=pb(5)[:, 0:N_EXP], axis=AX.X, op=ALU.max),
                   reads=[("pb", 5)], writes=[("dst6", i)])
            sc.add("dve", lambda e: e.tensor_scalar(out=lg, in0=pb(5)[:, 0:N_EXP], scalar1=st[:, 6:7], scalar2=None,
                                                    op0=ALU.subtract),
                   reads=[("pb", 5), ("dst6", i)], writes=[("lg", i)])
            sc.add("act", lambda e: e.activation(out=lg, in_=lg, func=AF.Exp, accum_out=st[:, 7:8]),
                   reads=[("lg", i)], writes=[("lge", i), ("dst7", i)])
            sc.add("dve", lambda e: e.reciprocal(out=st[:, 7:8], in_=st[:, 7:8]), reads=[("dst7", i)], writes=[("dst7r", i)])
            sc.add("dve", lambda e: e.tensor_scalar(out=af, in0=lg, scalar1=st[:, 7:8], scalar2=None, op0=ALU.mult),
                   reads=[("lge", i), ("dst7r", i)], writes=[("aff", i)])
            sc.add("sync", lambda e: e.dma_start(out=aff_in[i * 128:(i + 1) * 128, :], in_=af),
                   reads=[("aff", i)], writes=[("aff_in", i)], kind="dma")

        for i in range(8):
            d_tile(i)
        sc.add("pool", lambda e: e.collective_compute("AllGather", ALU.bypass, replica_groups=GROUPS,
                                                      ins=[aff_in.opt()], outs=[aff_all.opt()]),
               reads=[("aff_in", i) for i in range(8)], writes=["aff_all"], kind="cc")
        for j_ in range(4):
            sc.add("pool", lambda e, j_=j_: e.collective_compute(
                "AllGather", ALU.bypass, replica_groups=GROUPS,
                ins=[h2_in[256 * j_:256 * (j_ + 1), :].opt()], outs=[h2_all[1024 * j_:1024 * (j_ + 1), :].opt()]),
                reads=[("h2_in", i) for i in range(8)], writes=[("h2_all", j_)], kind="cc")
        if debug:
            sc.add("sync", lambda e: e.dma_start(out=dbg["aff"], in_=aff_all), reads=["aff_all"], writes=["dbg_aff"], kind="dma")
        sc.barrier(include_cc=False)
        if stop_after == "router":
            sc.add("sync", lambda e: e.dma_start(out=out[0:128, :], in_=X1[0]), writes=["outdummy"], kind="dma")
            sc.emit(block)
            return nc

        E0 = 0
        XSG = A.view(E0, [128, 4, D], BF16)
        XST = A.view(E0 + 16384, [128, 16, 512], BF16)
        HT = A.view(E0 + 32768, [128, NFC, 512], BF16)
        YT = [A.view(E0 + 55296 + i * 2048, [128, 512], F32) for i in range(4)]
        ST_ = [A.view(E0 + 63488 + i * 1024, [128, 512], BF16) for i in range(4)]
        SG = A.view(E0 + 67584, [128, 512], F32)
        SG2 = A.view(E0 + 69632, [128, 512], F32)
        NGU = 5
        WG = [A.view(E0 + 71680 + i * 8192, [128, 16, 128], BF16) for i in range(NGU)]
        WU = [A.view(E0 + 71680 + i * 8192 + 4096, [128, 16, 128], BF16) for i in range(NGU)]
        WDO = E0 + 71680 + NGU * 8192
        WD = [A.view(WDO + i * 22528, [128, NFC, 512], BF16) for i in range(2)]
        A16 = A.view(WDO + 45056, [128, NT, N_EXP], F32)
        SELT = A.view(WDO + 47104, [128, 4, N_EXP], F32)
        PRD = A.view(WDO + 47360, [128, NT, N_EXP], F32)
        A4 = A.view(WDO + 49408, [128, 4, NT], F32)
        CMP = A.view(WDO + 49920, [128, 4, NT], F32)
        AP3 = A.view(WDO + 50432, [128, 4, NT, 6], BF16)
        RES = A.view(WDO + 54400, [128, 4, NT], F32)
        UTRI = A.view(WDO + 52224, [128, 128], F32)
        LT32 = A.view(WDO + 52736, [128, NT], F32)
        MSK = A.view(WDO + 53376, [128, 4, NT], F32)
        POS = A.view(WDO + 53888, [128, 4, NT], F32)
        FCV = A.view(WDO + 54912, [128, NT], F32)
        ONESF = A.view(WDO + 55040, [128, 4], F32)
        CSB = A.view(WDO + 55296, [128, 4, 128], F32)
        assert WDO + 57344 <= ARENA_USE, WDO + 57344
        thr = sm(4)
        cand = sm(4)
        cntp = es.enter_context(nc.sbuf_tensor("cntp", [128, 4], BF16))
        ge = sm(4)
        idxf = sm(80)
        pselc = sm(128)
        gts = sm(16)
        idx_i = es.enter_context(nc.sbuf_tensor("idx_i", [128, 4, 20], I32))
        pidx = es.enter_context(nc.sbuf_tensor("pidx", [128, NT], F32))

        gu_n = [0]

        def load_gu(e_, fc):
            k = gu_n[0] % NGU
            gu_n[0] += 1
            gv = wg_e[e_].rearrange("(kc p) n -> p kc n", p=128)
            uv = wu_e[e_].rearrange("(kc p) n -> p kc n", p=128)
            sc.add("pool", lambda e: e.dma_start(out=WG[k], in_=gv[:, :, fc * 128:(fc + 1) * 128]),
                   writes=[("wg", k)], kind="dma")
            sc.add("pool", lambda e: e.dma_start(out=WU[k], in_=uv[:, :, fc * 128:(fc + 1) * 128]),
                   writes=[("wu", k)], kind="dma")
            return k

        wd_n = [0]

        def load_wd(e_, db):
            k = wd_n[0] % 2
            wd_n[0] += 1
            dv = wd_e[e_].rearrange("(fc p) n -> p fc n", p=128)
            for h_ in range(2):
                sc.add("pool", lambda e, h_=h_: e.dma_start(out=WD[k][:, h_ * 11:(h_ + 1) * 11, :],
                                                            in_=dv[:, h_ * 11:(h_ + 1) * 11, db * 512:(db + 1) * 512]),
                       writes=[("wd", k, h_)], kind="dma")
            return k

        sc.add("sync", lambda e: e.dma_start(out=A16, in_=aff_all.rearrange("(c p) j -> p c j", p=128)),
               reads=["aff_all"], writes=["a16"], kind="dma")
        sc.add("sync", lambda e: e.dma_start(out=SELT, in_=sel), writes=["selt"], kind="dma")
        for e_ in range(4):
            sc.add("dve", lambda e, e_=e_: e.tensor_tensor(out=PRD, in0=A16, in1=SELT[:, e_:e_ + 1, :].to_broadcast([128, NT, N_EXP]),
                                                           op=ALU.mult),
                   reads=["a16", "selt"], writes=["prd"])
            sc.add("dve", lambda e, e_=e_: e.tensor_reduce(out=A4[:, e_, :], in_=PRD, axis=AX.X, op=ALU.add),
                   reads=["prd"], writes=[("a4", e_)])
        a4r = [("a4", e_) for e_ in range(4)]
        sc.add("dve", lambda e: e.tensor_scalar(out=UTRI, in0=iota_f[:, 0:128], scalar1=iota_p[:, 0:1], scalar2=None,
                                                op0=ALU.is_gt), reads=["iota_f", "iota_p"], writes=["utri"])
        sc.add("dve", lambda e: e.tensor_scalar(out=LT32, in0=iota_f[:, 0:NT], scalar1=iota_p[:, 0:1], scalar2=None,
                                                op0=ALU.is_gt), reads=["iota_f", "iota_p"], writes=["lt32"])
        sc.add("dve", lambda e: e.tensor_copy(out=pidx[:], in_=iota_p[:, 0:1].to_broadcast([128, NT])),
               reads=["iota_p"], writes=["pidx"])
        sc.add("dve", lambda e: e.tensor_copy(out=AP3[:, :, :, 0], in_=iota_f[:, 0:NT].unsqueeze(1).to_broadcast([128, 4, NT])),
               reads=["iota_f"], writes=["ap3_0"])
        sc.add("sync", lambda e: e.dma_start(out=FCV, in_=fcv), writes=["fcv"], kind="dma")
        sc.add("pool", lambda e: e.memset(ONESF, 1.0), writes=["ones_f"])
        sc.add("dve", lambda e: e.tensor_copy(out=AP3[:, :, :, 1], in_=FCV.unsqueeze(1).to_broadcast([128, 4, NT])),
               reads=["fcv"], writes=["ap3_1"])
        sc.add("dve", lambda e: e.tensor_copy(out=AP3[:, :, :, 2], in_=pidx[:].unsqueeze(1).to_broadcast([128, 4, NT])),
               reads=["pidx"], writes=["ap3_2"])
        sc.add("dve", lambda e: e.tensor_copy(out=AP3[:, :, :, 3], in_=A4), reads=a4r, writes=["ap3_3"])
        sc.add("dve", lambda e: e.tensor_tensor(out=RES, in0=A4, in1=AP3[:, :, :, 3], op=ALU.subtract),
               reads=a4r + ["ap3_3"], writes=["res1"])
        sc.add("dve", lambda e: e.tensor_copy(out=AP3[:, :, :, 4], in_=RES), reads=["res1"], writes=["ap3_4"])
        sc.add("dve", lambda e: e.tensor_tensor(out=RES, in0=RES, in1=AP3[:, :, :, 4], op=ALU.subtract),
               reads=["res1", "ap3_4"], writes=["res2"])
        sc.add("dve", lambda e: e.tensor_copy(out=AP3[:, :, :, 5], in_=RES), reads=["res2"], writes=["ap3_5"])
        ap3r = ["ap3_0", "ap3_1", "ap3_2", "ap3_3", "ap3_4", "ap3_5"]

        sc.add("dve", lambda e: e.memset(thr, 0.0), writes=["thr"])
        for it in range(1, BISECT_ITERS + 1):
            step = 2.0 ** (-it)
            sc.add("dve", lambda e, step=step: e.tensor_scalar(out=cand, in0=thr, scalar1=step, scalar2=None, op0=ALU.add),
                   reads=["thr"], writes=["cand"])
            sc.add("dve", lambda e: e.tensor_tensor(out=CMP, in0=A4, in1=bc(cand, [128, 4, NT]), op=ALU.is_gt),
                   reads=a4r + ["cand"], writes=["cmp"])
            def fcnt(e):
                with nc.allow_low_precision(reason="per-partition counts <= 32 are exact in bf16"):
                    return e.tensor_reduce(out=cntp[:], in_=CMP, axis=AX.X, op=ALU.add)
            sc.add("dve", fcnt, reads=["cmp"], writes=["cntp"])
            sc.add("pe", lambda e: e.matmul(pb(7)[:, 0:4], lhsT=ones_b, rhs=cntp[:], start=True, stop=True),
                   reads=["cntp", "ones_b"], writes=[("pb", 7)])
            sc.add("dve", lambda e: e.tensor_scalar(out=ge, in0=pb(7)[:, 0:4], scalar1=float(CAP) - 0.5, scalar2=None,
                                                    op0=ALU.is_gt), reads=[("pb", 7)], writes=["ge"])
            sc.add("dve", lambda e, step=step: e.scalar_tensor_tensor(out=thr, in0=ge, scalar=step, in1=thr,
                                                                      op0=ALU.mult, op1=ALU.add),
                   reads=["ge", "thr"], writes=["thr"])
        sc.add("dve", lambda e: e.tensor_tensor(out=MSK, in0=A4, in1=bc(thr, [128, 4, NT]), op=ALU.is_gt),
               reads=a4r + ["thr"], writes=["msk"])

        for e_ in range(4):
            sc.add("pe", lambda e, e_=e_: e.matmul(pb(6)[0:NT, e_:e_ + 1], lhsT=MSK[:, e_, :], rhs=ONESF[:, 0:1],
                                                   start=True, stop=True),
                   reads=["msk", "ones_f"], writes=[("pb", 6)])
        for e_ in range(4):
            sc.add("dve", lambda e, e_=e_: e.tensor_copy(out=CSB[0:NT, e_, :], in_=pb(6)[0:NT, e_:e_ + 1].to_broadcast([NT, 128])),
                   reads=[("pb", 6)], writes=[("csb", e_)])
        for e_ in range(4):
            def fpos(e, e_=e_):
                e.matmul(pb(7)[:, e_ * NT:(e_ + 1) * NT], lhsT=UTRI, rhs=MSK[:, e_, :], start=True, stop=False)
                return e.matmul(pb(7)[:, e_ * NT:(e_ + 1) * NT], lhsT=CSB[0:NT, e_, :], rhs=LT32[0:NT, :], start=False, stop=True)
            sc.add("pe", fpos, reads=["msk", "utri", "lt32", ("csb", e_)], writes=[("pb", 7)])
        sc.add("dve", lambda e: e.scalar_tensor_tensor(out=POS, in0=pb(7)[:, 0:4 * NT].rearrange("p (a b) -> p a b", a=4),
                                                       scalar=1.0, in1=MSK, op0=ALU.add, op1=ALU.mult),
               reads=[("pb", 7), "msk"], writes=["pos0"])
        sc.add("dve", lambda e: e.tensor_scalar(out=POS, in0=POS, scalar1=-1.0, scalar2=None, op0=ALU.add),
               reads=["pos0"], writes=["pos"])

        gu_list = [(e_, fc) for e_ in range(4) for fc in range(NFC)]
        wd_list = [(e_, db) for e_ in range(4) for db in range(4)]
        gu_loaded, wd_loaded = [], []

        def gu_prefetch(upto):
            while len(gu_loaded) < min(upto, len(gu_list)):
                gu_loaded.append(load_gu(*gu_list[len(gu_loaded)]))

        def wd_prefetch(upto):
            while len(wd_loaded) < min(upto, len(wd_list)):
                wd_loaded.append(load_wd(*wd_list[len(wd_loaded)]))

        gu_prefetch(NGU - 1)
        wd_prefetch(1)
        prev_scatter = []
        h2r = [("h2_all", j_) for j_ in range(4)]
        def expert(e_, prev_scatter):
            for c in range(NT):
                stile = ST_[c % 4]
                sc.add("dve", lambda e, c=c, stile=stile: e.tensor_scalar(out=stile, in0=iota_f[:], scalar1=POS[:, e_, c:c + 1],
                                                                          scalar2=None, op0=ALU.is_equal),
                       reads=["pos", "iota_f"], writes=[("stile", c % 4)])

                def fsel(e, c=c, stile=stile):
                    r_ = None
                    if c == 0:
                        e.matmul(pb(6)[:, 0:32], lhsT=ident_b[:], rhs=zero_b[:], start=True, stop=False)
                    for sg in range(4):
                        r_ = e.matmul(pb(6)[:, sg * 8:sg * 8 + 6], lhsT=stile[:, sg * 128:(sg + 1) * 128],
                                      rhs=AP3[:, e_, c, :], start=False, stop=(c == NT - 1))
                    return r_
                sc.add("pe", fsel, reads=[("stile", c % 4), "zero_b", "ident_b"] + ap3r, writes=[("pb", 6)])
            ik = ("idx", e_)
            psc = pselc[:, e_ * 32:(e_ + 1) * 32]
            sc.add("dve", lambda e, psc=psc: e.tensor_copy(out=psc, in_=pb(6)[:, 0:32]), reads=[("pb", 6)], writes=[("psc", e_)])
            psv = psc.rearrange("p (a b) -> p a b", a=4)
            fb = e_ * 20
            sc.add("dve", lambda e, psv=psv, fb=fb: e.scalar_tensor_tensor(out=idxf[:, fb:fb + 4], in0=psv[:, :, 0], scalar=128.0,
                                                                           in1=psv[:, :, 2], op0=ALU.mult, op1=ALU.add),
                   reads=[("psc", e_)], writes=[ik + (0,)])
            for db in range(1, 4):
                sc.add("dve", lambda e, fb=fb, db=db: e.tensor_scalar(out=idxf[:, fb + 4 * db:fb + 4 * db + 4], in0=idxf[:, fb:fb + 4],
                                                                      scalar1=float(S * db), scalar2=None, op0=ALU.add),
                       reads=[ik + (0,)], writes=[ik + (0, db)])
            sc.add("dve", lambda e, psv=psv, fb=fb: e.scalar_tensor_tensor(out=idxf[:, fb + 16:fb + 20], in0=psv[:, :, 1], scalar=128.0,
                                                                           in1=psv[:, :, 2], op0=ALU.mult, op1=ALU.add),
                   reads=[("psc", e_)], writes=[ik + (1,)])
            sc.add("dve", lambda e, fb=fb: e.tensor_copy(out=idx_i[:, e_, :], in_=idxf[:, fb:fb + 20]),
                   reads=[ik + (0,), ik + (1,)] + [ik + (0, db) for db in range(1, 4)], writes=[ik])
            sc.add("dve", lambda e, psv=psv: e.tensor_reduce(out=gts[:, e_ * 4:(e_ + 1) * 4], in_=psv[:, :, 3:6], axis=AX.X, op=ALU.add),
                   reads=[("psc", e_)], writes=[("gate", e_)])
            if debug:
                sc.add("sync", lambda e: e.dma_start(out=dbg["idx"][:, e_ * 4:(e_ + 1) * 4], in_=idx_i[:, e_, 0:4]),
                       reads=[ik], writes=[("dbgidx", e_)], kind="dma")
                sc.add("sync", lambda e: e.dma_start(out=dbg["gate"][:, e_ * 4:(e_ + 1) * 4], in_=gts[:, e_ * 4:(e_ + 1) * 4]),
                       reads=[("gate", e_)], writes=[("dbggate", e_)], kind="dma")
            for sg in range(4):
                sc.add("pool", lambda e, sg=sg: e.indirect_dma_start(
                    out=XSG[:, sg, :], out_offset=None, in_=h2_all,
                    in_offset=bass.IndirectOffsetOnAxis(ap=idx_i[:, e_, 16 + sg:17 + sg], axis=0)),
                    reads=h2r + [ik], writes=[("xsg", sg)], kind="dma")
            for sg in range(4):
                for half in range(2):
                    bk = (sg * 2 + half) % 2

                    def ftr(e, sg=sg, half=half, bk=bk):
                        r_ = None
                        for j in range(8):
                            kc = half * 8 + j
                            r_ = e.transpose(out=pbb(bk)[:, j * 128:(j + 1) * 128], in_=XSG[:, sg, kc * 128:(kc + 1) * 128],
                                             identity=ident_b[:])
                        return r_
                    sc.add("pe", ftr, reads=[("xsg", sg), "ident_b"], writes=[("pb", bk)])
                    ce = "act" if half == 0 else "dve"
                    if ce == "act":
                        sc.add("act", lambda e, sg=sg, half=half, bk=bk: e.copy(
                            out=XST[:, half * 8:(half + 1) * 8, sg * 128:(sg + 1) * 128],
                            in_=pbb(bk).rearrange("p (a b) -> p a b", a=8)),
                            reads=[("pb", bk)], writes=[("xst", sg, half)])
                    else:
                        sc.add("dve", lambda e, sg=sg, half=half, bk=bk: e.tensor_copy(
                            out=XST[:, half * 8:(half + 1) * 8, sg * 128:(sg + 1) * 128],
                            in_=pbb(bk).rearrange("p (a b) -> p a b", a=8)),
                            reads=[("pb", bk)], writes=[("xst", sg, half)])
            xstr = [("xst", sg, half) for sg in range(4) for half in range(2)]
            for fc in range(NFC):
                n_ = e_ * NFC + fc
                gu_prefetch(n_ + NGU)
                k = gu_loaded[n_]
                pg, pu = 2 + 2 * (fc % 2), 3 + 2 * (fc % 2)

                def fgu(e, k=k, pg=pg, pu=pu):
                    r_ = None
                    for kc in range(16):
                        r_ = e.matmul(pb(pg), lhsT=WG[k][:, kc, :], rhs=XST[:, kc, :], start=(kc == 0), stop=(kc == 15))
                    for kc in range(16):
                        r_ = e.matmul(pb(pu), lhsT=WU[k][:, kc, :], rhs=XST[:, kc, :], start=(kc == 0), stop=(kc == 15))
                    return r_
                sc.add("pe", fgu, reads=xstr + [("wg", k), ("wu", k)], writes=[("pb", pg), ("pb", pu)])
                sgt = SG if fc % 2 == 0 else SG2
                sc.add("act", lambda e, pg=pg, sgt=sgt: e.activation(out=sgt, in_=pb(pg), func=AF.Silu),
                       reads=[("pb", pg)], writes=[("sg", fc % 2)])
                sc.add("dve", lambda e, pu=pu, sgt=sgt, fc=fc: e.tensor_tensor(out=HT[:, fc, :], in0=sgt, in1=pb(pu), op=ALU.mult),
                       reads=[("sg", fc % 2), ("pb", pu)], writes=[("ht", fc)])
            htr = [("ht", fc) for fc in range(NFC)]
            scat = []
            for db in range(4):
                nd = e_ * 4 + db
                wd_prefetch(nd + 2)
                kd = wd_loaded[nd]
                for sg in range(4):
                    m_ = db * 4 + sg
                    bk = m_ % 2

                    def fdn(e, sg=sg, kd=kd, bk=bk):
                        r_ = None
                        for fc in range(NFC):
                            r_ = e.matmul(pb(bk), lhsT=HT[:, fc, sg * 128:(sg + 1) * 128], rhs=WD[kd][:, fc, :],
                                          start=(fc == 0), stop=(fc == NFC - 1))
                        return r_
                    sc.add("pe", fdn, reads=htr + [("wd", kd, 0), ("wd", kd, 1)], writes=[("pb", bk)])
                    yt = YT[m_ % 4]
                    if m_ % 2 == 0:
                        sc.add("act", lambda e, bk=bk, yt=yt, sg=sg: e.activation(out=yt, in_=pb(bk), func=AF.Copy,
                                                                                  scale=gts[:, e_ * 4 + sg:e_ * 4 + sg + 1]),
                               reads=[("pb", bk), ("gate", e_)], writes=[("yt", m_ % 4)])
                    else:
                        sc.add("dve", lambda e, bk=bk, yt=yt, sg=sg: e.tensor_scalar(out=yt, in0=pb(bk),
                                                                                     scalar1=gts[:, e_ * 4 + sg:e_ * 4 + sg + 1],
                                                                                     scalar2=None, op0=ALU.mult),
                               reads=[("pb", bk), ("gate", e_)], writes=[("yt", m_ % 4)])
                    scat.append(sc.add("pool", lambda e, db=db, sg=sg, yt=yt: e.indirect_dma_start(
                        out=part,
                        out_offset=bass.IndirectOffsetOnAxis(ap=idx_i[:, e_, db * 4 + sg:db * 4 + sg + 1], axis=0),
                        in_=yt, in_offset=None, compute_op=ALU.add),
                        reads=[("yt", m_ % 4), ik] + partz_all + [("part_x1", i, db_) for i in range(8) for db_ in range(4)],
                        writes=[("part_sc", e_, db, sg)], kind="dma", extra=prev_scatter))
            return scat

        for e_ in range(4):
            prev_scatter = expert(e_, prev_scatter)
        all_sc = [("part_sc", e_, db, sg) for e_ in range(4) for db in range(4) for sg in range(4)]
        sc.add("pool", lambda e: e.collective_compute("ReduceScatter", ALU.add, replica_groups=GROUPS,
                                                      ins=[part.opt()], outs=[rs_out.opt()]),
               reads=all_sc + partz_all + [("part_x1", i, db_) for i in range(8) for db_ in range(4)], writes=["rs_out"], kind="cc")
        for i in range(8):
            sc.add("sync" if i % 2 == 0 else "pool",
                   lambda e, i=i: e.dma_start(out=out[i * 512:(i + 1) * 512, :], in_=rs_out[i * 512:(i + 1) * 512, :]),
                   reads=["rs_out"], writes=[("out", i)], kind="dma")
        sc.emit(block)
    return nc


def _na_bias_tables(rpb_h):
    ki = np.arange(128)[:, None]
    qi = np.arange(128)[None, :]

    def pat(m, kt):
        qr = 2 * m + qi // GRID_W
        qc = qi % GRID_W
        kr = 2 * kt + ki // GRID_W
        kc = ki % GRID_W
        rs = np.clip(qr - 4, 0, 56)
        cs = np.clip(qc - 8, 0, GRID_W - 16)
        valid = (kr >= rs) & (kr < rs + 8) & (kc >= cs) & (kc < cs + 16)
        ro = np.clip(kr - qr + 7, 0, 14)
        co = np.clip(kc - qc, -15, 15) + 15
        return np.where(valid, rpb_h[ro, co], np.float32(NEG)).astype(np.float32)

    full_mask = np.full((128, 128), NEG, np.float32)
    pats = [pat(10, 10 + d) for d in (-2, -1, 0, 1, 2)]
    for m in (0, 1):
        pats += [pat(m, kt) for kt in (0, 1, 2, 3)] + [full_mask]
    for m in (30, 31):
        pats += [pat(m, kt) for kt in (28, 29, 30, 31)] + [full_mask]
    return np.ascontiguousarray(np.stack(pats, axis=1))


def _alibi_tables(g):
    slope = np.float32(2.0 ** (-8.0 * (g + 1) / 4))
    ki = np.arange(128, dtype=np.float32)[:, None]
    qi = np.arange(256, dtype=np.float32)[None, :]
    t = np.stack([-slope * (qi - ki), slope * (qi - ki), -slope * np.abs(qi - ki), -slope * np.abs(qi - ki - 128.0)],
                 axis=1).astype(np.float32)
    cb = np.zeros((128, 64), np.float32)
    for delta in range(-31, 31):
        if delta >= 1:
            cb[:, delta + 32] = -slope * 128.0 * delta
        elif delta <= -2:
            cb[:, delta + 32] = slope * 128.0 * delta
    return np.ascontiguousarray(t), cb


def _prep_inputs(inp):
    f = lambda a: np.ascontiguousarray(np.asarray(a, dtype=np.float32))
    x = f(inp["x"])
    w_in = f(inp["w_in"])[0]
    w_out = f(inp["w_out"])[0]
    on_a = f(inp["on_a"])[0]
    subln = f(inp["subln_b"])[0]
    rpb = f(inp["rpb_a"])[0]
    wg, wu, wd = np.asarray(inp["w_gate"])[0], np.asarray(inp["w_up"])[0], np.asarray(inp["w_down"])[0]
    ln1T = np.ascontiguousarray(f(inp["ln1_g"])[0].reshape(16, 128).T)
    qkg = np.ascontiguousarray(np.stack([f(inp["qn_a"])[0]] * 2 + [f(inp["kn_a"])[0]] * 2 + [f(inp["qn_b"])[0]] * 2
                                        + [f(inp["kn_b"])[0]] * 2, axis=1))
    lamv = np.ascontiguousarray(np.stack([f(inp["lam_q1"])[0], f(inp["lam_k1"])[0], f(inp["lam_q2"])[0], f(inp["lam_k2"])[0]]))
    ln2 = f(inp["ln2_g"])
    w_router = f(inp["w_router"])[0]
    maps = []
    p = np.arange(128)
    cc_ = np.arange(NT)
    fcv = np.ascontiguousarray(np.broadcast_to((8 * ((cc_ % 8) // 2) + 2 * (cc_ // 8) + (cc_ % 2)).astype(np.float32), (128, NT)))
    for c in range(8):
        b, g = c // 4, c % 4
        cols = np.concatenate([
            np.arange(256 * g, 256 * g + 256), 1024 + np.arange(256 * g, 256 * g + 256),
            3072 + np.arange(128 * g, 128 * g + 128), 3584 + np.arange(128 * g, 128 * g + 128),
            4096 + np.arange(128 * g, 128 * g + 128), 4608 + np.arange(128 * g, 128 * g + 128),
            2048 + np.arange(256 * g, 256 * g + 256), 5120 + np.arange(256 * g, 256 * g + 256)])
        rows, wog_cols = [], []
        for r in range(4):
            rows += [np.arange(256 * r, 256 * r + 256), 1024 + np.arange(256 * r, 256 * r + 256)]
            wog_cols += [on_a[256 * r:256 * r + 128], on_a[256 * r + 128:256 * r + 256], subln[0:128], subln[128:256]]
        rows = np.concatenate(rows)
        alib, cb = _alibi_tables(g)
        sel = np.zeros((128, 4, N_EXP), np.float32)
        for e_ in range(4):
            sel[:, e_, 4 * g + e_] = 1.0
        own = (1024 * g + np.arange(8)[None, :] * 128 + p[:, None]).astype(np.int32)
        mixi = (4096 * g + np.arange(4)[None, None, :] * 1024 + (np.arange(8)[None, :] * 128 + p[:, None])[:, :, None]).astype(np.int32)
        maps.append({
            "x_b": x[b], "x_own": np.ascontiguousarray(x[b, 1024 * g:1024 * (g + 1)]),
            "w_in_g": np.ascontiguousarray(w_in[:, cols]), "ln1T": ln1T, "qkg": qkg,
            "nab": np.ascontiguousarray(np.stack([_na_bias_tables(rpb[2 * g]), _na_bias_tables(rpb[2 * g + 1])])),
            "alib": alib, "cbias": cb, "lamv": lamv,
            "w_out_p": np.ascontiguousarray(w_out[rows]), "wog": np.ascontiguousarray(np.stack(wog_cols, axis=1)),
            "ln2": ln2, "w_router": w_router, "sel": sel, "own_tok": np.ascontiguousarray((own[:, :, None] + S * np.arange(4)[None, None, :]).astype(np.int32)),
            "mix_idx": np.ascontiguousarray(mixi), "fcv": fcv,
            "wg_e": np.ascontiguousarray(wg[4 * g:4 * g + 4], dtype=np.float32),
            "wu_e": np.ascontiguousarray(wu[4 * g:4 * g + 4], dtype=np.float32),
            "wd_e": np.ascontiguousarray(wd[4 * g:4 * g + 4], dtype=np.float32),
        })
    return maps


def kernel(**inputs):
    maps = _prep_inputs(inputs)
    nc = build_nc()
    res = run_bass_kernel_spmd(nc, maps, core_ids=list(range(8)))
    out = np.empty((2, S, D), np.float32)
    for c in range(8):
        b, g = c // 4, c % 4
        out[b, :, 512 * g:512 * (g + 1)] = np.asarray(res.results[c]["out"], dtype=np.float32)
    return out
```

```python
import numpy as np
from contextlib import ExitStack
import concourse.bass as bass
import concourse.mybir as mybir
from concourse.bass_utils import run_bass_kernel_spmd

F32 = mybir.dt.float32
BF16 = mybir.dt.bfloat16
I32 = mybir.dt.int32
U8 = mybir.dt.uint8
AF = mybir.ActivationFunctionType
ALU = mybir.AluOpType
AX = mybir.AxisListType

D = 2048
S = 4096
NT = 32
GRID_W = 64
EPS = 1e-6
N_EXP = 16
CAP = 512
FF = 2816
NFC = FF // 128
GROUPS = [[0, 1, 2, 3], [4, 5, 6, 7]]
SCALE = 128.0 ** -0.5
LAM_INIT = 0.8 - 0.6
NEG = -30000.0
NPAT = 25
BISECT_ITERS = 24


class Sched:
    COMPUTE = ("act", "dve", "pool", "pe")

    def __init__(self, nc, es, rings):
        self.nc = nc
        self.ops = []
        self.lastw = {}
        self.readers = {}
        self.sems = []
        self.csem = {}
        for e in self.COMPUTE:
            self.csem[e] = self._sem(es, "c_" + e)
        self.rings = {e: [self._sem(es, f"d_{e}_{i}") for i in range(k)] for e, k in rings.items()}
        self.dma_count = {e: 0 for e in rings}
        self.es = es
        self.barrier_deps = []
        self.recent = {e: None for e in self.COMPUTE}
        self.recent_dma = {e: [] for e in rings}
        self.cc_ops = []
        self.cc_pool = [self._sem(es, f"cc_{i}") for i in range(12)]

    def _sem(self, es, name):
        self.sems.append(es.enter_context(self.nc.semaphore(name)))
        return len(self.sems) - 1

    def add(self, eng, fn, reads=(), writes=(), kind="c", extra=()):
        oid = len(self.ops)
        deps = {}
        for r in reads:
            w = self.lastw.get(r)
            if w is not None:
                deps.setdefault(w, set()).add("raw")
        for w_ in writes:
            w = self.lastw.get(w_)
            if w is not None:
                deps.setdefault(w, set()).add("waw")
            for rd in self.readers.get(w_, ()):
                deps.setdefault(rd, set()).add("war")
        for d in self.barrier_deps:
            deps.setdefault(d, set()).add("raw")
        for d in extra:
            deps.setdefault(d, set()).add("raw")
        fdeps = []
        for d, types in deps.items():
            p = self.ops[d]
            if p["kind"] == "c" and p["eng"] == eng and kind == "c":
                if eng == "pe" or "raw" not in types:
                    continue
            fdeps.append(d)
        op = dict(id=oid, eng=eng, fn=fn, kind=kind, deps=fdeps, has_dep=False, sem=None, val=0, prev=0)
        if kind == "dma":
            j = self.dma_count[eng]
            self.dma_count[eng] += 1
            K = len(self.rings[eng])
            op["sem"] = self.rings[eng][j % K]
            op["val"] = 16 * (j // K + 1)
            op["prev"] = 16 * (j // K)
            self.recent_dma[eng].append(oid)
            self.recent_dma[eng] = self.recent_dma[eng][-K:]
        elif kind == "cc":
            op["sem"] = self.cc_pool[len(self.cc_ops)]
            op["val"] = 1
            self.cc_ops.append(oid)
        else:
            self.recent[eng] = oid
        for r in reads:
            self.readers.setdefault(r, []).append(oid)
        for w_ in writes:
            self.lastw[w_] = oid
            self.readers[w_] = []
        self.ops.append(op)
        return oid

    def barrier(self, include_cc=True):
        deps = [v for v in self.recent.values() if v is not None]
        for lst in self.recent_dma.values():
            deps += lst
        if include_cc:
            deps += self.cc_ops
        self.barrier_deps = deps

    def emit(self, block):
        ops = self.ops
        for op in ops:
            for d in op["deps"]:
                ops[d]["has_dep"] = True
        cnt = {e: 0 for e in self.COMPUTE}
        for op in ops:
            if op["kind"] == "c" and op["has_dep"]:
                cnt[op["eng"]] += 1
                op["sem"] = self.csem[op["eng"]]
                op["val"] = cnt[op["eng"]]
        for e, c in cnt.items():
            assert c < 60000, (e, c)
        lists = {}
        for op in ops:
            lists.setdefault(op["eng"], []).append(op)
        final = {}
        for op in ops:
            if op["sem"] is not None:
                final[op["sem"]] = max(final.get(op["sem"], 0), op["val"])
        sems = self.sems

        def run(name, eng):
            waited = {}
            for op in lists.get(name, []):
                waits = {}
                for d in op["deps"]:
                    p = ops[d]
                    waits[p["sem"]] = max(waits.get(p["sem"], 0), p["val"])
                if op["kind"] == "dma" and op["prev"] > 0:
                    waits[op["sem"]] = max(waits.get(op["sem"], 0), op["prev"])
                for s_, v in waits.items():
                    if waited.get(s_, 0) < v:
                        eng.wait_ge(sems[s_], v)
                        waited[s_] = v
                ins = op["fn"](eng)
                if op["kind"] == "dma":
                    ins.then_inc(sems[op["sem"]], 16)
                elif op["kind"] == "cc":
                    ins.then_inc(sems[op["sem"]], 1)
                elif op["has_dep"]:
                    ins.then_inc(sems[op["sem"]], 1)
            if name == "sync":
                for s_, v in final.items():
                    if waited.get(s_, 0) < v:
                        eng.wait_ge(sems[s_], v)

        @block.sync
        def _(e):
            run("sync", e)

        @block.scalar
        def _(e):
            run("act", e)

        @block.vector
        def _(e):
            run("dve", e)

        @block.gpsimd
        def _(e):
            run("pool", e)

        @block.tensor
        def _(e):
            run("pe", e)


class Arena:
    def __init__(self, ar, size):
        self.ar = ar
        self.size = size

    def view(self, off, shape, dt):
        esz = {F32: 4, BF16: 2, I32: 4}[dt]
        n = int(np.prod(shape[1:]))
        assert off % 4 == 0 and off + n * esz <= self.size, (off, shape, self.size)
        v = self.ar[:, off:off + n * esz].bitcast(dt)
        if len(shape) == 3:
            v = v.rearrange("p (a b) -> p a b", a=shape[1])
        elif len(shape) == 4:
            v = v.rearrange("p (a b c) -> p a b c", a=shape[1], b=shape[2])
        return v


def bc(ap, shape):
    return ap.unsqueeze(len(ap.shape)).to_broadcast(list(shape))


def build_nc(debug=False, stop_after=None):
    nc = bass.Bass("TRN2", target_bir_lowering=False)

    def din(name, shape, dt=F32):
        return nc.dram_tensor(name, list(shape), dt, kind="ExternalInput").ap()

    x_b = din("x_b", [S, D])
    x_own = din("x_own", [1024, D])
    w_in_g = din("w_in_g", [D, 1536])
    ln1T = din("ln1T", [128, 16])
    qkg = din("qkg", [128, 8])
    nab = din("nab", [2, 128, NPAT, 128])
    alib = din("alib", [128, 4, 256])
    cbias = din("cbias", [128, 64])
    lamv = din("lamv", [4, 128])
    w_out_p = din("w_out_p", [D, D])
    wog = din("wog", [128, 16])
    ln2 = din("ln2", [1, D])
    w_router = din("w_router", [D, N_EXP])
    sel = din("sel", [128, 4, N_EXP])
    fcv = din("fcv", [128, NT])
    own_tok = din("own_tok", [128, 8, 4], I32)
    mix_idx = din("mix_idx", [128, 8, 4], I32)
    if stop_after is None:
        wg_e = din("wg_e", [4, D, FF])
        wu_e = din("wu_e", [4, D, FF])
        wd_e = din("wd_e", [4, FF, D])
    out = nc.dram_tensor("out", [S, 512], F32, kind="ExternalOutput").ap()

    ag1_in = nc.dram_tensor("ag1_in", [S, 512], BF16).ap()
    ag1_out = nc.dram_tensor("ag1_out", [4 * S, 512], BF16).ap()
    h2_in = nc.dram_tensor("h2_in", [1024, D], BF16).ap()
    h2_all = nc.dram_tensor("h2_all", [S, D], BF16).ap()
    aff_in = nc.dram_tensor("aff_in", [1024, N_EXP], F32).ap()
    aff_all = nc.dram_tensor("aff_all", [S, N_EXP], F32).ap()
    part = nc.dram_tensor("part", [4 * S, 512], F32).ap()
    rs_out = nc.dram_tensor("rs_out", [S, 512], F32).ap()
    dbg = {}
    if debug:
        dbg["mix"] = nc.dram_tensor("dbg_mix", [S, 512], BF16, kind="ExternalOutput").ap()
        dbg["x1"] = nc.dram_tensor("dbg_x1", [1024, D], F32, kind="ExternalOutput").ap()
        dbg["aff"] = nc.dram_tensor("dbg_aff", [S, N_EXP], F32, kind="ExternalOutput").ap()
        dbg["idx"] = nc.dram_tensor("dbg_idx", [128, 16], I32, kind="ExternalOutput").ap()
        dbg["gate"] = nc.dram_tensor("dbg_gate", [128, 16], F32, kind="ExternalOutput").ap()

    ARENA = 196 * 1024
    with ExitStack() as es:
        ar_t = es.enter_context(nc.sbuf_tensor("arena", [128, ARENA], U8))
        A = Arena(ar_t, ARENA)
        small = es.enter_context(nc.sbuf_tensor("small", [128, 1024], F32))
        ident_b = es.enter_context(nc.sbuf_tensor("ident_b", [128, 128], BF16))
        ident_f = es.enter_context(nc.sbuf_tensor("ident_f", [128, 128], F32))
        iota_f = es.enter_context(nc.sbuf_tensor("iota_f", [128, 512], F32))
        iota_p = es.enter_context(nc.sbuf_tensor("iota_p", [128, 1], F32))
        zero_b = es.enter_context(nc.sbuf_tensor("zero_b", [128, 32], BF16))
        pbanks = [es.enter_context(nc.psum_tensor(f"pb{i}", [128, 512], F32)) for i in range(8)]
        sc = Sched(nc, es, rings={"sync": 16, "pool": 24})
        block = es.enter_context(nc.Block())

        so = [0]

        def sm(n):
            v = small[:, so[0]:so[0] + n]
            so[0] += n
            assert so[0] <= 1024
            return v

        eps_c = sm(1)
        ones_b = A.view(ARENA - 256, [128, 128], BF16)
        ARENA_USE = ARENA - 256

        def pb(i):
            return pbanks[i][:]

        def pbb(i):
            return pbanks[i][:].bitcast(BF16)

        sc.add("pool", lambda e: e.memset(eps_c, EPS), writes=["eps"])
        sc.add("pool", lambda e: e.iota(iota_f[:], pattern=[[1, 512]], base=0, channel_multiplier=0,
                                        allow_small_or_imprecise_dtypes=True), writes=["iota_f"])
        sc.add("pool", lambda e: e.iota(iota_p[:], pattern=[[0, 1]], base=0, channel_multiplier=1,
                                        allow_small_or_imprecise_dtypes=True), writes=["iota_p"])
        sc.add("dve", lambda e: e.tensor_scalar(out=ident_f[:], in0=iota_f[:, 0:128], scalar1=iota_p[:, 0:1],
                                                scalar2=None, op0=ALU.is_equal),
               reads=["iota_f", "iota_p"], writes=["ident_f"])
        sc.add("dve", lambda e: e.tensor_copy(out=ident_b[:], in_=ident_f[:]), reads=["ident_f"], writes=["ident_b"])
        sc.add("pool", lambda e: e.memset(ones_b, 1.0), writes=["ones_b"])
        sc.add("pool", lambda e: e.memset(zero_b[:], 0.0), writes=["zero_b"])

        QKT = A.view(0, [128, 8, S], BF16)
        VA = A.view(65536, [128, NT, 2, 130], BF16)
        VB = A.view(82176, [128, NT, 258], BF16)
        R1 = 98688
        WP = A.view(R1, [128, 16, 1536], BF16)
        XIN = [A.view(R1 + 49152 + i * 8192, [128, D], F32) for i in range(2)]
        R2 = R1 + 65536
        XS = [A.view(R2 + i * 4096, [128, D], BF16) for i in range(2)]
        XT = [A.view(R2 + 8192 + i * 4096, [128, 16, 128], BF16) for i in range(2)]
        SQ = A.view(R2 + 16384, [128, 8, 128], F32)
        QKB = [A.view(R2 + 20480 + i * 2048, [128, 8, 128], BF16) for i in range(2)]
        JUNK = A.view(R2 + 24576, [128, D], BF16)
        assert R2 + 28672 <= ARENA_USE

        ln1T_s = sm(16)
        qkg_s = sm(8)
        ssx = sm(NT)
        rsx = sm(NT)
        ssqk = sm(8 * 2)
        rsqk = sm(8 * 2)

        sc.add("sync", lambda e: e.dma_start(out=ln1T_s, in_=ln1T), writes=["ln1T"], kind="dma")
        sc.add("sync", lambda e: e.dma_start(out=qkg_s, in_=qkg), writes=["qkg"], kind="dma")
        sc.add("pool", lambda e: e.memset(VA[:, :, :, 128:130], 1.0), writes=["VAones"])
        sc.add("pool", lambda e: e.memset(VB[:, :, 256:258], 1.0), writes=["VBones"])
        w_in_v = w_in_g.rearrange("(kc p) n -> p kc n", p=128)
        for kc in range(16):
            sc.add("pool", lambda e, kc=kc: e.dma_start(out=WP[:, kc, :], in_=w_in_v[:, kc, :]),
                   writes=[("WPraw", kc)], kind="dma")
        for kc in range(16):
            if kc % 2 == 0:
                sc.add("dve", lambda e, kc=kc: e.tensor_scalar(out=WP[:, kc, :], in0=WP[:, kc, :],
                                                               scalar1=ln1T_s[:, kc:kc + 1], scalar2=None, op0=ALU.mult),
                       reads=[("WPraw", kc), "ln1T"], writes=[("WP", kc)])
            else:
                sc.add("act", lambda e, kc=kc: e.activation(out=WP[:, kc, :], in_=WP[:, kc, :], func=AF.Copy,
                                                            scale=ln1T_s[:, kc:kc + 1]),
                       reads=[("WPraw", kc), "ln1T"], writes=[("WP", kc)])

        def a_load(tt):
            sc.add("sync", lambda e: e.dma_start(out=XIN[tt % 2], in_=x_b[tt * 128:(tt + 1) * 128, :]),
                   writes=[("xin", tt % 2)], kind="dma")

        def a_norm(tt):
            s = tt % 2
            sc.add("act", lambda e: e.activation(out=JUNK, in_=XIN[s], func=AF.Square, accum_out=ssx[:, tt:tt + 1]),
                   reads=[("xin", s)], writes=["junk", ("ssx", tt)])
            sc.add("act", lambda e: e.activation(out=rsx[:, tt:tt + 1], in_=ssx[:, tt:tt + 1], func=AF.Sqrt,
                                                 scale=1.0 / D, bias=eps_c),
                   reads=[("ssx", tt), "eps"], writes=[("rsx0", tt)])
            sc.add("dve", lambda e: e.reciprocal(out=rsx[:, tt:tt + 1], in_=rsx[:, tt:tt + 1]),
                   reads=[("rsx0", tt)], writes=[("rsx", tt)])
            sc.add("act", lambda e: e.activation(out=XS[s], in_=XIN[s], func=AF.Copy, scale=rsx[:, tt:tt + 1]),
                   reads=[("xin", s), ("rsx", tt)], writes=[("xs", s)])

        def a_transpose(tt):
            s = tt % 2
            for half in range(2):
                def f(e, half=half):
                    r = None
                    for j in range(8):
                        kc = half * 8 + j
                        r = e.transpose(out=pbb(0)[:, j * 128:(j + 1) * 128], in_=XS[s][:, kc * 128:(kc + 1) * 128],
                                        identity=ident_b[:])
                    return r
                sc.add("pe", f, reads=[("xs", s), "ident_b"], writes=[("pb", 0)])
                sc.add("dve", lambda e, half=half: e.tensor_copy(
                    out=XT[s][:, half * 8:(half + 1) * 8, :],
                    in_=pbb(0).rearrange("p (a b) -> p a b", a=8)),
                    reads=[("pb", 0)], writes=[("xT", s, half)])

        def a_proj(tt):
            s = tt % 2
            base = 2 + 3 * (tt % 2)

            def f(e):
                r = None
                for kc in range(16):
                    for nb in range(3):
                        r = e.matmul(pb(base + nb), lhsT=XT[s][:, kc, :], rhs=WP[:, kc, nb * 512:(nb + 1) * 512],
                                     start=(kc == 0), stop=(kc == 15))
                return r
            sc.add("pe", f, reads=[("xT", s, 0), ("xT", s, 1)] + [("WP", kc) for kc in range(16)],
                   writes=[("pb", base + q_) for q_ in range(3)])

        def a_evac(tt):
            s = tt % 2
            base = 2 + 3 * (tt % 2)
            qk_ps = [pb(base), pb(base + 1)]
            for h2_ in range(2):
                sc.add("act", lambda e, h2_=h2_: e.activation(
                    out=SQ[:, h2_ * 4:(h2_ + 1) * 4, :], in_=qk_ps[h2_].rearrange("p (a b) -> p a b", a=4),
                    func=AF.Square), reads=[("pb", base + h2_)], writes=[("sq", h2_)])
            sc.add("dve", lambda e: e.tensor_reduce(out=ssqk[:, s * 8:(s + 1) * 8], in_=SQ, axis=AX.X, op=ALU.add),
                   reads=[("sq", 0), ("sq", 1)], writes=[("ssqk", s)])
            sc.add("act", lambda e: e.activation(out=rsqk[:, s * 8:(s + 1) * 8], in_=ssqk[:, s * 8:(s + 1) * 8],
                                                 func=AF.Sqrt, scale=1.0 / 128, bias=eps_c),
                   reads=[("ssqk", s), "eps"], writes=[("rsqk0", s)])
            sc.add("dve", lambda e: e.reciprocal(out=rsqk[:, s * 8:(s + 1) * 8], in_=rsqk[:, s * 8:(s + 1) * 8]),
                   reads=[("rsqk0", s)], writes=[("rsqk", s)])
            for h2_ in range(2):
                sc.add("dve", lambda e, h2_=h2_: e.tensor_tensor(
                    out=QKB[s][:, h2_ * 4:(h2_ + 1) * 4, :], in0=qk_ps[h2_].rearrange("p (a b) -> p a b", a=4),
                    in1=bc(rsqk[:, s * 8 + h2_ * 4:s * 8 + (h2_ + 1) * 4], [128, 4, 128]), op=ALU.mult),
                    reads=[("pb", base + h2_), ("rsqk", s)], writes=[("qkb", s, h2_)])
            sc.add("act", lambda e: e.copy(out=VA[:, tt, :, 0:128],
                                           in_=pb(base + 2)[:, 0:256].rearrange("p (a b) -> p a b", a=2)),
                   reads=[("pb", base + 2)], writes=[("VA", tt)])
            sc.add("act", lambda e: e.copy(out=VB[:, tt, 0:256], in_=pb(base + 2)[:, 256:512]),
                   reads=[("pb", base + 2)], writes=[("VB", tt)])

        def a_qkT(tt):
            s = tt % 2

            def f(e):
                r = None
                for j in range(8):
                    r = e.transpose(out=pbb(1)[:, j * 128:(j + 1) * 128], in_=QKB[s][:, j, :], identity=ident_b[:])
                return r
            sc.add("pe", f, reads=[("qkb", s, 0), ("qkb", s, 1), "ident_b"], writes=[("pb", 1)])
            sc.add("dve", lambda e: e.tensor_tensor(out=QKT[:, :, tt * 128:(tt + 1) * 128],
                                                    in0=pbb(1).rearrange("p (a b) -> p a b", a=8),
                                                    in1=bc(qkg_s, [128, 8, 128]), op=ALU.mult),
                   reads=[("pb", 1), "qkg"], writes=[("QKT", tt)])

        ZERO = A.view(R2 + 28672, [128, 1024], F32)
        sc.add("pool", lambda e: e.memset(ZERO, 0.0), writes=["zero"])
        part_v = part.rearrange("(a b p) n -> p a b n", p=128, b=2)

        def zero_fill(a_):
            sc.add("sync", lambda e: e.dma_start(out=part_v[:, a_, :, :], in_=ZERO.rearrange("p (b n) -> p b n", b=2)),
                   reads=["zero"], writes=[("partz", a_)], kind="dma")
        partz_all = [("partz", a_) for a_ in range(64)]
        a_load(0)
        a_load(1)
        a_norm(0)
        a_transpose(0)
        for tt in range(NT):
            a_proj(tt)
            if tt + 1 < NT:
                a_norm(tt + 1)
                a_transpose(tt + 1)
            if tt + 2 < NT:
                a_load(tt + 2)
            zero_fill(2 * tt)
            zero_fill(2 * tt + 1)
            if tt >= 1:
                a_qkT(tt - 1)
            a_evac(tt)
        a_qkT(NT - 1)
        sc.barrier()
        if stop_after == "A":
            sc.add("sync", lambda e: e.dma_start(out=out[0:128, :], in_=XIN[0]), writes=["outdummy"], kind="dma")
            sc.emit(block)
            return nc

        WO = A.view(R1, [128, 16, D], BF16)
        TS = [A.view(R2 + i * 2048, [128, 2, 256], F32) for i in range(3)]
        PT = [A.view(R2 + 6144 + i * 1024, [128, 2, 256], BF16) for i in range(3)]
        ALB = A.view(R2 + 9216, [128, 4, 256], F32)
        OB1 = A.view(R2 + 13312, [128, 256], F32)
        OBN = [A.view(R2 + 14336 + i * 1024, [128, 512], BF16) for i in range(2)]
        NAB = A.view(R2 + 16384, [128, NPAT, 128], F32)
        TNA = A.view(R2 + 29184, [128, 5, 128], F32)
        PNA = A.view(R2 + 31744, [128, 5, 128], BF16)
        LAMT = A.view(R2 + 33024, [128, 4, 128], F32)
        JB = A.view(R2 + 35072, [128, 256], F32)
        assert R2 + 36096 <= ARENA_USE
        cb_s = sm(64)
        wog_s = sm(16)
        lam_s = sm(8)
        dst = sm(64)
        nst = sm(8)

        sc.add("sync", lambda e: e.dma_start(out=ALB, in_=alib), writes=["alb"], kind="dma")
        sc.add("sync", lambda e: e.dma_start(out=cb_s, in_=cbias), writes=["cb"], kind="dma")
        sc.add("sync", lambda e: e.dma_start(out=wog_s, in_=wog), writes=["wog"], kind="dma")
        for i in range(4):
            sc.add("sync", lambda e, i=i: e.dma_start(out=LAMT[:, i, :], in_=lamv[i:i + 1, :].partition_broadcast(128)),
                   writes=[("lamt", i)], kind="dma")
        sc.add("dve", lambda e: e.tensor_tensor(out=LAMT[:, 0, :], in0=LAMT[:, 0, :], in1=LAMT[:, 1, :], op=ALU.mult),
               reads=[("lamt", 0), ("lamt", 1)], writes=["lp1"])
        sc.add("dve", lambda e: e.tensor_tensor(out=LAMT[:, 2, :], in0=LAMT[:, 2, :], in1=LAMT[:, 3, :], op=ALU.mult),
               reads=[("lamt", 2), ("lamt", 3)], writes=["lp2"])
        sc.add("dve", lambda e: e.tensor_reduce(out=lam_s[:, 0:1], in_=LAMT[:, 0, :], axis=AX.X, op=ALU.add),
               reads=["lp1"], writes=["ls1"])
        sc.add("dve", lambda e: e.tensor_reduce(out=lam_s[:, 1:2], in_=LAMT[:, 2, :], axis=AX.X, op=ALU.add),
               reads=["lp2"], writes=["ls2"])
        sc.add("act", lambda e: e.activation(out=lam_s[:, 2:4], in_=lam_s[:, 0:2], func=AF.Exp),
               reads=["ls1", "ls2"], writes=["lexp"])
        sc.add("dve", lambda e: e.tensor_tensor(out=lam_s[:, 4:5], in0=lam_s[:, 3:4], in1=lam_s[:, 2:3], op=ALU.subtract),
               reads=["lexp"], writes=["ldiff"])
        sc.add("dve", lambda e: e.tensor_scalar(out=lam_s[:, 5:6], in0=lam_s[:, 4:5], scalar1=-LAM_INIT, scalar2=None,
                                                op0=ALU.add),
               reads=["ldiff"], writes=["neglam"])

        w_out_v = w_out_p.rearrange("(kc p) n -> p kc n", p=128)
        for kc in range(16):
            sc.add("pool", lambda e, kc=kc: e.dma_start(out=WO[:, kc, :], in_=w_out_v[:, kc, :]),
                   writes=[("WOraw", kc)], kind="dma")
        for kc in range(16):
            mul2 = 1.0 if (kc % 4) < 2 else (1.0 - LAM_INIT)
            sc.add("pool", lambda e, kc=kc, mul2=mul2: e.tensor_scalar(
                out=WO[:, kc, :], in0=WO[:, kc, :], scalar1=wog_s[:, kc:kc + 1], scalar2=mul2, op0=ALU.mult, op1=ALU.mult),
                reads=[("WOraw", kc), "wog"], writes=[("WO", kc)])

        if stop_after == "B0":
            sc.add("sync", lambda e: e.dma_start(out=out[0:128, :], in_=XIN[0]), writes=["outdummy"], kind="dma")
            sc.emit(block)
            return nc
        SB = [4, 5, 6, 7]
        ACC = [0, 1, 2, 3]
        Q1, Q2, K1, K2 = 4, 5, 6, 7

        def b_score(qb, kt, n):
            bk = SB[n % 4]

            def f(e):
                e.matmul(pb(bk)[:, 0:256], lhsT=QKT[:, K1, kt * 128:(kt + 1) * 128],
                         rhs=QKT[:, Q1, qb * 256:(qb + 1) * 256], start=True, stop=True)
                return e.matmul(pb(bk)[:, 256:512], lhsT=QKT[:, K2, kt * 128:(kt + 1) * 128],
                                rhs=QKT[:, Q2, qb * 256:(qb + 1) * 256], start=True, stop=True)
            sc.add("pe", f, reads=[("QKT", kt), ("QKT", 2 * qb), ("QKT", 2 * qb + 1)], writes=[("pb", bk)])

        def b_soft(qb, kt, n):
            bk = SB[n % 4]
            delta = 2 * qb - kt
            if delta >= 1:
                ti = 0
            elif delta <= -2:
                ti = 1
            elif delta == 0:
                ti = 2
            else:
                ti = 3
            ci = delta + 32
            sc.add("dve", lambda e: e.scalar_tensor_tensor(
                out=TS[n % 3], in0=pb(bk).rearrange("p (a b) -> p a b", a=2), scalar=SCALE,
                in1=ALB[:, ti:ti + 1, :].to_broadcast([128, 2, 256]), op0=ALU.mult, op1=ALU.add),
                reads=[("pb", bk), "alb"], writes=[("ts", n % 3)])
            sc.add("act", lambda e: e.activation(out=PT[n % 3], in_=TS[n % 3], func=AF.Exp, bias=cb_s[:, ci:ci + 1]),
                   reads=[("ts", n % 3), "cb"], writes=[("pt", n % 3)])

        def b_pv(qb, kt, n):
            def f(e):
                r = None
                for i in range(2):
                    for j in range(2):
                        r = e.matmul(pb(ACC[i * 2 + j])[:, 0:258], lhsT=PT[n % 3][:, i, j * 128:(j + 1) * 128],
                                     rhs=VB[:, kt, :], start=(kt == 0), stop=(kt == NT - 1))
                return r
            sc.add("pe", f, reads=[("pt", n % 3), ("VB", kt), "VBones"], writes=[("pb", q_) for q_ in range(4)])

        def b_epilogue(qb):
            for j in range(2):
                tq = 2 * qb + j
                a1 = pb(ACC[j])
                a2 = pb(ACC[2 + j])
                st = dst[:, (tq % 4) * 8:(tq % 4) * 8 + 8]
                k = ("dst", tq % 4)
                ka1, ka2 = ("pb", ACC[j]), ("pb", ACC[2 + j])
                sc.add("dve", lambda e, a1=a1, st=st: e.reciprocal(out=st[:, 0:1], in_=a1[:, 256:257]),
                       reads=[ka1], writes=[k + (0,)])
                sc.add("dve", lambda e, a2=a2, st=st: e.reciprocal(out=st[:, 1:2], in_=a2[:, 256:257]),
                       reads=[ka2], writes=[k + (1,)])
                sc.add("dve", lambda e, st=st: e.tensor_tensor(out=st[:, 2:3], in0=st[:, 1:2], in1=lam_s[:, 5:6], op=ALU.mult),
                       reads=[k + (1,), "neglam"], writes=[k + (2,)])
                sc.add("dve", lambda e, a1=a1, st=st: e.tensor_scalar(out=OB1, in0=a1[:, 0:256], scalar1=st[:, 0:1],
                                                                      scalar2=None, op0=ALU.mult),
                       reads=[ka1, k + (0,)], writes=["ob1a"])
                sc.add("dve", lambda e, a2=a2, st=st: e.scalar_tensor_tensor(out=OB1, in0=a2[:, 0:256], scalar=st[:, 2:3],
                                                                             in1=OB1, op0=ALU.mult, op1=ALU.add),
                       reads=[ka2, "ob1a", k + (2,)], writes=["ob1"])
                sc.add("dve", lambda e: e.tensor_tensor(out=JB, in0=OB1, in1=OB1, op=ALU.mult), reads=["ob1"], writes=["jb"])
                sc.add("dve", lambda e, st=st: e.tensor_reduce(out=st[:, 3:4], in_=JB, axis=AX.X, op=ALU.add),
                       reads=["jb"], writes=[k + (3,)])
                sc.add("act", lambda e, st=st: e.activation(out=st[:, 4:5], in_=st[:, 3:4], func=AF.Ln, scale=1.0 / 256,
                                                            bias=eps_c),
                       reads=[k + (3,), "eps"], writes=[k + (4,)])
                sc.add("act", lambda e, st=st: e.activation(out=st[:, 5:6], in_=st[:, 4:5], func=AF.Exp, scale=-0.5),
                       reads=[k + (4,)], writes=[k + (5,)])
                sc.add("dve", lambda e, st=st, tq=tq: e.tensor_scalar(out=OBN[tq % 2][:, 256:512], in0=OB1, scalar1=st[:, 5:6],
                                                                      scalar2=None, op0=ALU.mult),
                       reads=["ob1", k + (5,)], writes=[("obn_b", tq % 2)])
                sc.add("sync", lambda e, tq=tq: e.dma_start(out=ag1_in[tq * 128:(tq + 1) * 128, 256:512],
                                                            in_=OBN[tq % 2][:, 256:512]),
                       reads=[("obn_b", tq % 2)], writes=[("ag1_in_b", tq)], kind="dma")

        if stop_after == "B":
            sc.add("sync", lambda e: e.dma_start(out=out[0:128, :], in_=XIN[0]), writes=["outdummy"], kind="dma")
            sc.emit(block)
            return nc
        na2 = es.enter_context(nc.sbuf_tensor("na2", [128, 3840], U8))
        TNAs = [TNA, na2[:, 0:2560].bitcast(F32).rearrange("p (a b) -> p a b", a=5)]
        PNAs = [PNA, na2[:, 2560:3840].bitcast(BF16).rearrange("p (a b) -> p a b", a=5)]
        NSAs, NSBs, NOs = [0, 2], [1, 3], [4, 5]

        def na_info(m):
            if m in (0, 1, 30, 31):
                sp = {0: 0, 1: 1, 30: 2, 31: 3}[m]
                p0 = 5 + sp * 5
            else:
                p0 = 0
            if m in (0, 1):
                kts = [0, 1, 2, 3, 3]
            elif m in (30, 31):
                kts = [28, 29, 30, 31, 31]
            else:
                kts = [m - 2 + i for i in range(5)]
            return p0, kts

        def na_score(hh, m):
            par = (hh * NT + m) % 2
            qch, kch = hh, 2 + hh
            p0, kts = na_info(m)
            nsa, nsb = NSAs[par], NSBs[par]

            def f(e):
                r = None
                for i in range(5):
                    o = pb(nsa)[:, i * 128:(i + 1) * 128] if i < 4 else pb(nsb)[:, 0:128]
                    r = e.matmul(o, lhsT=QKT[:, kch, kts[i] * 128:(kts[i] + 1) * 128],
                                 rhs=QKT[:, qch, m * 128:(m + 1) * 128], start=True, stop=True)
                return r
            sc.add("pe", f, reads=[("QKT", k_) for k_ in set(kts + [m])], writes=[("pb", nsa), ("pb", nsb)])

        def na_rest(hh, m):
            par = (hh * NT + m) % 2
            p0, kts = na_info(m)
            nsa, nsb, no = NSAs[par], NSBs[par], NOs[par]
            tna, pna = TNAs[par], PNAs[par]
            sc.add("dve", lambda e: e.scalar_tensor_tensor(
                out=tna[:, 0:4, :], in0=pb(nsa).rearrange("p (a b) -> p a b", a=4), scalar=SCALE,
                in1=NAB[:, p0:p0 + 4, :], op0=ALU.mult, op1=ALU.add),
                reads=[("pb", nsa), ("nab", hh)], writes=[("tna0", par)])
            sc.add("dve", lambda e: e.scalar_tensor_tensor(
                out=tna[:, 4, :], in0=pb(nsb)[:, 0:128], scalar=SCALE,
                in1=NAB[:, p0 + 4, :], op0=ALU.mult, op1=ALU.add),
                reads=[("pb", nsb), ("nab", hh)], writes=[("tna1", par)])
            sc.add("act", lambda e: e.activation(out=pna, in_=tna, func=AF.Exp),
                   reads=[("tna0", par), ("tna1", par)], writes=[("pna", par)])

            def f2(e):
                r = None
                for i in range(5):
                    r = e.matmul(pb(no)[:, 0:130], lhsT=pna[:, i, :], rhs=VA[:, kts[i], hh, :],
                                 start=(i == 0), stop=(i == 4))
                return r
            sc.add("pe", f2, reads=[("pna", par), "VAones"] + [("VA", k_) for k_ in set(kts)], writes=[("pb", no)])
            nsc = nst[:, par * 2 + hh:par * 2 + hh + 1]
            sc.add("dve", lambda e: e.reciprocal(out=nsc, in_=pb(no)[:, 128:129]), reads=[("pb", no)],
                   writes=[("nst", par, hh)])
            ob = OBN[m % 2]
            sc.add("dve", lambda e: e.tensor_scalar(out=ob[:, hh * 128:(hh + 1) * 128], in0=pb(no)[:, 0:128],
                                                    scalar1=nsc, scalar2=None, op0=ALU.mult),
                   reads=[("pb", no), ("nst", par, hh)], writes=[("obn_a", m % 2)])
            sc.add("sync", lambda e: e.dma_start(out=ag1_in[m * 128:(m + 1) * 128, hh * 128:(hh + 1) * 128],
                                                 in_=ob[:, hh * 128:(hh + 1) * 128]),
                   reads=[("obn_a", m % 2)], writes=[("ag1_in_a", hh, m)], kind="dma")

        for hh in range(2):
            sc.add("sync", lambda e, hh=hh: e.dma_start(out=NAB, in_=nab[hh]), writes=[("nab", hh)], kind="dma")
            na_score(hh, 0)
            for m in range(NT):
                if m + 1 < NT:
                    na_score(hh, m + 1)
                na_rest(hh, m)

        def ag_slab(k_):
            rd = [("ag1_in_b", t) for t in range(8 * k_, 8 * k_ + 8)] + \
                 [("ag1_in_a", h_, t) for h_ in range(2) for t in range(8 * k_, 8 * k_ + 8)]
            sc.add("pool", lambda e: e.collective_compute(
                "AllGather", ALU.bypass, replica_groups=GROUPS,
                ins=[ag1_in[1024 * k_:1024 * (k_ + 1), :].opt()], outs=[ag1_out[4096 * k_:4096 * (k_ + 1), :].opt()]),
                reads=rd, writes=[("ag1_out", k_)], kind="cc")

        seq = [(qb, kt) for qb in range(16) for kt in range(NT)]
        LOOK = 3
        for i in range(min(LOOK, len(seq))):
            b_score(seq[i][0], seq[i][1], i)
        for i, (qb, kt) in enumerate(seq):
            if i + LOOK < len(seq):
                b_score(seq[i + LOOK][0], seq[i + LOOK][1], i + LOOK)
            b_soft(qb, kt, i)
            b_pv(qb, kt, i)
            if kt == NT - 1:
                b_epilogue(qb)
                if qb % 4 == 3:
                    ag_slab(qb // 4)

        if stop_after == "C":
            sc.add("sync", lambda e: e.dma_start(out=out[0:128, :], in_=XIN[0]), writes=["outdummy"], kind="dma")
            sc.emit(block)
            return nc
        ag_reads = [("ag1_in_b", t) for t in range(NT)] + [("ag1_in_a", h_, t) for h_ in range(2) for t in range(NT)]
        if debug:
            for t_ in range(NT):
                sc.add("sync", lambda e, t_=t_: e.dma_start(out=dbg["mix"][t_ * 128:(t_ + 1) * 128, :],
                                                            in_=ag1_in[t_ * 128:(t_ + 1) * 128, :]),
                       reads=ag_reads, writes=[("dbg_mix", t_)], kind="dma")
        sc.barrier()
        if stop_after == "attn":
            sc.add("sync", lambda e: e.dma_start(out=out[0:128, :], in_=XIN[0]), writes=["outdummy"], kind="dma")
            sc.emit(block)
            return nc

        P0 = 0
        MIXT = [A.view(P0 + i * 4096, [128, 4, 512], BF16) for i in range(2)]
        MIXN = [A.view(P0 + 8192 + i * 4096, [128, 4, 512], BF16) for i in range(2)]
        MXT = [A.view(P0 + 16384 + i * 4096, [128, 16, 128], BF16) for i in range(2)]
        XO = [A.view(P0 + 24576 + i * 8192, [128, D], F32) for i in range(2)]
        X1 = [A.view(P0 + 40960 + i * 8192, [128, D], F32) for i in range(2)]
        H2F = A.view(P0 + 57344, [128, D], F32)
        H2T = A.view(P0 + 65536, [128, 16, 128], F32)
        H2B = [A.view(P0 + 73728 + i * 4096, [128, D], BF16) for i in range(2)]
        LN2R = A.view(P0 + 81920, [128, D], F32)
        WR = A.view(P0 + 90112, [128, 16, N_EXP], F32)
        JD = A.view(P0 + 95232, [128, D], BF16)
        assert P0 + 95232 + 4096 <= R1 + 65536
        JD = A.view(R2, [128, D], BF16)
        own_s = es.enter_context(nc.sbuf_tensor("own_s", [128, 8, 4], I32))
        mixi_s = es.enter_context(nc.sbuf_tensor("mixi_s", [128, 8, 4], I32))
        dstat = sm(8 * 8)
        logit = sm(8 * 16)
        affs = sm(8 * 16)

        sc.add("sync", lambda e: e.dma_start(out=own_s[:], in_=own_tok), writes=["own"], kind="dma")
        sc.add("sync", lambda e: e.dma_start(out=mixi_s[:], in_=mix_idx), writes=["mixi"], kind="dma")
        sc.add("sync", lambda e: e.dma_start(out=LN2R, in_=ln2[0:1, :].partition_broadcast(128)), writes=["ln2r"], kind="dma")
        sc.add("sync", lambda e: e.dma_start(out=WR, in_=w_router.rearrange("(kc p) n -> p kc n", p=128)),
               writes=["wr"], kind="dma")

        def d_tile(i):
            s = i % 2
            st = dstat[:, i * 8:(i + 1) * 8]
            for r in range(4):
                sc.add("pool", lambda e, r=r: e.indirect_dma_start(
                    out=MIXT[s][:, r, :], out_offset=None, in_=ag1_out,
                    in_offset=bass.IndirectOffsetOnAxis(ap=mixi_s[:, i, r:r + 1], axis=0)),
                    reads=[("ag1_out", k_) for k_ in range(4)] + ["mixi"], writes=[("mixt", s, r)], kind="dma")
            sc.add("sync", lambda e: e.dma_start(out=XO[s], in_=x_own[i * 128:(i + 1) * 128, :]),
                   writes=[("xo", s)], kind="dma")
            mr = [("mixt", s, r) for r in range(4)]
            sc.add("act", lambda e: e.activation(out=JD[:, 0:1024].rearrange("p (a b) -> p a b", a=4),
                                                 in_=MIXT[s][:, :, 0:256], func=AF.Square, accum_out=st[:, 0:1]),
                   reads=mr, writes=["jd", ("dst0", i)])
            sc.add("act", lambda e: e.activation(out=st[:, 1:2], in_=st[:, 0:1], func=AF.Sqrt, scale=1.0 / 1024, bias=eps_c),
                   reads=[("dst0", i), "eps"], writes=[("dst1", i)])
            sc.add("dve", lambda e: e.reciprocal(out=st[:, 2:3], in_=st[:, 1:2]), reads=[("dst1", i)], writes=[("dst2", i)])
            sc.add("dve", lambda e: e.tensor_scalar(out=MIXN[s][:, :, 0:256], in0=MIXT[s][:, :, 0:256], scalar1=st[:, 2:3],
                                                    scalar2=None, op0=ALU.mult),
                   reads=mr + [("dst2", i)], writes=[("mixn_a", s)])
            sc.add("dve", lambda e: e.tensor_copy(out=MIXN[s][:, :, 256:512], in_=MIXT[s][:, :, 256:512]),
                   reads=mr, writes=[("mixn_b", s)])
            for half in range(2):
                def f(e, half=half):
                    r_ = None
                    for j in range(8):
                        kc = half * 8 + j
                        r_ = e.transpose(out=pbb(0)[:, j * 128:(j + 1) * 128],
                                         in_=MIXN[s][:, kc // 4, (kc % 4) * 128:(kc % 4 + 1) * 128], identity=ident_b[:])
                    return r_
                sc.add("pe", f, reads=[("mixn_a", s), ("mixn_b", s), "ident_b"], writes=[("pb", 0)])
                sc.add("act", lambda e, half=half: e.copy(out=MXT[s][:, half * 8:(half + 1) * 8, :],
                                                          in_=pbb(0).rearrange("p (a b) -> p a b", a=8)),
                       reads=[("pb", 0)], writes=[("mxt", s, half)])
            for nb in range(4):
                bk = 1 + (nb % 2)

                def f(e, nb=nb, bk=bk):
                    r_ = None
                    for kc in range(16):
                        r_ = e.matmul(pb(bk), lhsT=MXT[s][:, kc, :], rhs=WO[:, kc, nb * 512:(nb + 1) * 512],
                                      start=(kc == 0), stop=(kc == 15))
                    return r_
                sc.add("pe", f, reads=[("mxt", s, 0), ("mxt", s, 1)] + [("WO", kc) for kc in range(16)],
                       writes=[("pb", bk)])
                sc.add("dve", lambda e, nb=nb, bk=bk: e.tensor_tensor(out=X1[s][:, nb * 512:(nb + 1) * 512], in0=pb(bk),
                                                                      in1=XO[s][:, nb * 512:(nb + 1) * 512], op=ALU.add),
                       reads=[("pb", bk), ("xo", s)], writes=[("x1", s, nb)])
            x1r = [("x1", s, nb) for nb in range(4)]
            for db in range(4):
                sc.add("pool", lambda e, db=db: e.indirect_dma_start(
                    out=part, out_offset=bass.IndirectOffsetOnAxis(ap=own_s[:, i, db:db + 1], axis=0),
                    in_=X1[s][:, db * 512:(db + 1) * 512], in_offset=None),
                    reads=x1r + ["own"] + partz_all, writes=[("part_x1", i, db)], kind="dma")
            if debug:
                sc.add("sync", lambda e: e.dma_start(out=dbg["x1"][i * 128:(i + 1) * 128, :], in_=X1[s]),
                       reads=x1r, writes=[("dbgx1", i)], kind="dma")
            sc.add("act", lambda e: e.activation(out=JD, in_=X1[s], func=AF.Square, accum_out=st[:, 3:4]),
                   reads=x1r, writes=["jd", ("dst3", i)])
            sc.add("act", lambda e: e.activation(out=st[:, 4:5], in_=st[:, 3:4], func=AF.Sqrt, scale=1.0 / D, bias=eps_c),
                   reads=[("dst3", i), "eps"], writes=[("dst4", i)])
            sc.add("dve", lambda e: e.reciprocal(out=st[:, 5:6], in_=st[:, 4:5]), reads=[("dst4", i)], writes=[("dst5", i)])
            sc.add("dve", lambda e: e.scalar_tensor_tensor(out=H2F, in0=X1[s], scalar=st[:, 5:6], in1=LN2R,
                                                           op0=ALU.mult, op1=ALU.mult),
                   reads=x1r + [("dst5", i), "ln2r"], writes=["h2f"])
            sc.add("act", lambda e: e.copy(out=H2B[s], in_=H2F), reads=["h2f"], writes=[("h2b", s)])
            sc.add("sync", lambda e: e.dma_start(out=h2_in[i * 128:(i + 1) * 128, :], in_=H2B[s]),
                   reads=[("h2b", s)], writes=[("h2_in", i)], kind="dma")
            for q4 in range(4):
                def f(e, q4=q4):
                    r_ = None
                    for j in range(4):
                        kc = q4 * 4 + j
                        r_ = e.transpose(out=pb(3 + (q4 % 2))[:, j * 128:(j + 1) * 128], in_=H2F[:, kc * 128:(kc + 1) * 128],
                                         identity=ident_f[:])
                    return r_
                sc.add("pe", f, reads=["h2f", "ident_f"], writes=[("pb", 3 + (q4 % 2))])
                sc.add("act", lambda e, q4=q4: e.copy(out=H2T[:, q4 * 4:(q4 + 1) * 4, :],
                                                      in_=pb(3 + (q4 % 2)).rearrange("p (a b) -> p a b", a=4)),
                       reads=[("pb", 3 + (q4 % 2))], writes=[("h2t", q4)])

            def fl(e):
                r_ = None
                for kc in range(16):
                    r_ = e.matmul(pb(5)[:, 0:N_EXP], lhsT=H2T[:, kc, :], rhs=WR[:, kc, :], start=(kc == 0), stop=(kc == 15))
                return r_
            sc.add("pe", fl, reads=[("h2t", q4) for q4 in range(4)] + ["wr"], writes=[("pb", 5)])
            lg = logit[:, i * 16:(i + 1) * 16]
            af = affs[:, i * 16:(i + 1) * 16]
            sc.add("dve", lambda e: e.tensor_reduce(out=st[:, 6:7], in_=pb(5)[:, 0:N_EXP], axis=AX.X, op=ALU.max),
                   reads=[("pb", 5)], writes=[("dst6", i)])
            sc.add("dve", lambda e: e.tensor_scalar(out=lg, in0=pb(5)[:, 0:N_EXP], scalar1=st[:, 6:7], scalar2=None,
                                                    op0=ALU.subtract),
                   reads=[("pb", 5), ("dst6", i)], writes=[("lg", i)])
            sc.add("act", lambda e: e.activation(out=lg, in_=lg, func=AF.Exp, accum_out=st[:, 7:8]),
                   reads=[("lg", i)], writes=[("lge", i), ("dst7", i)])
            sc.add("dve", lambda e: e.reciprocal(out=st[:, 7:8], in_=st[:, 7:8]), reads=[("dst7", i)], writes=[("dst7r", i)])
            sc.add("dve", lambda e: e.tensor_scalar(out=af, in0=lg, scalar1=st[:, 7:8], scalar2=None, op0=ALU.mult),
                   reads=[("lge", i), ("dst7r", i)], writes=[("aff", i)])
            sc.add("sync", lambda e: e.dma_start(out=aff_in[i * 128:(i + 1) * 128, :], in_=af),
                   reads=[("aff", i)], writes=[("aff_in", i)], kind="dma")

        for i in range(8):
            d_tile(i)
            if i % 2 == 1:
                j_ = i // 2
                sc.add("pool", lambda e, j_=j_: e.collective_compute(
                    "AllGather", ALU.bypass, replica_groups=GROUPS,
                    ins=[h2_in[256 * j_:256 * (j_ + 1), :].opt()], outs=[h2_all[1024 * j_:1024 * (j_ + 1), :].opt()]),
                    reads=[("h2_in", 2 * j_), ("h2_in", 2 * j_ + 1)], writes=[("h2_all", j_)], kind="cc")
        sc.add("pool", lambda e: e.collective_compute("AllGather", ALU.bypass, replica_groups=GROUPS,
                                                      ins=[aff_in.opt()], outs=[aff_all.opt()]),
               reads=[("aff_in", i) for i in range(8)], writes=["aff_all"], kind="cc")
        if debug:
            sc.add("sync", lambda e: e.dma_start(out=dbg["aff"], in_=aff_all), reads=["aff_all"], writes=["dbg_aff"], kind="dma")
        sc.barrier(include_cc=False)
        if stop_after == "router":
            sc.add("sync", lambda e: e.dma_start(out=out[0:128, :], in_=X1[0]), writes=["outdummy"], kind="dma")
            sc.emit(block)
            return nc

        E0 = 0
        XSG = A.view(E0, [128, 4, D], BF16)
        XST = A.view(E0 + 16384, [128, 16, 512], BF16)
        HT = A.view(E0 + 32768, [128, NFC, 512], BF16)
        YT = [A.view(E0 + 55296 + i * 2048, [128, 512], F32) for i in range(4)]
        ST_ = [A.view(E0 + 63488 + i * 1024, [128, 512], BF16) for i in range(4)]
        SG = A.view(E0 + 67584, [128, 512], F32)
        SG2 = A.view(E0 + 69632, [128, 512], F32)
        NGU = 5
        WG = [A.view(E0 + 71680 + i * 8192, [128, 16, 128], BF16) for i in range(NGU)]
        WU = [A.view(E0 + 71680 + i * 8192 + 4096, [128, 16, 128], BF16) for i in range(NGU)]
        WDO = E0 + 71680 + NGU * 8192
        WD = [A.view(WDO + i * 22528, [128, NFC, 512], BF16) for i in range(2)]
        A16 = A.view(WDO + 45056, [128, NT, N_EXP], F32)
        SELT = A.view(WDO + 47104, [128, 4, N_EXP], F32)
        PRD = A.view(WDO + 47360, [128, NT, N_EXP], F32)
        A4 = A.view(WDO + 49408, [128, 4, NT], F32)
        CMP = A.view(WDO + 49920, [128, 4, NT], F32)
        AP3 = A.view(WDO + 50432, [128, 4, NT, 6], BF16)
        RES = A.view(WDO + 54400, [128, 4, NT], F32)
        UTRI = A.view(WDO + 52224, [128, 128], F32)
        LT32 = A.view(WDO + 52736, [128, NT], F32)
        MSK = A.view(WDO + 53376, [128, 4, NT], F32)
        POS = A.view(WDO + 53888, [128, 4, NT], F32)
        FCV = A.view(WDO + 54912, [128, NT], F32)
        ONESF = A.view(WDO + 55040, [128, 4], F32)
        CSB = A.view(WDO + 55296, [128, 4, 128], F32)
        assert WDO + 57344 <= ARENA_USE, WDO + 57344
        thr = sm(4)
        cand = sm(4)
        cntp = es.enter_context(nc.sbuf_tensor("cntp", [128, 4], BF16))
        ge = sm(4)
        idxf = sm(80)
        pselc = sm(128)
        gts = sm(16)
        idx_i = es.enter_context(nc.sbuf_tensor("idx_i", [128, 4, 20], I32))
        pidx = es.enter_context(nc.sbuf_tensor("pidx", [128, NT], F32))

        gu_n = [0]

        def load_gu(e_, fc):
            k = gu_n[0] % NGU
            gu_n[0] += 1
            gv = wg_e[e_].rearrange("(kc p) n -> p kc n", p=128)
            uv = wu_e[e_].rearrange("(kc p) n -> p kc n", p=128)
            sc.add("pool", lambda e: e.dma_start(out=WG[k], in_=gv[:, :, fc * 128:(fc + 1) * 128]),
                   writes=[("wg", k)], kind="dma")
            sc.add("pool", lambda e: e.dma_start(out=WU[k], in_=uv[:, :, fc * 128:(fc + 1) * 128]),
                   writes=[("wu", k)], kind="dma")
            return k

        wd_n = [0]

        def load_wd(e_, db):
            k = wd_n[0] % 2
            wd_n[0] += 1
            dv = wd_e[e_].rearrange("(fc p) n -> p fc n", p=128)
            for h_ in range(2):
                sc.add("pool", lambda e, h_=h_: e.dma_start(out=WD[k][:, h_ * 11:(h_ + 1) * 11, :],
                                                            in_=dv[:, h_ * 11:(h_ + 1) * 11, db * 512:(db + 1) * 512]),
                       writes=[("wd", k, h_)], kind="dma")
            return k

        sc.add("sync", lambda e: e.dma_start(out=A16, in_=aff_all.rearrange("(c p) j -> p c j", p=128)),
               reads=["aff_all"], writes=["a16"], kind="dma")
        sc.add("sync", lambda e: e.dma_start(out=SELT, in_=sel), writes=["selt"], kind="dma")
        for e_ in range(4):
            sc.add("dve", lambda e, e_=e_: e.tensor_tensor(out=PRD, in0=A16, in1=SELT[:, e_:e_ + 1, :].to_broadcast([128, NT, N_EXP]),
                                                           op=ALU.mult),
                   reads=["a16", "selt"], writes=["prd"])
            sc.add("dve", lambda e, e_=e_: e.tensor_reduce(out=A4[:, e_, :], in_=PRD, axis=AX.X, op=ALU.add),
                   reads=["prd"], writes=[("a4", e_)])
        a4r = [("a4", e_) for e_ in range(4)]
        sc.add("dve", lambda e: e.tensor_scalar(out=UTRI, in0=iota_f[:, 0:128], scalar1=iota_p[:, 0:1], scalar2=None,
                                                op0=ALU.is_gt), reads=["iota_f", "iota_p"], writes=["utri"])
        sc.add("dve", lambda e: e.tensor_scalar(out=LT32, in0=iota_f[:, 0:NT], scalar1=iota_p[:, 0:1], scalar2=None,
                                                op0=ALU.is_gt), reads=["iota_f", "iota_p"], writes=["lt32"])
        sc.add("dve", lambda e: e.tensor_copy(out=pidx[:], in_=iota_p[:, 0:1].to_broadcast([128, NT])),
               reads=["iota_p"], writes=["pidx"])
        sc.add("dve", lambda e: e.tensor_copy(out=AP3[:, :, :, 0], in_=iota_f[:, 0:NT].unsqueeze(1).to_broadcast([128, 4, NT])),
               reads=["iota_f"], writes=["ap3_0"])
        sc.add("sync", lambda e: e.dma_start(out=FCV, in_=fcv), writes=["fcv"], kind="dma")
        sc.add("pool", lambda e: e.memset(ONESF, 1.0), writes=["ones_f"])
        sc.add("dve", lambda e: e.tensor_copy(out=AP3[:, :, :, 1], in_=FCV.unsqueeze(1).to_broadcast([128, 4, NT])),
               reads=["fcv"], writes=["ap3_1"])
        sc.add("dve", lambda e: e.tensor_copy(out=AP3[:, :, :, 2], in_=pidx[:].unsqueeze(1).to_broadcast([128, 4, NT])),
               reads=["pidx"], writes=["ap3_2"])
        sc.add("dve", lambda e: e.tensor_copy(out=AP3[:, :, :, 3], in_=A4), reads=a4r, writes=["ap3_3"])
        sc.add("dve", lambda e: e.tensor_tensor(out=RES, in0=A4, in1=AP3[:, :, :, 3], op=ALU.subtract),
               reads=a4r + ["ap3_3"], writes=["res1"])
        sc.add("dve", lambda e: e.tensor_copy(out=AP3[:, :, :, 4], in_=RES), reads=["res1"], writes=["ap3_4"])
        sc.add("dve", lambda e: e.tensor_tensor(out=RES, in0=RES, in1=AP3[:, :, :, 4], op=ALU.subtract),
               reads=["res1", "ap3_4"], writes=["res2"])
        sc.add("dve", lambda e: e.tensor_copy(out=AP3[:, :, :, 5], in_=RES), reads=["res2"], writes=["ap3_5"])
        ap3r = ["ap3_0", "ap3_1", "ap3_2", "ap3_3", "ap3_4", "ap3_5"]

        sc.add("dve", lambda e: e.memset(thr, 0.0), writes=["thr"])
        for it in range(1, BISECT_ITERS + 1):
            step = 2.0 ** (-it)
            sc.add("dve", lambda e, step=step: e.tensor_scalar(out=cand, in0=thr, scalar1=step, scalar2=None, op0=ALU.add),
                   reads=["thr"], writes=["cand"])
            sc.add("dve", lambda e: e.tensor_tensor(out=CMP, in0=A4, in1=bc(cand, [128, 4, NT]), op=ALU.is_gt),
                   reads=a4r + ["cand"], writes=["cmp"])
            def fcnt(e):
                with nc.allow_low_precision(reason="per-partition counts <= 32 are exact in bf16"):
                    return e.tensor_reduce(out=cntp[:], in_=CMP, axis=AX.X, op=ALU.add)
            sc.add("dve", fcnt, reads=["cmp"], writes=["cntp"])
            sc.add("pe", lambda e: e.matmul(pb(7)[:, 0:4], lhsT=ones_b, rhs=cntp[:], start=True, stop=True),
                   reads=["cntp", "ones_b"], writes=[("pb", 7)])
            sc.add("dve", lambda e: e.tensor_scalar(out=ge, in0=pb(7)[:, 0:4], scalar1=float(CAP) - 0.5, scalar2=None,
                                                    op0=ALU.is_gt), reads=[("pb", 7)], writes=["ge"])
            sc.add("dve", lambda e, step=step: e.scalar_tensor_tensor(out=thr, in0=ge, scalar=step, in1=thr,
                                                                      op0=ALU.mult, op1=ALU.add),
                   reads=["ge", "thr"], writes=["thr"])
        sc.add("dve", lambda e: e.tensor_tensor(out=MSK, in0=A4, in1=bc(thr, [128, 4, NT]), op=ALU.is_gt),
               reads=a4r + ["thr"], writes=["msk"])

        for e_ in range(4):
            sc.add("pe", lambda e, e_=e_: e.matmul(pb(6)[0:NT, e_:e_ + 1], lhsT=MSK[:, e_, :], rhs=ONESF[:, 0:1],
                                                   start=True, stop=True),
                   reads=["msk", "ones_f"], writes=[("pb", 6)])
        for e_ in range(4):
            sc.add("dve", lambda e, e_=e_: e.tensor_copy(out=CSB[0:NT, e_, :], in_=pb(6)[0:NT, e_:e_ + 1].to_broadcast([NT, 128])),
                   reads=[("pb", 6)], writes=[("csb", e_)])
        for e_ in range(4):
            def fpos(e, e_=e_):
                e.matmul(pb(7)[:, e_ * NT:(e_ + 1) * NT], lhsT=UTRI, rhs=MSK[:, e_, :], start=True, stop=False)
                return e.matmul(pb(7)[:, e_ * NT:(e_ + 1) * NT], lhsT=CSB[0:NT, e_, :], rhs=LT32[0:NT, :], start=False, stop=True)
            sc.add("pe", fpos, reads=["msk", "utri", "lt32", ("csb", e_)], writes=[("pb", 7)])
        sc.add("dve", lambda e: e.scalar_tensor_tensor(out=POS, in0=pb(7)[:, 0:4 * NT].rearrange("p (a b) -> p a b", a=4),
                                                       scalar=1.0, in1=MSK, op0=ALU.add, op1=ALU.mult),
               reads=[("pb", 7), "msk"], writes=["pos0"])
        sc.add("dve", lambda e: e.tensor_scalar(out=POS, in0=POS, scalar1=-1.0, scalar2=None, op0=ALU.add),
               reads=["pos0"], writes=["pos"])

        gu_list = [(e_, fc) for e_ in range(4) for fc in range(NFC)]
        wd_list = [(e_, db) for e_ in range(4) for db in range(4)]
        gu_loaded, wd_loaded = [], []

        def gu_prefetch(upto):
            while len(gu_loaded) < min(upto, len(gu_list)):
                gu_loaded.append(load_gu(*gu_list[len(gu_loaded)]))

        def wd_prefetch(upto):
            while len(wd_loaded) < min(upto, len(wd_list)):
                wd_loaded.append(load_wd(*wd_list[len(wd_loaded)]))

        gu_prefetch(NGU - 1)
        wd_prefetch(1)
        prev_scatter = []
        h2r = [("h2_all", j_) for j_ in range(4)]
        def expert(e_, prev_scatter):
            for c in range(NT):
                stile = ST_[c % 4]
                sc.add("dve", lambda e, c=c, stile=stile: e.tensor_scalar(out=stile, in0=iota_f[:], scalar1=POS[:, e_, c:c + 1],
                                                                          scalar2=None, op0=ALU.is_equal),
                       reads=["pos", "iota_f"], writes=[("stile", c % 4)])

                def fsel(e, c=c, stile=stile):
                    r_ = None
                    if c == 0:
                        e.matmul(pb(6)[:, 0:32], lhsT=ident_b[:], rhs=zero_b[:], start=True, stop=False)
                    for sg in range(4):
                        r_ = e.matmul(pb(6)[:, sg * 8:sg * 8 + 6], lhsT=stile[:, sg * 128:(sg + 1) * 128],
                                      rhs=AP3[:, e_, c, :], start=False, stop=(c == NT - 1))
                    return r_
                sc.add("pe", fsel, reads=[("stile", c % 4), "zero_b", "ident_b"] + ap3r, writes=[("pb", 6)])
            ik = ("idx", e_)
            psc = pselc[:, e_ * 32:(e_ + 1) * 32]
            sc.add("dve", lambda e, psc=psc: e.tensor_copy(out=psc, in_=pb(6)[:, 0:32]), reads=[("pb", 6)], writes=[("psc", e_)])
            psv = psc.rearrange("p (a b) -> p a b", a=4)
            fb = e_ * 20
            sc.add("dve", lambda e, psv=psv, fb=fb: e.scalar_tensor_tensor(out=idxf[:, fb:fb + 4], in0=psv[:, :, 0], scalar=128.0,
                                                                           in1=psv[:, :, 2], op0=ALU.mult, op1=ALU.add),
                   reads=[("psc", e_)], writes=[ik + (0,)])
            for db in range(1, 4):
                sc.add("dve", lambda e, fb=fb, db=db: e.tensor_scalar(out=idxf[:, fb + 4 * db:fb + 4 * db + 4], in0=idxf[:, fb:fb + 4],
                                                                      scalar1=float(S * db), scalar2=None, op0=ALU.add),
                       reads=[ik + (0,)], writes=[ik + (0, db)])
            sc.add("dve", lambda e, psv=psv, fb=fb: e.scalar_tensor_tensor(out=idxf[:, fb + 16:fb + 20], in0=psv[:, :, 1], scalar=128.0,
                                                                           in1=psv[:, :, 2], op0=ALU.mult, op1=ALU.add),
                   reads=[("psc", e_)], writes=[ik + (1,)])
            sc.add("dve", lambda e, fb=fb: e.tensor_copy(out=idx_i[:, e_, :], in_=idxf[:, fb:fb + 20]),
                   reads=[ik + (0,), ik + (1,)] + [ik + (0, db) for db in range(1, 4)], writes=[ik])
            sc.add("dve", lambda e, psv=psv: e.tensor_reduce(out=gts[:, e_ * 4:(e_ + 1) * 4], in_=psv[:, :, 3:6], axis=AX.X, op=ALU.add),
                   reads=[("psc", e_)], writes=[("gate", e_)])
            if debug:
                sc.add("sync", lambda e: e.dma_start(out=dbg["idx"][:, e_ * 4:(e_ + 1) * 4], in_=idx_i[:, e_, 0:4]),
                       reads=[ik], writes=[("dbgidx", e_)], kind="dma")
                sc.add("sync", lambda e: e.dma_start(out=dbg["gate"][:, e_ * 4:(e_ + 1) * 4], in_=gts[:, e_ * 4:(e_ + 1) * 4]),
                       reads=[("gate", e_)], writes=[("dbggate", e_)], kind="dma")
            for sg in range(4):
                sc.add("pool", lambda e, sg=sg: e.indirect_dma_start(
                    out=XSG[:, sg, :], out_offset=None, in_=h2_all,
                    in_offset=bass.IndirectOffsetOnAxis(ap=idx_i[:, e_, 16 + sg:17 + sg], axis=0)),
                    reads=h2r + [ik], writes=[("xsg", sg)], kind="dma")
            for sg in range(4):
                for half in range(2):
                    bk = (sg * 2 + half) % 2

                    def ftr(e, sg=sg, half=half, bk=bk):
                        r_ = None
                        for j in range(8):
                            kc = half * 8 + j
                            r_ = e.transpose(out=pbb(bk)[:, j * 128:(j + 1) * 128], in_=XSG[:, sg, kc * 128:(kc + 1) * 128],
                                             identity=ident_b[:])
                        return r_
                    sc.add("pe", ftr, reads=[("xsg", sg), "ident_b"], writes=[("pb", bk)])
                    ce = "act" if half == 0 else "dve"
                    if ce == "act":
                        sc.add("act", lambda e, sg=sg, half=half, bk=bk: e.copy(
                            out=XST[:, half * 8:(half + 1) * 8, sg * 128:(sg + 1) * 128],
                            in_=pbb(bk).rearrange("p (a b) -> p a b", a=8)),
                            reads=[("pb", bk)], writes=[("xst", sg, half)])
                    else:
                        sc.add("dve", lambda e, sg=sg, half=half, bk=bk: e.tensor_copy(
                            out=XST[:, half * 8:(half + 1) * 8, sg * 128:(sg + 1) * 128],
                            in_=pbb(bk).rearrange("p (a b) -> p a b", a=8)),
                            reads=[("pb", bk)], writes=[("xst", sg, half)])
            xstr = [("xst", sg, half) for sg in range(4) for half in range(2)]
            for fc in range(NFC):
                n_ = e_ * NFC + fc
                gu_prefetch(n_ + NGU)
                k = gu_loaded[n_]
                pg, pu = 2 + 2 * (fc % 2), 3 + 2 * (fc % 2)

                def fgu(e, k=k, pg=pg, pu=pu):
                    r_ = None
                    for kc in range(16):
                        r_ = e.matmul(pb(pg), lhsT=WG[k][:, kc, :], rhs=XST[:, kc, :], start=(kc == 0), stop=(kc == 15))
                    for kc in range(16):
                        r_ = e.matmul(pb(pu), lhsT=WU[k][:, kc, :], rhs=XST[:, kc, :], start=(kc == 0), stop=(kc == 15))
                    return r_
                sc.add("pe", fgu, reads=xstr + [("wg", k), ("wu", k)], writes=[("pb", pg), ("pb", pu)])
                sgt = SG if fc % 2 == 0 else SG2
                sc.add("act", lambda e, pg=pg, sgt=sgt: e.activation(out=sgt, in_=pb(pg), func=AF.Silu),
                       reads=[("pb", pg)], writes=[("sg", fc % 2)])
                sc.add("dve", lambda e, pu=pu, sgt=sgt, fc=fc: e.tensor_tensor(out=HT[:, fc, :], in0=sgt, in1=pb(pu), op=ALU.mult),
                       reads=[("sg", fc % 2), ("pb", pu)], writes=[("ht", fc)])
            htr = [("ht", fc) for fc in range(NFC)]
            scat = []
            for db in range(4):
                nd = e_ * 4 + db
                wd_prefetch(nd + 2)
                kd = wd_loaded[nd]
                for sg in range(4):
                    m_ = db * 4 + sg
                    bk = m_ % 2

                    def fdn(e, sg=sg, kd=kd, bk=bk):
                        r_ = None
                        for fc in range(NFC):
                            r_ = e.matmul(pb(bk), lhsT=HT[:, fc, sg * 128:(sg + 1) * 128], rhs=WD[kd][:, fc, :],
                                          start=(fc == 0), stop=(fc == NFC - 1))
                        return r_
                    sc.add("pe", fdn, reads=htr + [("wd", kd, 0), ("wd", kd, 1)], writes=[("pb", bk)])
                    yt = YT[m_ % 4]
                    if m_ % 2 == 0:
                        sc.add("act", lambda e, bk=bk, yt=yt, sg=sg: e.activation(out=yt, in_=pb(bk), func=AF.Copy,
                                                                                  scale=gts[:, e_ * 4 + sg:e_ * 4 + sg + 1]),
                               reads=[("pb", bk), ("gate", e_)], writes=[("yt", m_ % 4)])
                    else:
                        sc.add("dve", lambda e, bk=bk, yt=yt, sg=sg: e.tensor_scalar(out=yt, in0=pb(bk),
                                                                                     scalar1=gts[:, e_ * 4 + sg:e_ * 4 + sg + 1],
                                                                                     scalar2=None, op0=ALU.mult),
                               reads=[("pb", bk), ("gate", e_)], writes=[("yt", m_ % 4)])
                    scat.append(sc.add("pool", lambda e, db=db, sg=sg, yt=yt: e.indirect_dma_start(
                        out=part,
                        out_offset=bass.IndirectOffsetOnAxis(ap=idx_i[:, e_, db * 4 + sg:db * 4 + sg + 1], axis=0),
                        in_=yt, in_offset=None, compute_op=ALU.add),
                        reads=[("yt", m_ % 4), ik] + partz_all + [("part_x1", i, db_) for i in range(8) for db_ in range(4)],
                        writes=[("part_sc", e_, db, sg)], kind="dma", extra=prev_scatter))
            return scat

        for e_ in range(4):
            prev_scatter = expert(e_, prev_scatter)
        all_sc = [("part_sc", e_, db, sg) for e_ in range(4) for db in range(4) for sg in range(4)]
        sc.add("pool", lambda e: e.collective_compute("ReduceScatter", ALU.add, replica_groups=GROUPS,
                                                      ins=[part.opt()], outs=[rs_out.opt()]),
               reads=all_sc + partz_all + [("part_x1", i, db_) for i in range(8) for db_ in range(4)], writes=["rs_out"], kind="cc")
        for i in range(8):
            sc.add("sync" if i % 2 == 0 else "pool",
                   lambda e, i=i: e.dma_start(out=out[i * 512:(i + 1) * 512, :], in_=rs_out[i * 512:(i + 1) * 512, :]),
                   reads=["rs_out"], writes=[("out", i)], kind="dma")
        sc.emit(block)
    return nc


def _na_bias_tables(rpb_h):
    ki = np.arange(128)[:, None]
    qi = np.arange(128)[None, :]

    def pat(m, kt):
        qr = 2 * m + qi // GRID_W
        qc = qi % GRID_W
        kr = 2 * kt + ki // GRID_W
        kc = ki % GRID_W
        rs = np.clip(qr - 4, 0, 56)
        cs = np.clip(qc - 8, 0, GRID_W - 16)
        valid = (kr >= rs) & (kr < rs + 8) & (kc >= cs) & (kc < cs + 16)
        ro = np.clip(kr - qr + 7, 0, 14)
        co = np.clip(kc - qc, -15, 15) + 15
        return np.where(valid, rpb_h[ro, co], np.float32(NEG)).astype(np.float32)

    full_mask = np.full((128, 128), NEG, np.float32)
    pats = [pat(10, 10 + d) for d in (-2, -1, 0, 1, 2)]
    for m in (0, 1):
        pats += [pat(m, kt) for kt in (0, 1, 2, 3)] + [full_mask]
    for m in (30, 31):
        pats += [pat(m, kt) for kt in (28, 29, 30, 31)] + [full_mask]
    return np.ascontiguousarray(np.stack(pats, axis=1))


def _alibi_tables(g):
    slope = np.float32(2.0 ** (-8.0 * (g + 1) / 4))
    ki = np.arange(128, dtype=np.float32)[:, None]
    qi = np.arange(256, dtype=np.float32)[None, :]
    t = np.stack([-slope * (qi - ki), slope * (qi - ki), -slope * np.abs(qi - ki), -slope * np.abs(qi - ki - 128.0)],
                 axis=1).astype(np.float32)
    cb = np.zeros((128, 64), np.float32)
    for delta in range(-31, 31):
        if delta >= 1:
            cb[:, delta + 32] = -slope * 128.0 * delta
        elif delta <= -2:
            cb[:, delta + 32] = slope * 128.0 * delta
    return np.ascontiguousarray(t), cb


def _prep_inputs(inp):
    f = lambda a: np.ascontiguousarray(np.asarray(a, dtype=np.float32))
    x = f(inp["x"])
    w_in = f(inp["w_in"])[0]
    w_out = f(inp["w_out"])[0]
    on_a = f(inp["on_a"])[0]
    subln = f(inp["subln_b"])[0]
    rpb = f(inp["rpb_a"])[0]
    wg, wu, wd = np.asarray(inp["w_gate"])[0], np.asarray(inp["w_up"])[0], np.asarray(inp["w_down"])[0]
    ln1T = np.ascontiguousarray(f(inp["ln1_g"])[0].reshape(16, 128).T)
    qkg = np.ascontiguousarray(np.stack([f(inp["qn_a"])[0]] * 2 + [f(inp["kn_a"])[0]] * 2 + [f(inp["qn_b"])[0]] * 2
                                        + [f(inp["kn_b"])[0]] * 2, axis=1))
    lamv = np.ascontiguousarray(np.stack([f(inp["lam_q1"])[0], f(inp["lam_k1"])[0], f(inp["lam_q2"])[0], f(inp["lam_k2"])[0]]))
    ln2 = f(inp["ln2_g"])
    w_router = f(inp["w_router"])[0]
    maps = []
    p = np.arange(128)
    cc_ = np.arange(NT)
    fcv = np.ascontiguousarray(np.broadcast_to((8 * ((cc_ % 8) // 2) + 2 * (cc_ // 8) + (cc_ % 2)).astype(np.float32), (128, NT)))
    for c in range(8):
        b, g = c // 4, c % 4
        cols = np.concatenate([
            np.arange(256 * g, 256 * g + 256), 1024 + np.arange(256 * g, 256 * g + 256),
            3072 + np.arange(128 * g, 128 * g + 128), 3584 + np.arange(128 * g, 128 * g + 128),
            4096 + np.arange(128 * g, 128 * g + 128), 4608 + np.arange(128 * g, 128 * g + 128),
            2048 + np.arange(256 * g, 256 * g + 256), 5120 + np.arange(256 * g, 256 * g + 256)])
        rows, wog_cols = [], []
        for r in range(4):
            rows += [np.arange(256 * r, 256 * r + 256), 1024 + np.arange(256 * r, 256 * r + 256)]
            wog_cols += [on_a[256 * r:256 * r + 128], on_a[256 * r + 128:256 * r + 256], subln[0:128], subln[128:256]]
        rows = np.concatenate(rows)
        alib, cb = _alibi_tables(g)
        sel = np.zeros((128, 4, N_EXP), np.float32)
        for e_ in range(4):
            sel[:, e_, 4 * g + e_] = 1.0
        own = (1024 * g + np.arange(8)[None, :] * 128 + p[:, None]).astype(np.int32)
        mixi = (4096 * g + np.arange(4)[None, None, :] * 1024 + (np.arange(8)[None, :] * 128 + p[:, None])[:, :, None]).astype(np.int32)
        maps.append({
            "x_b": x[b], "x_own": np.ascontiguousarray(x[b, 1024 * g:1024 * (g + 1)]),
            "w_in_g": np.ascontiguousarray(w_in[:, cols]), "ln1T": ln1T, "qkg": qkg,
            "nab": np.ascontiguousarray(np.stack([_na_bias_tables(rpb[2 * g]), _na_bias_tables(rpb[2 * g + 1])])),
            "alib": alib, "cbias": cb, "lamv": lamv,
            "w_out_p": np.ascontiguousarray(w_out[rows]), "wog": np.ascontiguousarray(np.stack(wog_cols, axis=1)),
            "ln2": ln2, "w_router": w_router, "sel": sel, "own_tok": np.ascontiguousarray((own[:, :, None] + S * np.arange(4)[None, None, :]).astype(np.int32)),
            "mix_idx": np.ascontiguousarray(mixi), "fcv": fcv,
            "wg_e": np.ascontiguousarray(wg[4 * g:4 * g + 4], dtype=np.float32),
            "wu_e": np.ascontiguousarray(wu[4 * g:4 * g + 4], dtype=np.float32),
            "wd_e": np.ascontiguousarray(wd[4 * g:4 * g + 4], dtype=np.float32),
        })
    return maps


def kernel(**inputs):
    maps = _prep_inputs(inputs)
    nc = build_nc()
    res = run_bass_kernel_spmd(nc, maps, core_ids=list(range(8)))
    out = np.empty((2, S, D), np.float32)
    for c in range(8):
        b, g = c // 4, c % 4
        out[b, :, 512 * g:512 * (g + 1)] = np.asarray(res.results[c]["out"], dtype=np.float32)
    return out
```

```python
import numpy as np
from contextlib import ExitStack
import concourse.bass as bass
import concourse.mybir as mybir
from concourse.bass_utils import run_bass_kernel_spmd

F32 = mybir.dt.float32
BF16 = mybir.dt.bfloat16
I32 = mybir.dt.int32
U8 = mybir.dt.uint8
AF = mybir.ActivationFunctionType
ALU = mybir.AluOpType
AX = mybir.AxisListType

D = 2048
S = 4096
NT = 32
GRID_W = 64
EPS = 1e-6
N_EXP = 16
CAP = 512
FF = 2816
NFC = FF // 128
GROUPS = [[0, 1, 2, 3], [4, 5, 6, 7]]
SCALE = 128.0 ** -0.5
LAM_INIT = 0.8 - 0.6
NEG = -30000.0
NPAT = 25
BISECT_ITERS = 24


class Sched:
    COMPUTE = ("act", "dve", "pool", "pe")

    def __init__(self, nc, es, rings):
        self.nc = nc
        self.ops = []
        self.lastw = {}
        self.readers = {}
        self.sems = []
        self.csem = {}
        for e in self.COMPUTE:
            self.csem[e] = self._sem(es, "c_" + e)
        self.rings = {e: [self._sem(es, f"d_{e}_{i}") for i in range(k)] for e, k in rings.items()}
        self.dma_count = {e: 0 for e in rings}
        self.es = es
        self.barrier_deps = []
        self.recent = {e: None for e in self.COMPUTE}
        self.recent_dma = {e: [] for e in rings}
        self.cc_ops = []
        self.cc_pool = [self._sem(es, f"cc_{i}") for i in range(12)]

    def _sem(self, es, name):
        self.sems.append(es.enter_context(self.nc.semaphore(name)))
        return len(self.sems) - 1

    def add(self, eng, fn, reads=(), writes=(), kind="c", extra=()):
        oid = len(self.ops)
        deps = {}
        for r in reads:
            w = self.lastw.get(r)
            if w is not None:
                deps.setdefault(w, set()).add("raw")
        for w_ in writes:
            w = self.lastw.get(w_)
            if w is not None:
                deps.setdefault(w, set()).add("waw")
            for rd in self.readers.get(w_, ()):
                deps.setdefault(rd, set()).add("war")
        for d in self.barrier_deps:
            deps.setdefault(d, set()).add("raw")
        for d in extra:
            deps.setdefault(d, set()).add("raw")
        fdeps = []
        for d, types in deps.items():
            p = self.ops[d]
            if p["kind"] == "c" and p["eng"] == eng and kind == "c":
                if eng == "pe" or "raw" not in types:
                    continue
            fdeps.append(d)
        op = dict(id=oid, eng=eng, fn=fn, kind=kind, deps=fdeps, has_dep=False, sem=None, val=0, prev=0)
        if kind == "dma":
            j = self.dma_count[eng]
            self.dma_count[eng] += 1
            K = len(self.rings[eng])
            op["sem"] = self.rings[eng][j % K]
            op["val"] = 16 * (j // K + 1)
            op["prev"] = 16 * (j // K)
            self.recent_dma[eng].append(oid)
            self.recent_dma[eng] = self.recent_dma[eng][-K:]
        elif kind == "cc":
            op["sem"] = self.cc_pool[len(self.cc_ops)]
            op["val"] = 1
            self.cc_ops.append(oid)
        else:
            self.recent[eng] = oid
        for r in reads:
            self.readers.setdefault(r, []).append(oid)
        for w_ in writes:
            self.lastw[w_] = oid
            self.readers[w_] = []
        self.ops.append(op)
        return oid

    def barrier(self, include_cc=True):
        deps = [v for v in self.recent.values() if v is not None]
        for lst in self.recent_dma.values():
            deps += lst
        if include_cc:
            deps += self.cc_ops
        self.barrier_deps = deps

    def emit(self, block):
        ops = self.ops
        for op in ops:
            for d in op["deps"]:
                ops[d]["has_dep"] = True
        cnt = {e: 0 for e in self.COMPUTE}
        for op in ops:
            if op["kind"] == "c" and op["has_dep"]:
                cnt[op["eng"]] += 1
                op["sem"] = self.csem[op["eng"]]
                op["val"] = cnt[op["eng"]]
        for e, c in cnt.items():
            assert c < 60000, (e, c)
        lists = {}
        for op in ops:
            lists.setdefault(op["eng"], []).append(op)
        final = {}
        for op in ops:
            if op["sem"] is not None:
                final[op["sem"]] = max(final.get(op["sem"], 0), op["val"])
        sems = self.sems

        def run(name, eng):
            waited = {}
            for op in lists.get(name, []):
                waits = {}
                for d in op["deps"]:
                    p = ops[d]
                    waits[p["sem"]] = max(waits.get(p["sem"], 0), p["val"])
                if op["kind"] == "dma" and op["prev"] > 0:
                    waits[op["sem"]] = max(waits.get(op["sem"], 0), op["prev"])
                for s_, v in waits.items():
                    if waited.get(s_, 0) < v:
                        eng.wait_ge(sems[s_], v)
                        waited[s_] = v
                ins = op["fn"](eng)
                if op["kind"] == "dma":
                    ins.then_inc(sems[op["sem"]], 16)
                elif op["kind"] == "cc":
                    ins.then_inc(sems[op["sem"]], 1)
                elif op["has_dep"]:
                    ins.then_inc(sems[op["sem"]], 1)
            if name == "sync":
                for s_, v in final.items():
                    if waited.get(s_, 0) < v:
                        eng.wait_ge(sems[s_], v)

        @block.sync
        def _(e):
            run("sync", e)

        @block.scalar
        def _(e):
            run("act", e)

        @block.vector
        def _(e):
            run("dve", e)

        @block.gpsimd
        def _(e):
            run("pool", e)

        @block.tensor
        def _(e):
            run("pe", e)


class Arena:
    def __init__(self, ar, size):
        self.ar = ar
        self.size = size

    def view(self, off, shape, dt):
        esz = {F32: 4, BF16: 2, I32: 4}[dt]
        n = int(np.prod(shape[1:]))
        assert off % 4 == 0 and off + n * esz <= self.size, (off, shape, self.size)
        v = self.ar[:, off:off + n * esz].bitcast(dt)
        if len(shape) == 3:
            v = v.rearrange("p (a b) -> p a b", a=shape[1])
        elif len(shape) == 4:
            v = v.rearrange("p (a b c) -> p a b c", a=shape[1], b=shape[2])
        return v


def bc(ap, shape):
    return ap.unsqueeze(len(ap.shape)).to_broadcast(list(shape))


def build_nc(debug=False, stop_after=None):
    nc = bass.Bass("TRN2", target_bir_lowering=False)

    def din(name, shape, dt=F32):
        return nc.dram_tensor(name, list(shape), dt, kind="ExternalInput").ap()

    x_b = din("x_b", [S, D])
    x_own = din("x_own", [1024, D])
    w_in_g = din("w_in_g", [D, 1536])
    ln1T = din("ln1T", [128, 16])
    qkg = din("qkg", [128, 8])
    nab = din("nab", [2, 128, NPAT, 128])
    alib = din("alib", [128, 4, 256])
    cbias = din("cbias", [128, 64])
    lamv = din("lamv", [4, 128])
    w_out_p = din("w_out_p", [D, D])
    wog = din("wog", [128, 16])
    ln2 = din("ln2", [1, D])
    w_router = din("w_router", [D, N_EXP])
    sel = din("sel", [128, 4, N_EXP])
    fcv = din("fcv", [128, NT])
    own_tok = din("own_tok", [128, 8, 4], I32)
    mix_idx = din("mix_idx", [128, 8, 4], I32)
    if stop_after is None:
        wg_e = din("wg_e", [4, D, FF])
        wu_e = din("wu_e", [4, D, FF])
        wd_e = din("wd_e", [4, FF, D])
    out = nc.dram_tensor("out", [S, 512], F32, kind="ExternalOutput").ap()

    ag1_in = nc.dram_tensor("ag1_in", [S, 512], BF16).ap()
    ag1_out = nc.dram_tensor("ag1_out", [4 * S, 512], BF16).ap()
    h2_in = nc.dram_tensor("h2_in", [1024, D], BF16).ap()
    h2_all = nc.dram_tensor("h2_all", [S, D], BF16).ap()
    aff_in = nc.dram_tensor("aff_in", [1024, N_EXP], F32).ap()
    aff_all = nc.dram_tensor("aff_all", [S, N_EXP], F32).ap()
    part = nc.dram_tensor("part", [4 * S, 512], F32).ap()
    rs_out = nc.dram_tensor("rs_out", [S, 512], F32).ap()
    dbg = {}
    if debug:
        dbg["mix"] = nc.dram_tensor("dbg_mix", [S, 512], BF16, kind="ExternalOutput").ap()
        dbg["x1"] = nc.dram_tensor("dbg_x1", [1024, D], F32, kind="ExternalOutput").ap()
        dbg["aff"] = nc.dram_tensor("dbg_aff", [S, N_EXP], F32, kind="ExternalOutput").ap()
        dbg["idx"] = nc.dram_tensor("dbg_idx", [128, 16], I32, kind="ExternalOutput").ap()
        dbg["gate"] = nc.dram_tensor("dbg_gate", [128, 16], F32, kind="ExternalOutput").ap()

    ARENA = 196 * 1024
    with ExitStack() as es:
        ar_t = es.enter_context(nc.sbuf_tensor("arena", [128, ARENA], U8))
        A = Arena(ar_t, ARENA)
        small = es.enter_context(nc.sbuf_tensor("small", [128, 1024], F32))
        ident_b = es.enter_context(nc.sbuf_tensor("ident_b", [128, 128], BF16))
        ident_f = es.enter_context(nc.sbuf_tensor("ident_f", [128, 128], F32))
        iota_f = es.enter_context(nc.sbuf_tensor("iota_f", [128, 512], F32))
        iota_p = es.enter_context(nc.sbuf_tensor("iota_p", [128, 1], F32))
        zero_b = es.enter_context(nc.sbuf_tensor("zero_b", [128, 32], BF16))
        pbanks = [es.enter_context(nc.psum_tensor(f"pb{i}", [128, 512], F32)) for i in range(8)]
        sc = Sched(nc, es, rings={"sync": 16, "pool": 24})
        block = es.enter_context(nc.Block())

        so = [0]

        def sm(n):
            v = small[:, so[0]:so[0] + n]
            so[0] += n
            assert so[0] <= 1024
            return v

        eps_c = sm(1)
        ones_b = A.view(ARENA - 256, [128, 128], BF16)
        ARENA_USE = ARENA - 256

        def pb(i):
            return pbanks[i][:]

        def pbb(i):
            return pbanks[i][:].bitcast(BF16)

        sc.add("pool", lambda e: e.memset(eps_c, EPS), writes=["eps"])
        sc.add("pool", lambda e: e.iota(iota_f[:], pattern=[[1, 512]], base=0, channel_multiplier=0,
                                        allow_small_or_imprecise_dtypes=True), writes=["iota_f"])
        sc.add("pool", lambda e: e.iota(iota_p[:], pattern=[[0, 1]], base=0, channel_multiplier=1,
                                        allow_small_or_imprecise_dtypes=True), writes=["iota_p"])
        sc.add("dve", lambda e: e.tensor_scalar(out=ident_f[:], in0=iota_f[:, 0:128], scalar1=iota_p[:, 0:1],
                                                scalar2=None, op0=ALU.is_equal),
               reads=["iota_f", "iota_p"], writes=["ident_f"])
        sc.add("dve", lambda e: e.tensor_copy(out=ident_b[:], in_=ident_f[:]), reads=["ident_f"], writes=["ident_b"])
        sc.add("pool", lambda e: e.memset(ones_b, 1.0), writes=["ones_b"])
        sc.add("pool", lambda e: e.memset(zero_b[:], 0.0), writes=["zero_b"])

        QKT = A.view(0, [128, 8, S], BF16)
        VA = A.view(65536, [128, NT, 2, 130], BF16)
        VB = A.view(82176, [128, NT, 258], BF16)
        R1 = 98688
        WP = A.view(R1, [128, 16, 1536], BF16)
        XIN = [A.view(R1 + 49152 + i * 8192, [128, D], F32) for i in range(2)]
        R2 = R1 + 65536
        XS = [A.view(R2 + i * 4096, [128, D], BF16) for i in range(2)]
        XT = [A.view(R2 + 8192 + i * 4096, [128, 16, 128], BF16) for i in range(2)]
        SQ = A.view(R2 + 16384, [128, 8, 128], F32)
        QKB = [A.view(R2 + 20480 + i * 2048, [128, 8, 128], BF16) for i in range(2)]
        JUNK = A.view(R2 + 24576, [128, D], BF16)
        assert R2 + 28672 <= ARENA_USE

        ln1T_s = sm(16)
        qkg_s = sm(8)
        ssx = sm(NT)
        rsx = sm(NT)
        ssqk = sm(8 * 2)
        rsqk = sm(8 * 2)

        sc.add("sync", lambda e: e.dma_start(out=ln1T_s, in_=ln1T), writes=["ln1T"], kind="dma")
        sc.add("sync", lambda e: e.dma_start(out=qkg_s, in_=qkg), writes=["qkg"], kind="dma")
        sc.add("pool", lambda e: e.memset(VA[:, :, :, 128:130], 1.0), writes=["VAones"])
        sc.add("pool", lambda e: e.memset(VB[:, :, 256:258], 1.0), writes=["VBones"])
        w_in_v = w_in_g.rearrange("(kc p) n -> p kc n", p=128)
        for kc in range(16):
            sc.add("pool", lambda e, kc=kc: e.dma_start(out=WP[:, kc, :], in_=w_in_v[:, kc, :]),
                   writes=[("WPraw", kc)], kind="dma")
        for kc in range(16):
            if kc % 2 == 0:
                sc.add("dve", lambda e, kc=kc: e.tensor_scalar(out=WP[:, kc, :], in0=WP[:, kc, :],
                                                               scalar1=ln1T_s[:, kc:kc + 1], scalar2=None, op0=ALU.mult),
                       reads=[("WPraw", kc), "ln1T"], writes=[("WP", kc)])
            else:
                sc.add("act", lambda e, kc=kc: e.activation(out=WP[:, kc, :], in_=WP[:, kc, :], func=AF.Copy,
                                                            scale=ln1T_s[:, kc:kc + 1]),
                       reads=[("WPraw", kc), "ln1T"], writes=[("WP", kc)])

        def a_load(tt):
            sc.add("sync", lambda e: e.dma_start(out=XIN[tt % 2], in_=x_b[tt * 128:(tt + 1) * 128, :]),
                   writes=[("xin", tt % 2)], kind="dma")

        def a_norm(tt):
            s = tt % 2
            sc.add("act", lambda e: e.activation(out=JUNK, in_=XIN[s], func=AF.Square, accum_out=ssx[:, tt:tt + 1]),
                   reads=[("xin", s)], writes=["junk", ("ssx", tt)])
            sc.add("act", lambda e: e.activation(out=rsx[:, tt:tt + 1], in_=ssx[:, tt:tt + 1], func=AF.Sqrt,
                                                 scale=1.0 / D, bias=eps_c),
                   reads=[("ssx", tt), "eps"], writes=[("rsx0", tt)])
            sc.add("dve", lambda e: e.reciprocal(out=rsx[:, tt:tt + 1], in_=rsx[:, tt:tt + 1]),
                   reads=[("rsx0", tt)], writes=[("rsx", tt)])
            sc.add("act", lambda e: e.activation(out=XS[s], in_=XIN[s], func=AF.Copy, scale=rsx[:, tt:tt + 1]),
                   reads=[("xin", s), ("rsx", tt)], writes=[("xs", s)])

        def a_transpose(tt):
            s = tt % 2
            for half in range(2):
                def f(e, half=half):
                    r = None
                    for j in range(8):
                        kc = half * 8 + j
                        r = e.transpose(out=pbb(0)[:, j * 128:(j + 1) * 128], in_=XS[s][:, kc * 128:(kc + 1) * 128],
                                        identity=ident_b[:])
                    return r
                sc.add("pe", f, reads=[("xs", s), "ident_b"], writes=[("pb", 0)])
                sc.add("dve", lambda e, half=half: e.tensor_copy(
                    out=XT[s][:, half * 8:(half + 1) * 8, :],
                    in_=pbb(0).rearrange("p (a b) -> p a b", a=8)),
                    reads=[("pb", 0)], writes=[("xT", s, half)])

        def a_proj(tt):
            s = tt % 2
            base = 2 + 3 * (tt % 2)

            def f(e):
                r = None
                for kc in range(16):
                    for nb in range(3):
                        r = e.matmul(pb(base + nb), lhsT=XT[s][:, kc, :], rhs=WP[:, kc, nb * 512:(nb + 1) * 512],
                                     start=(kc == 0), stop=(kc == 15))
                return r
            sc.add("pe", f, reads=[("xT", s, 0), ("xT", s, 1)] + [("WP", kc) for kc in range(16)],
                   writes=[("pb", base + q_) for q_ in range(3)])

        def a_evac(tt):
            s = tt % 2
            base = 2 + 3 * (tt % 2)
            qk_ps = [pb(base), pb(base + 1)]
            for h2_ in range(2):
                sc.add("act", lambda e, h2_=h2_: e.activation(
                    out=SQ[:, h2_ * 4:(h2_ + 1) * 4, :], in_=qk_ps[h2_].rearrange("p (a b) -> p a b", a=4),
                    func=AF.Square), reads=[("pb", base + h2_)], writes=[("sq", h2_)])
            sc.add("dve", lambda e: e.tensor_reduce(out=ssqk[:, s * 8:(s + 1) * 8], in_=SQ, axis=AX.X, op=ALU.add),
                   reads=[("sq", 0), ("sq", 1)], writes=[("ssqk", s)])
            sc.add("act", lambda e: e.activation(out=rsqk[:, s * 8:(s + 1) * 8], in_=ssqk[:, s * 8:(s + 1) * 8],
                                                 func=AF.Sqrt, scale=1.0 / 128, bias=eps_c),
                   reads=[("ssqk", s), "eps"], writes=[("rsqk0", s)])
            sc.add("dve", lambda e: e.reciprocal(out=rsqk[:, s * 8:(s + 1) * 8], in_=rsqk[:, s * 8:(s + 1) * 8]),
                   reads=[("rsqk0", s)], writes=[("rsqk", s)])
            for h2_ in range(2):
                sc.add("dve", lambda e, h2_=h2_: e.tensor_tensor(
                    out=QKB[s][:, h2_ * 4:(h2_ + 1) * 4, :], in0=qk_ps[h2_].rearrange("p (a b) -> p a b", a=4),
                    in1=bc(rsqk[:, s * 8 + h2_ * 4:s * 8 + (h2_ + 1) * 4], [128, 4, 128]), op=ALU.mult),
                    reads=[("pb", base + h2_), ("rsqk", s)], writes=[("qkb", s, h2_)])
            sc.add("act", lambda e: e.copy(out=VA[:, tt, :, 0:128],
                                           in_=pb(base + 2)[:, 0:256].rearrange("p (a b) -> p a b", a=2)),
                   reads=[("pb", base + 2)], writes=[("VA", tt)])
            sc.add("act", lambda e: e.copy(out=VB[:, tt, 0:256], in_=pb(base + 2)[:, 256:512]),
                   reads=[("pb", base + 2)], writes=[("VB", tt)])

        def a_qkT(tt):
            s = tt % 2

            def f(e):
                r = None
                for j in range(8):
                    r = e.transpose(out=pbb(1)[:, j * 128:(j + 1) * 128], in_=QKB[s][:, j, :], identity=ident_b[:])
                return r
            sc.add("pe", f, reads=[("qkb", s, 0), ("qkb", s, 1), "ident_b"], writes=[("pb", 1)])
            sc.add("dve", lambda e: e.tensor_tensor(out=QKT[:, :, tt * 128:(tt + 1) * 128],
                                                    in0=pbb(1).rearrange("p (a b) -> p a b", a=8),
                                                    in1=bc(qkg_s, [128, 8, 128]), op=ALU.mult),
                   reads=[("pb", 1), "qkg"], writes=[("QKT", tt)])

        ZERO = A.view(R2 + 28672, [128, 1024], F32)
        sc.add("pool", lambda e: e.memset(ZERO, 0.0), writes=["zero"])
        part_v = part.rearrange("(a b p) n -> p a b n", p=128, b=2)

        def zero_fill(a_):
            sc.add("sync", lambda e: e.dma_start(out=part_v[:, a_, :, :], in_=ZERO.rearrange("p (b n) -> p b n", b=2)),
                   reads=["zero"], writes=[("partz", a_)], kind="dma")
        partz_all = [("partz", a_) for a_ in range(64)]
        a_load(0)
        a_load(1)
        a_norm(0)
        a_transpose(0)
        for tt in range(NT):
            a_proj(tt)
            if tt + 1 < NT:
                a_norm(tt + 1)
                a_transpose(tt + 1)
            if tt + 2 < NT:
                a_load(tt + 2)
            zero_fill(2 * tt)
            zero_fill(2 * tt + 1)
            if tt >= 1:
                a_qkT(tt - 1)
            a_evac(tt)
        a_qkT(NT - 1)
        sc.barrier()
        if stop_after == "A":
            sc.add("sync", lambda e: e.dma_start(out=out[0:128, :], in_=XIN[0]), writes=["outdummy"], kind="dma")
            sc.emit(block)
            return nc

        WO = A.view(R1, [128, 16, D], BF16)
        TS = [A.view(R2 + i * 2048, [128, 2, 256], F32) for i in range(3)]
        PT = [A.view(R2 + 6144 + i * 1024, [128, 2, 256], BF16) for i in range(3)]
        ALB = A.view(R2 + 9216, [128, 4, 256], F32)
        OB1 = A.view(R2 + 13312, [128, 256], F32)
        OBN = [A.view(R2 + 14336 + i * 1024, [128, 512], BF16) for i in range(2)]
        NAB = A.view(R2 + 16384, [128, NPAT, 128], F32)
        TNA = A.view(R2 + 29184, [128, 5, 128], F32)
        PNA = A.view(R2 + 31744, [128, 5, 128], BF16)
        LAMT = A.view(R2 + 33024, [128, 4, 128], F32)
        JB = A.view(R2 + 35072, [128, 256], F32)
        assert R2 + 36096 <= ARENA_USE
        cb_s = sm(64)
        wog_s = sm(16)
        lam_s = sm(8)
        dst = sm(64)
        nst = sm(8)

        sc.add("sync", lambda e: e.dma_start(out=ALB, in_=alib), writes=["alb"], kind="dma")
        sc.add("sync", lambda e: e.dma_start(out=cb_s, in_=cbias), writes=["cb"], kind="dma")
        sc.add("sync", lambda e: e.dma_start(out=wog_s, in_=wog), writes=["wog"], kind="dma")
        for i in range(4):
            sc.add("sync", lambda e, i=i: e.dma_start(out=LAMT[:, i, :], in_=lamv[i:i + 1, :].partition_broadcast(128)),
                   writes=[("lamt", i)], kind="dma")
        sc.add("dve", lambda e: e.tensor_tensor(out=LAMT[:, 0, :], in0=LAMT[:, 0, :], in1=LAMT[:, 1, :], op=ALU.mult),
               reads=[("lamt", 0), ("lamt", 1)], writes=["lp1"])
        sc.add("dve", lambda e: e.tensor_tensor(out=LAMT[:, 2, :], in0=LAMT[:, 2, :], in1=LAMT[:, 3, :], op=ALU.mult),
               reads=[("lamt", 2), ("lamt", 3)], writes=["lp2"])
        sc.add("dve", lambda e: e.tensor_reduce(out=lam_s[:, 0:1], in_=LAMT[:, 0, :], axis=AX.X, op=ALU.add),
               reads=["lp1"], writes=["ls1"])
        sc.add("dve", lambda e: e.tensor_reduce(out=lam_s[:, 1:2], in_=LAMT[:, 2, :], axis=AX.X, op=ALU.add),
               reads=["lp2"], writes=["ls2"])
        sc.add("act", lambda e: e.activation(out=lam_s[:, 2:4], in_=lam_s[:, 0:2], func=AF.Exp),
               reads=["ls1", "ls2"], writes=["lexp"])
        sc.add("dve", lambda e: e.tensor_tensor(out=lam_s[:, 4:5], in0=lam_s[:, 3:4], in1=lam_s[:, 2:3], op=ALU.subtract),
               reads=["lexp"], writes=["ldiff"])
        sc.add("dve", lambda e: e.tensor_scalar(out=lam_s[:, 5:6], in0=lam_s[:, 4:5], scalar1=-LAM_INIT, scalar2=None,
                                                op0=ALU.add),
               reads=["ldiff"], writes=["neglam"])

        w_out_v = w_out_p.rearrange("(kc p) n -> p kc n", p=128)
        for kc in range(16):
            sc.add("pool", lambda e, kc=kc: e.dma_start(out=WO[:, kc, :], in_=w_out_v[:, kc, :]),
                   writes=[("WOraw", kc)], kind="dma")
        for kc in range(16):
            mul2 = 1.0 if (kc % 4) < 2 else (1.0 - LAM_INIT)
            sc.add("pool", lambda e, kc=kc, mul2=mul2: e.tensor_scalar(
                out=WO[:, kc, :], in0=WO[:, kc, :], scalar1=wog_s[:, kc:kc + 1], scalar2=mul2, op0=ALU.mult, op1=ALU.mult),
                reads=[("WOraw", kc), "wog"], writes=[("WO", kc)])

        if stop_after == "B0":
            sc.add("sync", lambda e: e.dma_start(out=out[0:128, :], in_=XIN[0]), writes=["outdummy"], kind="dma")
            sc.emit(block)
            return nc
        SB = [4, 5, 6, 7]
        ACC = [0, 1, 2, 3]
        Q1, Q2, K1, K2 = 4, 5, 6, 7

        def b_score(qb, kt, n):
            bk = SB[n % 4]

            def f(e):
                e.matmul(pb(bk)[:, 0:256], lhsT=QKT[:, K1, kt * 128:(kt + 1) * 128],
                         rhs=QKT[:, Q1, qb * 256:(qb + 1) * 256], start=True, stop=True)
                return e.matmul(pb(bk)[:, 256:512], lhsT=QKT[:, K2, kt * 128:(kt + 1) * 128],
                                rhs=QKT[:, Q2, qb * 256:(qb + 1) * 256], start=True, stop=True)
            sc.add("pe", f, reads=[("QKT", kt), ("QKT", 2 * qb), ("QKT", 2 * qb + 1)], writes=[("pb", bk)])

        def b_soft(qb, kt, n):
            bk = SB[n % 4]
            delta = 2 * qb - kt
            if delta >= 1:
                ti = 0
            elif delta <= -2:
                ti = 1
            elif delta == 0:
                ti = 2
            else:
                ti = 3
            ci = delta + 32
            sc.add("dve", lambda e: e.scalar_tensor_tensor(
                out=TS[n % 3], in0=pb(bk).rearrange("p (a b) -> p a b", a=2), scalar=SCALE,
                in1=ALB[:, ti:ti + 1, :].to_broadcast([128, 2, 256]), op0=ALU.mult, op1=ALU.add),
                reads=[("pb", bk), "alb"], writes=[("ts", n % 3)])
            sc.add("act", lambda e: e.activation(out=PT[n % 3], in_=TS[n % 3], func=AF.Exp, bias=cb_s[:, ci:ci + 1]),
                   reads=[("ts", n % 3), "cb"], writes=[("pt", n % 3)])

        def b_pv(qb, kt, n):
            def f(e):
                r = None
                for i in range(2):
                    for j in range(2):
                        r = e.matmul(pb(ACC[i * 2 + j])[:, 0:258], lhsT=PT[n % 3][:, i, j * 128:(j + 1) * 128],
                                     rhs=VB[:, kt, :], start=(kt == 0), stop=(kt == NT - 1))
                return r
            sc.add("pe", f, reads=[("pt", n % 3), ("VB", kt), "VBones"], writes=[("pb", q_) for q_ in range(4)])

        def b_epilogue(qb):
            for j in range(2):
                tq = 2 * qb + j
                a1 = pb(ACC[j])
                a2 = pb(ACC[2 + j])
                st = dst[:, (tq % 4) * 8:(tq % 4) * 8 + 8]
                k = ("dst", tq % 4)
                ka1, ka2 = ("pb", ACC[j]), ("pb", ACC[2 + j])
                sc.add("dve", lambda e, a1=a1, st=st: e.reciprocal(out=st[:, 0:1], in_=a1[:, 256:257]),
                       reads=[ka1], writes=[k + (0,)])
                sc.add("dve", lambda e, a2=a2, st=st: e.reciprocal(out=st[:, 1:2], in_=a2[:, 256:257]),
                       reads=[ka2], writes=[k + (1,)])
                sc.add("dve", lambda e, st=st: e.tensor_tensor(out=st[:, 2:3], in0=st[:, 1:2], in1=lam_s[:, 5:6], op=ALU.mult),
                       reads=[k + (1,), "neglam"], writes=[k + (2,)])
                sc.add("dve", lambda e, a1=a1, st=st: e.tensor_scalar(out=OB1, in0=a1[:, 0:256], scalar1=st[:, 0:1],
                                                                      scalar2=None, op0=ALU.mult),
                       reads=[ka1, k + (0,)], writes=["ob1a"])
                sc.add("dve", lambda e, a2=a2, st=st: e.scalar_tensor_tensor(out=OB1, in0=a2[:, 0:256], scalar=st[:, 2:3],
                                                                             in1=OB1, op0=ALU.mult, op1=ALU.add),
                       reads=[ka2, "ob1a", k + (2,)], writes=["ob1"])
                sc.add("dve", lambda e: e.tensor_tensor(out=JB, in0=OB1, in1=OB1, op=ALU.mult), reads=["ob1"], writes=["jb"])
                sc.add("dve", lambda e, st=st: e.tensor_reduce(out=st[:, 3:4], in_=JB, axis=AX.X, op=ALU.add),
                       reads=["jb"], writes=[k + (3,)])
                sc.add("act", lambda e, st=st: e.activation(out=st[:, 4:5], in_=st[:, 3:4], func=AF.Ln, scale=1.0 / 256,
                                                            bias=eps_c),
                       reads=[k + (3,), "eps"], writes=[k + (4,)])
                sc.add("act", lambda e, st=st: e.activation(out=st[:, 5:6], in_=st[:, 4:5], func=AF.Exp, scale=-0.5),
                       reads=[k + (4,)], writes=[k + (5,)])
                sc.add("dve", lambda e, st=st, tq=tq: e.tensor_scalar(out=OBN[tq % 2][:, 256:512], in0=OB1, scalar1=st[:, 5:6],
                                                                      scalar2=None, op0=ALU.mult),
                       reads=["ob1", k + (5,)], writes=[("obn_b", tq % 2)])
                sc.add("sync", lambda e, tq=tq: e.dma_start(out=ag1_in[tq * 128:(tq + 1) * 128, 256:512],
                                                            in_=OBN[tq % 2][:, 256:512]),
                       reads=[("obn_b", tq % 2)], writes=[("ag1_in_b", tq)], kind="dma")

        if stop_after == "B":
            sc.add("sync", lambda e: e.dma_start(out=out[0:128, :], in_=XIN[0]), writes=["outdummy"], kind="dma")
            sc.emit(block)
            return nc
        na2 = es.enter_context(nc.sbuf_tensor("na2", [128, 3840], U8))
        TNAs = [TNA, na2[:, 0:2560].bitcast(F32).rearrange("p (a b) -> p a b", a=5)]
        PNAs = [PNA, na2[:, 2560:3840].bitcast(BF16).rearrange("p (a b) -> p a b", a=5)]
        NSAs, NSBs, NOs = [0, 2, 4], [1, 3, 5], [6, 7]

        def na_info(m):
            if m in (0, 1, 30, 31):
                sp = {0: 0, 1: 1, 30: 2, 31: 3}[m]
                p0 = 5 + sp * 5
            else:
                p0 = 0
            if m in (0, 1):
                kts = [0, 1, 2, 3, 3]
            elif m in (30, 31):
                kts = [28, 29, 30, 31, 31]
            else:
                kts = [m - 2 + i for i in range(5)]
            return p0, kts

        def na_score(hh, m):
            par3 = (hh * NT + m) % 3
            qch, kch = hh, 2 + hh
            p0, kts = na_info(m)
            nsa, nsb = NSAs[par3], NSBs[par3]

            def f(e):
                r = None
                for i in range(5):
                    o = pb(nsa)[:, i * 128:(i + 1) * 128] if i < 4 else pb(nsb)[:, 0:128]
                    r = e.matmul(o, lhsT=QKT[:, kch, kts[i] * 128:(kts[i] + 1) * 128],
                                 rhs=QKT[:, qch, m * 128:(m + 1) * 128], start=True, stop=True)
                return r
            sc.add("pe", f, reads=[("QKT", k_) for k_ in set(kts + [m])], writes=[("pb", nsa), ("pb", nsb)])

        def na_soft(hh, m):
            par = (hh * NT + m) % 2
            par3 = (hh * NT + m) % 3
            p0, kts = na_info(m)
            nsa, nsb = NSAs[par3], NSBs[par3]
            tna, pna = TNAs[par], PNAs[par]
            sc.add("dve", lambda e: e.scalar_tensor_tensor(
                out=tna[:, 0:4, :], in0=pb(nsa).rearrange("p (a b) -> p a b", a=4), scalar=SCALE,
                in1=NAB[:, p0:p0 + 4, :], op0=ALU.mult, op1=ALU.add),
                reads=[("pb", nsa), "nab"], writes=[("tna0", par)])
            sc.add("dve", lambda e: e.scalar_tensor_tensor(
                out=tna[:, 4, :], in0=pb(nsb)[:, 0:128], scalar=SCALE,
                in1=NAB[:, p0 + 4, :], op0=ALU.mult, op1=ALU.add),
                reads=[("pb", nsb), "nab"], writes=[("tna1", par)])
            sc.add("act", lambda e: e.activation(out=pna, in_=tna, func=AF.Exp),
                   reads=[("tna0", par), ("tna1", par)], writes=[("pna", par)])

        def na_out(hh, m):
            par = (hh * NT + m) % 2
            p0, kts = na_info(m)
            no = NOs[par]
            pna = PNAs[par]

            def f2(e):
                r = None
                for i in range(5):
                    r = e.matmul(pb(no)[:, 0:130], lhsT=pna[:, i, :], rhs=VA[:, kts[i], hh, :],
                                 start=(i == 0), stop=(i == 4))
                return r
            sc.add("pe", f2, reads=[("pna", par), "VAones"] + [("VA", k_) for k_ in set(kts)], writes=[("pb", no)])
            nsc = nst[:, par * 2 + hh:par * 2 + hh + 1]
            sc.add("dve", lambda e: e.reciprocal(out=nsc, in_=pb(no)[:, 128:129]), reads=[("pb", no)],
                   writes=[("nst", par, hh)])
            ob = OBN[m % 2]
            sc.add("dve", lambda e: e.tensor_scalar(out=ob[:, hh * 128:(hh + 1) * 128], in0=pb(no)[:, 0:128],
                                                    scalar1=nsc, scalar2=None, op0=ALU.mult),
                   reads=[("pb", no), ("nst", par, hh)], writes=[("obn_a", m % 2)])
            sc.add("sync", lambda e: e.dma_start(out=ag1_in[m * 128:(m + 1) * 128, hh * 128:(hh + 1) * 128],
                                                 in_=ob[:, hh * 128:(hh + 1) * 128]),
                   reads=[("obn_a", m % 2)], writes=[("ag1_in_a", hh, m)], kind="dma")

        for hh in range(2):
            sc.add("sync", lambda e, hh=hh: e.dma_start(out=NAB, in_=nab[hh]), writes=["nab"], kind="dma")
            na_score(hh, 0)
            na_score(hh, 1)
            na_soft(hh, 0)
            for m in range(NT):
                if m + 2 < NT:
                    na_score(hh, m + 2)
                if m + 1 < NT:
                    na_soft(hh, m + 1)
                na_out(hh, m)

        def ag_slab(k_):
            rd = [("ag1_in_b", t) for t in range(8 * k_, 8 * k_ + 8)] + \
                 [("ag1_in_a", h_, t) for h_ in range(2) for t in range(8 * k_, 8 * k_ + 8)]
            sc.add("pool", lambda e: e.collective_compute(
                "AllGather", ALU.bypass, replica_groups=GROUPS,
                ins=[ag1_in[1024 * k_:1024 * (k_ + 1), :].opt()], outs=[ag1_out[4096 * k_:4096 * (k_ + 1), :].opt()]),
                reads=rd, writes=[("ag1_out", k_)], kind="cc")

        seq = [(qb, kt) for qb in range(16) for kt in range(NT)]
        LOOK = 3
        for i in range(min(LOOK, len(seq))):
            b_score(seq[i][0], seq[i][1], i)
        for i, (qb, kt) in enumerate(seq):
            if i + LOOK < len(seq):
                b_score(seq[i + LOOK][0], seq[i + LOOK][1], i + LOOK)
            b_soft(qb, kt, i)
            b_pv(qb, kt, i)
            if kt == NT - 1:
                b_epilogue(qb)
                if qb % 4 == 3:
                    ag_slab(qb // 4)

        if stop_after == "C":
            sc.add("sync", lambda e: e.dma_start(out=out[0:128, :], in_=XIN[0]), writes=["outdummy"], kind="dma")
            sc.emit(block)
            return nc
        ag_reads = [("ag1_in_b", t) for t in range(NT)] + [("ag1_in_a", h_, t) for h_ in range(2) for t in range(NT)]
        if debug:
            for t_ in range(NT):
                sc.add("sync", lambda e, t_=t_: e.dma_start(out=dbg["mix"][t_ * 128:(t_ + 1) * 128, :],
                                                            in_=ag1_in[t_ * 128:(t_ + 1) * 128, :]),
                       reads=ag_reads, writes=[("dbg_mix", t_)], kind="dma")
        sc.barrier()
        if stop_after == "attn":
            sc.add("sync", lambda e: e.dma_start(out=out[0:128, :], in_=XIN[0]), writes=["outdummy"], kind="dma")
            sc.emit(block)
            return nc

        P0 = 0
        MIXT = [A.view(P0 + i * 4096, [128, 4, 512], BF16) for i in range(2)]
        MIXN = [A.view(P0 + 8192 + i * 4096, [128, 4, 512], BF16) for i in range(2)]
        MXT = [A.view(P0 + 16384 + i * 4096, [128, 16, 128], BF16) for i in range(2)]
        XO = [A.view(P0 + 24576 + i * 8192, [128, D], F32) for i in range(2)]
        X1 = [A.view(P0 + 40960 + i * 8192, [128, D], F32) for i in range(2)]
        H2F = A.view(P0 + 57344, [128, D], F32)
        H2T = A.view(P0 + 65536, [128, 16, 128], F32)
        H2B = [A.view(P0 + 73728 + i * 4096, [128, D], BF16) for i in range(2)]
        LN2R = A.view(P0 + 81920, [128, D], F32)
        WR = A.view(P0 + 90112, [128, 16, N_EXP], F32)
        JD = A.view(P0 + 95232, [128, D], BF16)
        assert P0 + 95232 + 4096 <= R1 + 65536
        JD = A.view(R2, [128, D], BF16)
        own_s = es.enter_context(nc.sbuf_tensor("own_s", [128, 8, 4], I32))
        mixi_s = es.enter_context(nc.sbuf_tensor("mixi_s", [128, 8, 4], I32))
        dstat = sm(8 * 8)
        logit = sm(8 * 16)
        affs = sm(8 * 16)

        sc.add("sync", lambda e: e.dma_start(out=own_s[:], in_=own_tok), writes=["own"], kind="dma")
        sc.add("sync", lambda e: e.dma_start(out=mixi_s[:], in_=mix_idx), writes=["mixi"], kind="dma")
        sc.add("sync", lambda e: e.dma_start(out=LN2R, in_=ln2[0:1, :].partition_broadcast(128)), writes=["ln2r"], kind="dma")
        sc.add("sync", lambda e: e.dma_start(out=WR, in_=w_router.rearrange("(kc p) n -> p kc n", p=128)),
               writes=["wr"], kind="dma")

        def d_front(i):
            s = i % 2
            st = dstat[:, i * 8:(i + 1) * 8]
            for r in range(4):
                sc.add("pool", lambda e, r=r: e.indirect_dma_start(
                    out=MIXT[s][:, r, :], out_offset=None, in_=ag1_out,
                    in_offset=bass.IndirectOffsetOnAxis(ap=mixi_s[:, i, r:r + 1], axis=0)),
                    reads=[("ag1_out", k_) for k_ in range(4)] + ["mixi"], writes=[("mixt", s, r)], kind="dma")
            sc.add("sync", lambda e: e.dma_start(out=XO[s], in_=x_own[i * 128:(i + 1) * 128, :]),
                   writes=[("xo", s)], kind="dma")
            mr = [("mixt", s, r) for r in range(4)]
            sc.add("act", lambda e: e.activation(out=JD[:, 0:1024].rearrange("p (a b) -> p a b", a=4),
                                                 in_=MIXT[s][:, :, 0:256], func=AF.Square, accum_out=st[:, 0:1]),
                   reads=mr, writes=["jd", ("dst0", i)])
            sc.add("act", lambda e: e.activation(out=st[:, 1:2], in_=st[:, 0:1], func=AF.Sqrt, scale=1.0 / 1024, bias=eps_c),
                   reads=[("dst0", i), "eps"], writes=[("dst1", i)])
            sc.add("dve", lambda e: e.reciprocal(out=st[:, 2:3], in_=st[:, 1:2]), reads=[("dst1", i)], writes=[("dst2", i)])
            sc.add("dve", lambda e: e.tensor_scalar(out=MIXN[s][:, :, 0:256], in0=MIXT[s][:, :, 0:256], scalar1=st[:, 2:3],
                                                    scalar2=None, op0=ALU.mult),
                   reads=mr + [("dst2", i)], writes=[("mixn_a", s)])
            sc.add("dve", lambda e: e.tensor_copy(out=MIXN[s][:, :, 256:512], in_=MIXT[s][:, :, 256:512]),
                   reads=mr, writes=[("mixn_b", s)])
            for half in range(2):
                def f(e, half=half):
                    r_ = None
                    for j in range(8):
                        kc = half * 8 + j
                        r_ = e.transpose(out=pbb(0)[:, j * 128:(j + 1) * 128],
                                         in_=MIXN[s][:, kc // 4, (kc % 4) * 128:(kc % 4 + 1) * 128], identity=ident_b[:])
                    return r_
                sc.add("pe", f, reads=[("mixn_a", s), ("mixn_b", s), "ident_b"], writes=[("pb", 0)])
                sc.add("act", lambda e, half=half: e.copy(out=MXT[s][:, half * 8:(half + 1) * 8, :],
                                                          in_=pbb(0).rearrange("p (a b) -> p a b", a=8)),
                       reads=[("pb", 0)], writes=[("mxt", s, half)])

        def d_proj(i):
            s = i % 2
            st = dstat[:, i * 8:(i + 1) * 8]
            x1r = [("x1", s, nb) for nb in range(4)]
            for nb in range(4):
                bk = 1 + (nb % 2)

                def f(e, nb=nb, bk=bk):
                    r_ = None
                    for kc in range(16):
                        r_ = e.matmul(pb(bk), lhsT=MXT[s][:, kc, :], rhs=WO[:, kc, nb * 512:(nb + 1) * 512],
                                      start=(kc == 0), stop=(kc == 15))
                    return r_
                sc.add("pe", f, reads=[("mxt", s, 0), ("mxt", s, 1)] + [("WO", kc) for kc in range(16)],
                       writes=[("pb", bk)])
                sc.add("dve", lambda e, nb=nb, bk=bk: e.tensor_tensor(out=X1[s][:, nb * 512:(nb + 1) * 512], in0=pb(bk),
                                                                      in1=XO[s][:, nb * 512:(nb + 1) * 512], op=ALU.add),
                       reads=[("pb", bk), ("xo", s)], writes=[("x1", s, nb)])
            for db in range(4):
                sc.add("pool", lambda e, db=db: e.indirect_dma_start(
                    out=part, out_offset=bass.IndirectOffsetOnAxis(ap=own_s[:, i, db:db + 1], axis=0),
                    in_=X1[s][:, db * 512:(db + 1) * 512], in_offset=None),
                    reads=x1r + ["own"] + partz_all, writes=[("part_x1", i, db)], kind="dma")
            if debug:
                sc.add("sync", lambda e: e.dma_start(out=dbg["x1"][i * 128:(i + 1) * 128, :], in_=X1[s]),
                       reads=x1r, writes=[("dbgx1", i)], kind="dma")

        def d_back(i):
            s = i % 2
            st = dstat[:, i * 8:(i + 1) * 8]
            x1r = [("x1", s, nb) for nb in range(4)]
            sc.add("act", lambda e: e.activation(out=JD, in_=X1[s], func=AF.Square, accum_out=st[:, 3:4]),
                   reads=x1r, writes=["jd", ("dst3", i)])
            sc.add("act", lambda e: e.activation(out=st[:, 4:5], in_=st[:, 3:4], func=AF.Sqrt, scale=1.0 / D, bias=eps_c),
                   reads=[("dst3", i), "eps"], writes=[("dst4", i)])
            sc.add("dve", lambda e: e.reciprocal(out=st[:, 5:6], in_=st[:, 4:5]), reads=[("dst4", i)], writes=[("dst5", i)])
            sc.add("dve", lambda e: e.scalar_tensor_tensor(out=H2F, in0=X1[s], scalar=st[:, 5:6], in1=LN2R,
                                                           op0=ALU.mult, op1=ALU.mult),
                   reads=x1r + [("dst5", i), "ln2r"], writes=["h2f"])
            sc.add("act", lambda e: e.copy(out=H2B[s], in_=H2F), reads=["h2f"], writes=[("h2b", s)])
            sc.add("sync", lambda e: e.dma_start(out=h2_in[i * 128:(i + 1) * 128, :], in_=H2B[s]),
                   reads=[("h2b", s)], writes=[("h2_in", i)], kind="dma")
            for q4 in range(4):
                def f(e, q4=q4):
                    r_ = None
                    for j in range(4):
                        kc = q4 * 4 + j
                        r_ = e.transpose(out=pb(3 + (q4 % 2))[:, j * 128:(j + 1) * 128], in_=H2F[:, kc * 128:(kc + 1) * 128],
                                         identity=ident_f[:])
                    return r_
                sc.add("pe", f, reads=["h2f", "ident_f"], writes=[("pb", 3 + (q4 % 2))])
                sc.add("act", lambda e, q4=q4: e.copy(out=H2T[:, q4 * 4:(q4 + 1) * 4, :],
                                                      in_=pb(3 + (q4 % 2)).rearrange("p (a b) -> p a b", a=4)),
                       reads=[("pb", 3 + (q4 % 2))], writes=[("h2t", q4)])

            def fl(e):
                r_ = None
                for kc in range(16):
                    r_ = e.matmul(pb(5)[:, 0:N_EXP], lhsT=H2T[:, kc, :], rhs=WR[:, kc, :], start=(kc == 0), stop=(kc == 15))
                return r_
            sc.add("pe", fl, reads=[("h2t", q4) for q4 in range(4)] + ["wr"], writes=[("pb", 5)])
            lg = logit[:, i * 16:(i + 1) * 16]
            af = affs[:, i * 16:(i + 1) * 16]
            sc.add("dve", lambda e: e.tensor_reduce(out=st[:, 6:7], in_=pb(5)[:, 0:N_EXP], axis=AX.X, op=ALU.max),
                   reads=[("pb", 5)], writes=[("dst6", i)])
            sc.add("dve", lambda e: e.tensor_scalar(out=lg, in0=pb(5)[:, 0:N_EXP], scalar1=st[:, 6:7], scalar2=None,
                                                    op0=ALU.subtract),
                   reads=[("pb", 5), ("dst6", i)], writes=[("lg", i)])
            sc.add("act", lambda e: e.activation(out=lg, in_=lg, func=AF.Exp, accum_out=st[:, 7:8]),
                   reads=[("lg", i)], writes=[("lge", i), ("dst7", i)])
            sc.add("dve", lambda e: e.reciprocal(out=st[:, 7:8], in_=st[:, 7:8]), reads=[("dst7", i)], writes=[("dst7r", i)])
            sc.add("dve", lambda e: e.tensor_scalar(out=af, in0=lg, scalar1=st[:, 7:8], scalar2=None, op0=ALU.mult),
                   reads=[("lge", i), ("dst7r", i)], writes=[("aff", i)])
            sc.add("sync", lambda e: e.dma_start(out=aff_in[i * 128:(i + 1) * 128, :], in_=af),
                   reads=[("aff", i)], writes=[("aff_in", i)], kind="dma")

        def d_ag(i):
            if i % 2 == 1:
                j_ = i // 2
                sc.add("pool", lambda e, j_=j_: e.collective_compute(
                    "AllGather", ALU.bypass, replica_groups=GROUPS,
                    ins=[h2_in[256 * j_:256 * (j_ + 1), :].opt()], outs=[h2_all[1024 * j_:1024 * (j_ + 1), :].opt()]),
                    reads=[("h2_in", 2 * j_), ("h2_in", 2 * j_ + 1)], writes=[("h2_all", j_)], kind="cc")

        d_front(0)
        for i in range(8):
            if i + 1 < 8:
                d_front(i + 1)
            d_proj(i)
            if i >= 1:
                d_back(i - 1)
                d_ag(i - 1)
        d_back(7)
        d_ag(7)
        sc.add("pool", lambda e: e.collective_compute("AllGather", ALU.bypass, replica_groups=GROUPS,
                                                      ins=[aff_in.opt()], outs=[aff_all.opt()]),
               reads=[("aff_in", i) for i in range(8)], writes=["aff_all"], kind="cc")
        if debug:
            sc.add("sync", lambda e: e.dma_start(out=dbg["aff"], in_=aff_all), reads=["aff_all"], writes=["dbg_aff"], kind="dma")
        sc.barrier(include_cc=False)
        if stop_after == "router":
            sc.add("sync", lambda e: e.dma_start(out=out[0:128, :], in_=X1[0]), writes=["outdummy"], kind="dma")
            sc.emit(block)
            return nc

        E0 = 0
        XSG = A.view(E0, [128, 4, D], BF16)
        XST = A.view(E0 + 16384, [128, 16, 512], BF16)
        HT = A.view(E0 + 32768, [128, NFC, 512], BF16)
        YT = [A.view(E0 + 55296 + i * 2048, [128, 512], F32) for i in range(4)]
        ST_ = [A.view(E0 + 63488 + i * 1024, [128, 512], BF16) for i in range(4)]
        SG = A.view(E0 + 67584, [128, 512], F32)
        SG2 = A.view(E0 + 69632, [128, 512], F32)
        NGU = 5
        WG = [A.view(E0 + 71680 + i * 8192, [128, 16, 128], BF16) for i in range(NGU)]
        WU = [A.view(E0 + 71680 + i * 8192 + 4096, [128, 16, 128], BF16) for i in range(NGU)]
        WDO = E0 + 71680 + NGU * 8192
        WD = [A.view(WDO + i * 22528, [128, NFC, 512], BF16) for i in range(2)]
        A16 = A.view(WDO + 45056, [128, NT, N_EXP], F32)
        SELT = A.view(WDO + 47104, [128, 4, N_EXP], F32)
        PRD = A.view(WDO + 47360, [128, NT, N_EXP], F32)
        A4 = A.view(WDO + 49408, [128, 4, NT], F32)
        CMP = A.view(WDO + 49920, [128, 4, NT], F32)
        AP3 = A.view(WDO + 50432, [128, 4, NT, 6], BF16)
        RES = A.view(WDO + 54400, [128, 4, NT], F32)
        UTRI = A.view(WDO + 52224, [128, 128], F32)
        LT32 = A.view(WDO + 52736, [128, NT], F32)
        MSK = A.view(WDO + 53376, [128, 4, NT], F32)
        POS = A.view(WDO + 53888, [128, 4, NT], F32)
        FCV = A.view(WDO + 54912, [128, NT], F32)
        ONESF = A.view(WDO + 55040, [128, 4], F32)
        CSB = A.view(WDO + 55296, [128, 4, 128], F32)
        assert WDO + 57344 <= ARENA_USE, WDO + 57344
        thr = sm(4)
        cand = sm(4)
        cntp = es.enter_context(nc.sbuf_tensor("cntp", [128, 4], BF16))
        ge = sm(4)
        idxf = sm(80)
        pselc = sm(128)
        gts = sm(16)
        idx_i = es.enter_context(nc.sbuf_tensor("idx_i", [128, 4, 20], I32))
        pidx = es.enter_context(nc.sbuf_tensor("pidx", [128, NT], F32))

        gu_n = [0]

        def load_gu(e_, fc):
            k = gu_n[0] % NGU
            gu_n[0] += 1
            gv = wg_e[e_].rearrange("(kc p) n -> p kc n", p=128)
            uv = wu_e[e_].rearrange("(kc p) n -> p kc n", p=128)
            sc.add("pool", lambda e: e.dma_start(out=WG[k], in_=gv[:, :, fc * 128:(fc + 1) * 128]),
                   writes=[("wg", k)], kind="dma")
            sc.add("pool", lambda e: e.dma_start(out=WU[k], in_=uv[:, :, fc * 128:(fc + 1) * 128]),
                   writes=[("wu", k)], kind="dma")
            return k

        wd_n = [0]

        def load_wd(e_, db):
            k = wd_n[0] % 2
            wd_n[0] += 1
            dv = wd_e[e_].rearrange("(fc p) n -> p fc n", p=128)
            for h_ in range(2):
                sc.add("pool", lambda e, h_=h_: e.dma_start(out=WD[k][:, h_ * 11:(h_ + 1) * 11, :],
                                                            in_=dv[:, h_ * 11:(h_ + 1) * 11, db * 512:(db + 1) * 512]),
                       writes=[("wd", k, h_)], kind="dma")
            return k

        sc.add("sync", lambda e: e.dma_start(out=A16, in_=aff_all.rearrange("(c p) j -> p c j", p=128)),
               reads=["aff_all"], writes=["a16"], kind="dma")
        sc.add("sync", lambda e: e.dma_start(out=SELT, in_=sel), writes=["selt"], kind="dma")
        for e_ in range(4):
            sc.add("dve", lambda e, e_=e_: e.tensor_tensor(out=PRD, in0=A16, in1=SELT[:, e_:e_ + 1, :].to_broadcast([128, NT, N_EXP]),
                                                           op=ALU.mult),
                   reads=["a16", "selt"], writes=["prd"])
            sc.add("dve", lambda e, e_=e_: e.tensor_reduce(out=A4[:, e_, :], in_=PRD, axis=AX.X, op=ALU.add),
                   reads=["prd"], writes=[("a4", e_)])
        a4r = [("a4", e_) for e_ in range(4)]
        sc.add("dve", lambda e: e.tensor_scalar(out=UTRI, in0=iota_f[:, 0:128], scalar1=iota_p[:, 0:1], scalar2=None,
                                                op0=ALU.is_gt), reads=["iota_f", "iota_p"], writes=["utri"])
        sc.add("dve", lambda e: e.tensor_scalar(out=LT32, in0=iota_f[:, 0:NT], scalar1=iota_p[:, 0:1], scalar2=None,
                                                op0=ALU.is_gt), reads=["iota_f", "iota_p"], writes=["lt32"])
        sc.add("dve", lambda e: e.tensor_copy(out=pidx[:], in_=iota_p[:, 0:1].to_broadcast([128, NT])),
               reads=["iota_p"], writes=["pidx"])
        sc.add("dve", lambda e: e.tensor_copy(out=AP3[:, :, :, 0], in_=iota_f[:, 0:NT].unsqueeze(1).to_broadcast([128, 4, NT])),
               reads=["iota_f"], writes=["ap3_0"])
        sc.add("sync", lambda e: e.dma_start(out=FCV, in_=fcv), writes=["fcv"], kind="dma")
        sc.add("pool", lambda e: e.memset(ONESF, 1.0), writes=["ones_f"])
        sc.add("dve", lambda e: e.tensor_copy(out=AP3[:, :, :, 1], in_=FCV.unsqueeze(1).to_broadcast([128, 4, NT])),
               reads=["fcv"], writes=["ap3_1"])
        sc.add("dve", lambda e: e.tensor_copy(out=AP3[:, :, :, 2], in_=pidx[:].unsqueeze(1).to_broadcast([128, 4, NT])),
               reads=["pidx"], writes=["ap3_2"])
        sc.add("dve", lambda e: e.tensor_copy(out=AP3[:, :, :, 3], in_=A4), reads=a4r, writes=["ap3_3"])
        sc.add("dve", lambda e: e.tensor_tensor(out=RES, in0=A4, in1=AP3[:, :, :, 3], op=ALU.subtract),
               reads=a4r + ["ap3_3"], writes=["res1"])
        sc.add("dve", lambda e: e.tensor_copy(out=AP3[:, :, :, 4], in_=RES), reads=["res1"], writes=["ap3_4"])
        sc.add("dve", lambda e: e.tensor_tensor(out=RES, in0=RES, in1=AP3[:, :, :, 4], op=ALU.subtract),
               reads=["res1", "ap3_4"], writes=["res2"])
        sc.add("dve", lambda e: e.tensor_copy(out=AP3[:, :, :, 5], in_=RES), reads=["res2"], writes=["ap3_5"])
        ap3r = ["ap3_0", "ap3_1", "ap3_2", "ap3_3", "ap3_4", "ap3_5"]

        sc.add("dve", lambda e: e.memset(thr, 0.0), writes=["thr"])
        for it in range(1, BISECT_ITERS + 1):
            step = 2.0 ** (-it)
            sc.add("dve", lambda e, step=step: e.tensor_scalar(out=cand, in0=thr, scalar1=step, scalar2=None, op0=ALU.add),
                   reads=["thr"], writes=["cand"])
            sc.add("dve", lambda e: e.tensor_tensor(out=CMP, in0=A4, in1=bc(cand, [128, 4, NT]), op=ALU.is_gt),
                   reads=a4r + ["cand"], writes=["cmp"])
            def fcnt(e):
                with nc.allow_low_precision(reason="per-partition counts <= 32 are exact in bf16"):
                    return e.tensor_reduce(out=cntp[:], in_=CMP, axis=AX.X, op=ALU.add)
            sc.add("dve", fcnt, reads=["cmp"], writes=["cntp"])
            sc.add("pe", lambda e: e.matmul(pb(7)[:, 0:4], lhsT=ones_b, rhs=cntp[:], start=True, stop=True),
                   reads=["cntp", "ones_b"], writes=[("pb", 7)])
            sc.add("dve", lambda e: e.tensor_scalar(out=ge, in0=pb(7)[:, 0:4], scalar1=float(CAP) - 0.5, scalar2=None,
                                                    op0=ALU.is_gt), reads=[("pb", 7)], writes=["ge"])
            sc.add("dve", lambda e, step=step: e.scalar_tensor_tensor(out=thr, in0=ge, scalar=step, in1=thr,
                                                                      op0=ALU.mult, op1=ALU.add),
                   reads=["ge", "thr"], writes=["thr"])
        sc.add("dve", lambda e: e.tensor_tensor(out=MSK, in0=A4, in1=bc(thr, [128, 4, NT]), op=ALU.is_gt),
               reads=a4r + ["thr"], writes=["msk"])

        for e_ in range(4):
            sc.add("pe", lambda e, e_=e_: e.matmul(pb(6)[0:NT, e_:e_ + 1], lhsT=MSK[:, e_, :], rhs=ONESF[:, 0:1],
                                                   start=True, stop=True),
                   reads=["msk", "ones_f"], writes=[("pb", 6)])
        for e_ in range(4):
            sc.add("dve", lambda e, e_=e_: e.tensor_copy(out=CSB[0:NT, e_, :], in_=pb(6)[0:NT, e_:e_ + 1].to_broadcast([NT, 128])),
                   reads=[("pb", 6)], writes=[("csb", e_)])
        for e_ in range(4):
            def fpos(e, e_=e_):
                e.matmul(pb(7)[:, e_ * NT:(e_ + 1) * NT], lhsT=UTRI, rhs=MSK[:, e_, :], start=True, stop=False)
                return e.matmul(pb(7)[:, e_ * NT:(e_ + 1) * NT], lhsT=CSB[0:NT, e_, :], rhs=LT32[0:NT, :], start=False, stop=True)
            sc.add("pe", fpos, reads=["msk", "utri", "lt32", ("csb", e_)], writes=[("pb", 7)])
        sc.add("dve", lambda e: e.scalar_tensor_tensor(out=POS, in0=pb(7)[:, 0:4 * NT].rearrange("p (a b) -> p a b", a=4),
                                                       scalar=1.0, in1=MSK, op0=ALU.add, op1=ALU.mult),
               reads=[("pb", 7), "msk"], writes=["pos0"])
        sc.add("dve", lambda e: e.tensor_scalar(out=POS, in0=POS, scalar1=-1.0, scalar2=None, op0=ALU.add),
               reads=["pos0"], writes=["pos"])

        gu_list = [(e_, fc) for e_ in range(4) for fc in range(NFC)]
        wd_list = [(e_, db) for e_ in range(4) for db in range(4)]
        gu_loaded, wd_loaded = [], []

        def gu_prefetch(upto):
            while len(gu_loaded) < min(upto, len(gu_list)):
                gu_loaded.append(load_gu(*gu_list[len(gu_loaded)]))

        def wd_prefetch(upto):
            while len(wd_loaded) < min(upto, len(wd_list)):
                wd_loaded.append(load_wd(*wd_list[len(wd_loaded)]))

        gu_prefetch(NGU - 1)
        wd_prefetch(1)
        prev_scatter = []
        h2r = [("h2_all", j_) for j_ in range(4)]
        def expert(e_, prev_scatter):
            for c in range(NT):
                stile = ST_[c % 4]
                sc.add("dve", lambda e, c=c, stile=stile: e.tensor_scalar(out=stile, in0=iota_f[:], scalar1=POS[:, e_, c:c + 1],
                                                                          scalar2=None, op0=ALU.is_equal),
                       reads=["pos", "iota_f"], writes=[("stile", c % 4)])

                def fsel(e, c=c, stile=stile):
                    r_ = None
                    if c == 0:
                        e.matmul(pb(6)[:, 0:32], lhsT=ident_b[:], rhs=zero_b[:], start=True, stop=False)
                    for sg in range(4):
                        r_ = e.matmul(pb(6)[:, sg * 8:sg * 8 + 6], lhsT=stile[:, sg * 128:(sg + 1) * 128],
                                      rhs=AP3[:, e_, c, :], start=False, stop=(c == NT - 1))
                    return r_
                sc.add("pe", fsel, reads=[("stile", c % 4), "zero_b", "ident_b"] + ap3r, writes=[("pb", 6)])
            ik = ("idx", e_)
            psc = pselc[:, e_ * 32:(e_ + 1) * 32]
            sc.add("dve", lambda e, psc=psc: e.tensor_copy(out=psc, in_=pb(6)[:, 0:32]), reads=[("pb", 6)], writes=[("psc", e_)])
            psv = psc.rearrange("p (a b) -> p a b", a=4)
            fb = e_ * 20
            sc.add("dve", lambda e, psv=psv, fb=fb: e.scalar_tensor_tensor(out=idxf[:, fb:fb + 4], in0=psv[:, :, 0], scalar=128.0,
                                                                           in1=psv[:, :, 2], op0=ALU.mult, op1=ALU.add),
                   reads=[("psc", e_)], writes=[ik + (0,)])
            for db in range(1, 4):
                sc.add("dve", lambda e, fb=fb, db=db: e.tensor_scalar(out=idxf[:, fb + 4 * db:fb + 4 * db + 4], in0=idxf[:, fb:fb + 4],
                                                                      scalar1=float(S * db), scalar2=None, op0=ALU.add),
                       reads=[ik + (0,)], writes=[ik + (0, db)])
            sc.add("dve", lambda e, psv=psv, fb=fb: e.scalar_tensor_tensor(out=idxf[:, fb + 16:fb + 20], in0=psv[:, :, 1], scalar=128.0,
                                                                           in1=psv[:, :, 2], op0=ALU.mult, op1=ALU.add),
                   reads=[("psc", e_)], writes=[ik + (1,)])
            sc.add("dve", lambda e, fb=fb: e.tensor_copy(out=idx_i[:, e_, :], in_=idxf[:, fb:fb + 20]),
                   reads=[ik + (0,), ik + (1,)] + [ik + (0, db) for db in range(1, 4)], writes=[ik])
            sc.add("dve", lambda e, psv=psv: e.tensor_reduce(out=gts[:, e_ * 4:(e_ + 1) * 4], in_=psv[:, :, 3:6], axis=AX.X, op=ALU.add),
                   reads=[("psc", e_)], writes=[("gate", e_)])
            if debug:
                sc.add("sync", lambda e: e.dma_start(out=dbg["idx"][:, e_ * 4:(e_ + 1) * 4], in_=idx_i[:, e_, 0:4]),
                       reads=[ik], writes=[("dbgidx", e_)], kind="dma")
                sc.add("sync", lambda e: e.dma_start(out=dbg["gate"][:, e_ * 4:(e_ + 1) * 4], in_=gts[:, e_ * 4:(e_ + 1) * 4]),
                       reads=[("gate", e_)], writes=[("dbggate", e_)], kind="dma")
            for sg in range(4):
                sc.add("pool", lambda e, sg=sg: e.indirect_dma_start(
                    out=XSG[:, sg, :], out_offset=None, in_=h2_all,
                    in_offset=bass.IndirectOffsetOnAxis(ap=idx_i[:, e_, 16 + sg:17 + sg], axis=0)),
                    reads=h2r + [ik], writes=[("xsg", sg)], kind="dma")
            for sg in range(4):
                for half in range(2):
                    bk = (sg * 2 + half) % 2

                    def ftr(e, sg=sg, half=half, bk=bk):
                        r_ = None
                        for j in range(8):
                            kc = half * 8 + j
                            r_ = e.transpose(out=pbb(bk)[:, j * 128:(j + 1) * 128], in_=XSG[:, sg, kc * 128:(kc + 1) * 128],
                                             identity=ident_b[:])
                        return r_
                    sc.add("pe", ftr, reads=[("xsg", sg), "ident_b"], writes=[("pb", bk)])
                    ce = "act" if half == 0 else "dve"
                    if ce == "act":
                        sc.add("act", lambda e, sg=sg, half=half, bk=bk: e.copy(
                            out=XST[:, half * 8:(half + 1) * 8, sg * 128:(sg + 1) * 128],
                            in_=pbb(bk).rearrange("p (a b) -> p a b", a=8)),
                            reads=[("pb", bk)], writes=[("xst", sg, half)])
                    else:
                        sc.add("dve", lambda e, sg=sg, half=half, bk=bk: e.tensor_copy(
                            out=XST[:, half * 8:(half + 1) * 8, sg * 128:(sg + 1) * 128],
                            in_=pbb(bk).rearrange("p (a b) -> p a b", a=8)),
                            reads=[("pb", bk)], writes=[("xst", sg, half)])
            xstr = [("xst", sg, half) for sg in range(4) for half in range(2)]
            for fc in range(NFC):
                n_ = e_ * NFC + fc
                gu_prefetch(n_ + NGU)
                k = gu_loaded[n_]
                pg, pu = 2 + 2 * (fc % 2), 3 + 2 * (fc % 2)

                def fgu(e, k=k, pg=pg, pu=pu):
                    r_ = None
                    for kc in range(16):
                        r_ = e.matmul(pb(pg), lhsT=WG[k][:, kc, :], rhs=XST[:, kc, :], start=(kc == 0), stop=(kc == 15))
                    for kc in range(16):
                        r_ = e.matmul(pb(pu), lhsT=WU[k][:, kc, :], rhs=XST[:, kc, :], start=(kc == 0), stop=(kc == 15))
                    return r_
                sc.add("pe", fgu, reads=xstr + [("wg", k), ("wu", k)], writes=[("pb", pg), ("pb", pu)])
                sgt = SG if fc % 2 == 0 else SG2
                sc.add("act", lambda e, pg=pg, sgt=sgt: e.activation(out=sgt, in_=pb(pg), func=AF.Silu),
                       reads=[("pb", pg)], writes=[("sg", fc % 2)])
                sc.add("dve", lambda e, pu=pu, sgt=sgt, fc=fc: e.tensor_tensor(out=HT[:, fc, :], in0=sgt, in1=pb(pu), op=ALU.mult),
                       reads=[("sg", fc % 2), ("pb", pu)], writes=[("ht", fc)])
            htr = [("ht", fc) for fc in range(NFC)]
            scat = []
            for db in range(4):
                nd = e_ * 4 + db
                wd_prefetch(nd + 2)
                kd = wd_loaded[nd]
                for sg in range(4):
                    m_ = db * 4 + sg
                    bk = m_ % 2

                    def fdn(e, sg=sg, kd=kd, bk=bk):
                        r_ = None
                        for fc in range(NFC):
                            r_ = e.matmul(pb(bk), lhsT=HT[:, fc, sg * 128:(sg + 1) * 128], rhs=WD[kd][:, fc, :],
                                          start=(fc == 0), stop=(fc == NFC - 1))
                        return r_
                    sc.add("pe", fdn, reads=htr + [("wd", kd, 0), ("wd", kd, 1)], writes=[("pb", bk)])
                    yt = YT[m_ % 4]
                    if m_ % 2 == 0:
                        sc.add("act", lambda e, bk=bk, yt=yt, sg=sg: e.activation(out=yt, in_=pb(bk), func=AF.Copy,
                                                                                  scale=gts[:, e_ * 4 + sg:e_ * 4 + sg + 1]),
                               reads=[("pb", bk), ("gate", e_)], writes=[("yt", m_ % 4)])
                    else:
                        sc.add("dve", lambda e, bk=bk, yt=yt, sg=sg: e.tensor_scalar(out=yt, in0=pb(bk),
                                                                                     scalar1=gts[:, e_ * 4 + sg:e_ * 4 + sg + 1],
                                                                                     scalar2=None, op0=ALU.mult),
                               reads=[("pb", bk), ("gate", e_)], writes=[("yt", m_ % 4)])
                    scat.append(sc.add("pool", lambda e, db=db, sg=sg, yt=yt: e.indirect_dma_start(
                        out=part,
                        out_offset=bass.IndirectOffsetOnAxis(ap=idx_i[:, e_, db * 4 + sg:db * 4 + sg + 1], axis=0),
                        in_=yt, in_offset=None, compute_op=ALU.add),
                        reads=[("yt", m_ % 4), ik] + partz_all + [("part_x1", i, db_) for i in range(8) for db_ in range(4)],
                        writes=[("part_sc", e_, db, sg)], kind="dma", extra=prev_scatter))
            return scat

        for e_ in range(4):
            prev_scatter = expert(e_, prev_scatter)
        all_sc = [("part_sc", e_, db, sg) for e_ in range(4) for db in range(4) for sg in range(4)]
        sc.add("pool", lambda e: e.collective_compute("ReduceScatter", ALU.add, replica_groups=GROUPS,
                                                      ins=[part.opt()], outs=[rs_out.opt()]),
               reads=all_sc + partz_all + [("part_x1", i, db_) for i in range(8) for db_ in range(4)], writes=["rs_out"], kind="cc")
        for i in range(8):
            sc.add("sync" if i % 2 == 0 else "pool",
                   lambda e, i=i: e.dma_start(out=out[i * 512:(i + 1) * 512, :], in_=rs_out[i * 512:(i + 1) * 512, :]),
                   reads=["rs_out"], writes=[("out", i)], kind="dma")
        sc.emit(block)
    return nc


def _na_bias_tables(rpb_h):
    ki = np.arange(128)[:, None]
    qi = np.arange(128)[None, :]

    def pat(m, kt):
        qr = 2 * m + qi // GRID_W
        qc = qi % GRID_W
        kr = 2 * kt + ki // GRID_W
        kc = ki % GRID_W
        rs = np.clip(qr - 4, 0, 56)
        cs = np.clip(qc - 8, 0, GRID_W - 16)
        valid = (kr >= rs) & (kr < rs + 8) & (kc >= cs) & (kc < cs + 16)
        ro = np.clip(kr - qr + 7, 0, 14)
        co = np.clip(kc - qc, -15, 15) + 15
        return np.where(valid, rpb_h[ro, co], np.float32(NEG)).astype(np.float32)

    full_mask = np.full((128, 128), NEG, np.float32)
    pats = [pat(10, 10 + d) for d in (-2, -1, 0, 1, 2)]
    for m in (0, 1):
        pats += [pat(m, kt) for kt in (0, 1, 2, 3)] + [full_mask]
    for m in (30, 31):
        pats += [pat(m, kt) for kt in (28, 29, 30, 31)] + [full_mask]
    return np.ascontiguousarray(np.stack(pats, axis=1))


def _alibi_tables(g):
    slope = np.float32(2.0 ** (-8.0 * (g + 1) / 4))
    ki = np.arange(128, dtype=np.float32)[:, None]
    qi = np.arange(256, dtype=np.float32)[None, :]
    t = np.stack([-slope * (qi - ki), slope * (qi - ki), -slope * np.abs(qi - ki), -slope * np.abs(qi - ki - 128.0)],
                 axis=1).astype(np.float32)
    cb = np.zeros((128, 64), np.float32)
    for delta in range(-31, 31):
        if delta >= 1:
            cb[:, delta + 32] = -slope * 128.0 * delta
        elif delta <= -2:
            cb[:, delta + 32] = slope * 128.0 * delta
    return np.ascontiguousarray(t), cb


def _prep_inputs(inp):
    f = lambda a: np.ascontiguousarray(np.asarray(a, dtype=np.float32))
    x = f(inp["x"])
    w_in = f(inp["w_in"])[0]
    w_out = f(inp["w_out"])[0]
    on_a = f(inp["on_a"])[0]
    subln = f(inp["subln_b"])[0]
    rpb = f(inp["rpb_a"])[0]
    wg, wu, wd = np.asarray(inp["w_gate"])[0], np.asarray(inp["w_up"])[0], np.asarray(inp["w_down"])[0]
    ln1T = np.ascontiguousarray(f(inp["ln1_g"])[0].reshape(16, 128).T)
    qkg = np.ascontiguousarray(np.stack([f(inp["qn_a"])[0]] * 2 + [f(inp["kn_a"])[0]] * 2 + [f(inp["qn_b"])[0]] * 2
                                        + [f(inp["kn_b"])[0]] * 2, axis=1))
    lamv = np.ascontiguousarray(np.stack([f(inp["lam_q1"])[0], f(inp["lam_k1"])[0], f(inp["lam_q2"])[0], f(inp["lam_k2"])[0]]))
    ln2 = f(inp["ln2_g"])
    w_router = f(inp["w_router"])[0]
    maps = []
    p = np.arange(128)
    cc_ = np.arange(NT)
    fcv = np.ascontiguousarray(np.broadcast_to((8 * ((cc_ % 8) // 2) + 2 * (cc_ // 8) + (cc_ % 2)).astype(np.float32), (128, NT)))
    for c in range(8):
        b, g = c // 4, c % 4
        cols = np.concatenate([
            np.arange(256 * g, 256 * g + 256), 1024 + np.arange(256 * g, 256 * g + 256),
            3072 + np.arange(128 * g, 128 * g + 128), 3584 + np.arange(128 * g, 128 * g + 128),
            4096 + np.arange(128 * g, 128 * g + 128), 4608 + np.arange(128 * g, 128 * g + 128),
            2048 + np.arange(256 * g, 256 * g + 256), 5120 + np.arange(256 * g, 256 * g + 256)])
        rows, wog_cols = [], []
        for r in range(4):
            rows += [np.arange(256 * r, 256 * r + 256), 1024 + np.arange(256 * r, 256 * r + 256)]
            wog_cols += [on_a[256 * r:256 * r + 128], on_a[256 * r + 128:256 * r + 256], subln[0:128], subln[128:256]]
        rows = np.concatenate(rows)
        alib, cb = _alibi_tables(g)
        sel = np.zeros((128, 4, N_EXP), np.float32)
        for e_ in range(4):
            sel[:, e_, 4 * g + e_] = 1.0
        own = (1024 * g + np.arange(8)[None, :] * 128 + p[:, None]).astype(np.int32)
        mixi = (4096 * g + np.arange(4)[None, None, :] * 1024 + (np.arange(8)[None, :] * 128 + p[:, None])[:, :, None]).astype(np.int32)
        maps.append({
            "x_b": x[b], "x_own": np.ascontiguousarray(x[b, 1024 * g:1024 * (g + 1)]),
            "w_in_g": np.ascontiguousarray(w_in[:, cols]), "ln1T": ln1T, "qkg": qkg,
            "nab": np.ascontiguousarray(np.stack([_na_bias_tables(rpb[2 * g]), _na_bias_tables(rpb[2 * g + 1])])),
            "alib": alib, "cbias": cb, "lamv": lamv,
            "w_out_p": np.ascontiguousarray(w_out[rows]), "wog": np.ascontiguousarray(np.stack(wog_cols, axis=1)),
            "ln2": ln2, "w_router": w_router, "sel": sel, "own_tok": np.ascontiguousarray((own[:, :, None] + S * np.arange(4)[None, None, :]).astype(np.int32)),
            "mix_idx": np.ascontiguousarray(mixi), "fcv": fcv,
            "wg_e": np.ascontiguousarray(wg[4 * g:4 * g + 4], dtype=np.float32),
            "wu_e": np.ascontiguousarray(wu[4 * g:4 * g + 4], dtype=np.float32),
            "wd_e": np.ascontiguousarray(wd[4 * g:4 * g + 4], dtype=np.float32),
        })
    return maps


def kernel(**inputs):
    maps = _prep_inputs(inputs)
    nc = build_nc()
    res = run_bass_kernel_spmd(nc, maps, core_ids=list(range(8)))
    out = np.empty((2, S, D), np.float32)
    for c in range(8):
        b, g = c // 4, c % 4
        out[b, :, 512 * g:512 * (g + 1)] = np.asarray(res.results[c]["out"], dtype=np.float32)
    return out
```

```python
import numpy as np
from contextlib import ExitStack
import concourse.bass as bass
import concourse.mybir as mybir
from concourse.bass_utils import run_bass_kernel_spmd

F32 = mybir.dt.float32
BF16 = mybir.dt.bfloat16
I32 = mybir.dt.int32
U8 = mybir.dt.uint8
AF = mybir.ActivationFunctionType
ALU = mybir.AluOpType
AX = mybir.AxisListType

D = 2048
S = 4096
NT = 32
GRID_W = 64
EPS = 1e-6
N_EXP = 16
CAP = 512
FF = 2816
NFC = FF // 128
GROUPS = [[0, 1, 2, 3], [4, 5, 6, 7]]
SCALE = 128.0 ** -0.5
LAM_INIT = 0.8 - 0.6
NEG = -30000.0
NPAT = 25
BISECT_ITERS = 24


class Sched:
    COMPUTE = ("act", "dve", "pool", "pe")

    def __init__(self, nc, es, rings):
        self.nc = nc
        self.ops = []
        self.lastw = {}
        self.readers = {}
        self.sems = []
        self.csem = {}
        for e in self.COMPUTE:
            self.csem[e] = self._sem(es, "c_" + e)
        self.rings = {e: [self._sem(es, f"d_{e}_{i}") for i in range(k)] for e, k in rings.items()}
        self.dma_count = {e: 0 for e in rings}
        self.es = es
        self.barrier_deps = []
        self.recent = {e: None for e in self.COMPUTE}
        self.recent_dma = {e: [] for e in rings}
        self.cc_ops = []
        self.cc_pool = [self._sem(es, f"cc_{i}") for i in range(12)]

    def _sem(self, es, name):
        self.sems.append(es.enter_context(self.nc.semaphore(name)))
        return len(self.sems) - 1

    def add(self, eng, fn, reads=(), writes=(), kind="c", extra=()):
        oid = len(self.ops)
        deps = {}
        for r in reads:
            w = self.lastw.get(r)
            if w is not None:
                deps.setdefault(w, set()).add("raw")
        for w_ in writes:
            w = self.lastw.get(w_)
            if w is not None:
                deps.setdefault(w, set()).add("waw")
            for rd in self.readers.get(w_, ()):
                deps.setdefault(rd, set()).add("war")
        for d in self.barrier_deps:
            deps.setdefault(d, set()).add("raw")
        for d in extra:
            deps.setdefault(d, set()).add("raw")
        fdeps = []
        for d, types in deps.items():
            p = self.ops[d]
            if p["kind"] == "c" and p["eng"] == eng and kind == "c":
                if eng == "pe" or "raw" not in types:
                    continue
            fdeps.append(d)
        op = dict(id=oid, eng=eng, fn=fn, kind=kind, deps=fdeps, has_dep=False, sem=None, val=0, prev=0)
        if kind == "dma":
            j = self.dma_count[eng]
            self.dma_count[eng] += 1
            K = len(self.rings[eng])
            op["sem"] = self.rings[eng][j % K]
            op["val"] = 16 * (j // K + 1)
            op["prev"] = 16 * (j // K)
            self.recent_dma[eng].append(oid)
            self.recent_dma[eng] = self.recent_dma[eng][-K:]
        elif kind == "cc":
            op["sem"] = self.cc_pool[len(self.cc_ops)]
            op["val"] = 1
            self.cc_ops.append(oid)
        else:
            self.recent[eng] = oid
        for r in reads:
            self.readers.setdefault(r, []).append(oid)
        for w_ in writes:
            self.lastw[w_] = oid
            self.readers[w_] = []
        self.ops.append(op)
        return oid

    def barrier(self, include_cc=True):
        deps = [v for v in self.recent.values() if v is not None]
        for lst in self.recent_dma.values():
            deps += lst
        if include_cc:
            deps += self.cc_ops
        self.barrier_deps = deps

    def emit(self, block):
        ops = self.ops
        for op in ops:
            for d in op["deps"]:
                ops[d]["has_dep"] = True
        cnt = {e: 0 for e in self.COMPUTE}
        for op in ops:
            if op["kind"] == "c" and op["has_dep"]:
                cnt[op["eng"]] += 1
                op["sem"] = self.csem[op["eng"]]
                op["val"] = cnt[op["eng"]]
        for e, c in cnt.items():
            assert c < 60000, (e, c)
        lists = {}
        for op in ops:
            lists.setdefault(op["eng"], []).append(op)
        final = {}
        for op in ops:
            if op["sem"] is not None:
                final[op["sem"]] = max(final.get(op["sem"], 0), op["val"])
        sems = self.sems

        def run(name, eng):
            waited = {}
            for op in lists.get(name, []):
                waits = {}
                for d in op["deps"]:
                    p = ops[d]
                    waits[p["sem"]] = max(waits.get(p["sem"], 0), p["val"])
                if op["kind"] == "dma" and op["prev"] > 0:
                    waits[op["sem"]] = max(waits.get(op["sem"], 0), op["prev"])
                for s_, v in waits.items():
                    if waited.get(s_, 0) < v:
                        eng.wait_ge(sems[s_], v)
                        waited[s_] = v
                ins = op["fn"](eng)
                if op["kind"] == "dma":
                    ins.then_inc(sems[op["sem"]], 16)
                elif op["kind"] == "cc":
                    ins.then_inc(sems[op["sem"]], 1)
                elif op["has_dep"]:
                    ins.then_inc(sems[op["sem"]], 1)
            if name == "sync":
                for s_, v in final.items():
                    if waited.get(s_, 0) < v:
                        eng.wait_ge(sems[s_], v)

        @block.sync
        def _(e):
            run("sync", e)

        @block.scalar
        def _(e):
            run("act", e)

        @block.vector
        def _(e):
            run("dve", e)

        @block.gpsimd
        def _(e):
            run("pool", e)

        @block.tensor
        def _(e):
            run("pe", e)


class Arena:
    def __init__(self, ar, size):
        self.ar = ar
        self.size = size

    def view(self, off, shape, dt):
        esz = {F32: 4, BF16: 2, I32: 4}[dt]
        n = int(np.prod(shape[1:]))
        assert off % 4 == 0 and off + n * esz <= self.size, (off, shape, self.size)
        v = self.ar[:, off:off + n * esz].bitcast(dt)
        if len(shape) == 3:
            v = v.rearrange("p (a b) -> p a b", a=shape[1])
        elif len(shape) == 4:
            v = v.rearrange("p (a b c) -> p a b c", a=shape[1], b=shape[2])
        return v


def bc(ap, shape):
    return ap.unsqueeze(len(ap.shape)).to_broadcast(list(shape))


def build_nc(debug=False, stop_after=None):
    nc = bass.Bass("TRN2", target_bir_lowering=False)

    def din(name, shape, dt=F32):
        return nc.dram_tensor(name, list(shape), dt, kind="ExternalInput").ap()

    x_b = din("x_b", [S, D])
    x_own = din("x_own", [1024, D])
    w_in_g = din("w_in_g", [D, 1536])
    ln1T = din("ln1T", [128, 16])
    qkg = din("qkg", [128, 8])
    nab = din("nab", [2, 128, NPAT, 128])
    alib = din("alib", [128, 4, 256])
    cbias = din("cbias", [128, 64])
    lamv = din("lamv", [4, 128])
    w_out_p = din("w_out_p", [D, D])
    wog = din("wog", [128, 16])
    ln2 = din("ln2", [1, D])
    w_router = din("w_router", [D, N_EXP])
    sel = din("sel", [128, 4, N_EXP])
    fcv = din("fcv", [128, NT])
    own_tok = din("own_tok", [128, 8, 4], I32)
    mix_idx = din("mix_idx", [128, 8, 4], I32)
    if stop_after is None:
        wg_e = din("wg_e", [4, D, FF])
        wu_e = din("wu_e", [4, D, FF])
        wd_e = din("wd_e", [4, FF, D])
    out = nc.dram_tensor("out", [S, 512], F32, kind="ExternalOutput").ap()

    ag1_in = nc.dram_tensor("ag1_in", [S, 512], BF16).ap()
    ag1_out = nc.dram_tensor("ag1_out", [4 * S, 512], BF16).ap()
    h2_in = nc.dram_tensor("h2_in", [1024, D], BF16).ap()
    h2_all = nc.dram_tensor("h2_all", [S, D], BF16).ap()
    aff_in = nc.dram_tensor("aff_in", [1024, N_EXP], F32).ap()
    aff_all = nc.dram_tensor("aff_all", [S, N_EXP], F32).ap()
    part = nc.dram_tensor("part", [4 * S, 512], F32).ap()
    rs_out = nc.dram_tensor("rs_out", [S, 512], F32).ap()
    dbg = {}
    if debug:
        dbg["mix"] = nc.dram_tensor("dbg_mix", [S, 512], BF16, kind="ExternalOutput").ap()
        dbg["x1"] = nc.dram_tensor("dbg_x1", [1024, D], F32, kind="ExternalOutput").ap()
        dbg["aff"] = nc.dram_tensor("dbg_aff", [S, N_EXP], F32, kind="ExternalOutput").ap()
        dbg["idx"] = nc.dram_tensor("dbg_idx", [128, 16], I32, kind="ExternalOutput").ap()
        dbg["gate"] = nc.dram_tensor("dbg_gate", [128, 16], F32, kind="ExternalOutput").ap()

    ARENA = 196 * 1024
    with ExitStack() as es:
        ar_t = es.enter_context(nc.sbuf_tensor("arena", [128, ARENA], U8))
        A = Arena(ar_t, ARENA)
        small = es.enter_context(nc.sbuf_tensor("small", [128, 1024], F32))
        ident_b = es.enter_context(nc.sbuf_tensor("ident_b", [128, 128], BF16))
        ident_f = es.enter_context(nc.sbuf_tensor("ident_f", [128, 128], F32))
        iota_f = es.enter_context(nc.sbuf_tensor("iota_f", [128, 512], F32))
        iota_p = es.enter_context(nc.sbuf_tensor("iota_p", [128, 1], F32))
        zero_b = es.enter_context(nc.sbuf_tensor("zero_b", [128, 32], BF16))
        pbanks = [es.enter_context(nc.psum_tensor(f"pb{i}", [128, 512], F32)) for i in range(8)]
        sc = Sched(nc, es, rings={"sync": 16, "pool": 24})
        block = es.enter_context(nc.Block())

        so = [0]

        def sm(n):
            v = small[:, so[0]:so[0] + n]
            so[0] += n
            assert so[0] <= 1024
            return v

        eps_c = sm(1)
        ones_b = A.view(ARENA - 256, [128, 128], BF16)
        ARENA_USE = ARENA - 256

        def pb(i):
            return pbanks[i][:]

        def pbb(i):
            return pbanks[i][:].bitcast(BF16)

        sc.add("pool", lambda e: e.memset(eps_c, EPS), writes=["eps"])
        sc.add("pool", lambda e: e.iota(iota_f[:], pattern=[[1, 512]], base=0, channel_multiplier=0,
                                        allow_small_or_imprecise_dtypes=True), writes=["iota_f"])
        sc.add("pool", lambda e: e.iota(iota_p[:], pattern=[[0, 1]], base=0, channel_multiplier=1,
                                        allow_small_or_imprecise_dtypes=True), writes=["iota_p"])
        sc.add("dve", lambda e: e.tensor_scalar(out=ident_f[:], in0=iota_f[:, 0:128], scalar1=iota_p[:, 0:1],
                                                scalar2=None, op0=ALU.is_equal),
               reads=["iota_f", "iota_p"], writes=["ident_f"])
        sc.add("dve", lambda e: e.tensor_copy(out=ident_b[:], in_=ident_f[:]), reads=["ident_f"], writes=["ident_b"])
        sc.add("pool", lambda e: e.memset(ones_b, 1.0), writes=["ones_b"])
        sc.add("pool", lambda e: e.memset(zero_b[:], 0.0), writes=["zero_b"])

        QKT = A.view(0, [128, 8, S], BF16)
        VA = A.view(65536, [128, NT, 2, 130], BF16)
        VB = A.view(82176, [128, NT, 258], BF16)
        R1 = 98688
        WP = A.view(R1, [128, 16, 1536], BF16)
        XIN = [A.view(R1 + 49152 + i * 8192, [128, D], F32) for i in range(2)]
        R2 = R1 + 65536
        XS = [A.view(R2 + i * 4096, [128, D], BF16) for i in range(2)]
        XT = [A.view(R2 + 8192 + i * 4096, [128, 16, 128], BF16) for i in range(2)]
        SQ = A.view(R2 + 16384, [128, 8, 128], F32)
        QKB = [A.view(R2 + 20480 + i * 2048, [128, 8, 128], BF16) for i in range(2)]
        JUNK = A.view(R2 + 24576, [128, D], BF16)
        assert R2 + 28672 <= ARENA_USE

        ln1T_s = sm(16)
        qkg_s = sm(8)
        ssx = sm(NT)
        rsx = sm(NT)
        ssqk = sm(8 * 2)
        rsqk = sm(8 * 2)

        sc.add("sync", lambda e: e.dma_start(out=ln1T_s, in_=ln1T), writes=["ln1T"], kind="dma")
        sc.add("sync", lambda e: e.dma_start(out=qkg_s, in_=qkg), writes=["qkg"], kind="dma")
        sc.add("pool", lambda e: e.memset(VA[:, :, :, 128:130], 1.0), writes=["VAones"])
        sc.add("pool", lambda e: e.memset(VB[:, :, 256:258], 1.0), writes=["VBones"])
        w_in_v = w_in_g.rearrange("(kc p) n -> p kc n", p=128)
        for kc in range(16):
            sc.add("pool", lambda e, kc=kc: e.dma_start(out=WP[:, kc, :], in_=w_in_v[:, kc, :]),
                   writes=[("WPraw", kc)], kind="dma")
        for kc in range(16):
            if kc % 2 == 0:
                sc.add("dve", lambda e, kc=kc: e.tensor_scalar(out=WP[:, kc, :], in0=WP[:, kc, :],
                                                               scalar1=ln1T_s[:, kc:kc + 1], scalar2=None, op0=ALU.mult),
                       reads=[("WPraw", kc), "ln1T"], writes=[("WP", kc)])
            else:
                sc.add("act", lambda e, kc=kc: e.activation(out=WP[:, kc, :], in_=WP[:, kc, :], func=AF.Copy,
                                                            scale=ln1T_s[:, kc:kc + 1]),
                       reads=[("WPraw", kc), "ln1T"], writes=[("WP", kc)])

        def a_load(tt):
            sc.add("sync", lambda e: e.dma_start(out=XIN[tt % 2], in_=x_b[tt * 128:(tt + 1) * 128, :]),
                   writes=[("xin", tt % 2)], kind="dma")

        def a_norm(tt):
            s = tt % 2
            sc.add("act", lambda e: e.activation(out=JUNK, in_=XIN[s], func=AF.Square, accum_out=ssx[:, tt:tt + 1]),
                   reads=[("xin", s)], writes=["junk", ("ssx", tt)])
            sc.add("act", lambda e: e.activation(out=rsx[:, tt:tt + 1], in_=ssx[:, tt:tt + 1], func=AF.Sqrt,
                                                 scale=1.0 / D, bias=eps_c),
                   reads=[("ssx", tt), "eps"], writes=[("rsx0", tt)])
            sc.add("dve", lambda e: e.reciprocal(out=rsx[:, tt:tt + 1], in_=rsx[:, tt:tt + 1]),
                   reads=[("rsx0", tt)], writes=[("rsx", tt)])
            sc.add("act", lambda e: e.activation(out=XS[s], in_=XIN[s], func=AF.Copy, scale=rsx[:, tt:tt + 1]),
                   reads=[("xin", s), ("rsx", tt)], writes=[("xs", s)])

        def a_transpose(tt):
            s = tt % 2
            for half in range(2):
                def f(e, half=half):
                    r = None
                    for j in range(8):
                        kc = half * 8 + j
                        r = e.transpose(out=pbb(0)[:, j * 128:(j + 1) * 128], in_=XS[s][:, kc * 128:(kc + 1) * 128],
                                        identity=ident_b[:])
                    return r
                sc.add("pe", f, reads=[("xs", s), "ident_b"], writes=[("pb", 0)])
                sc.add("dve", lambda e, half=half: e.tensor_copy(
                    out=XT[s][:, half * 8:(half + 1) * 8, :],
                    in_=pbb(0).rearrange("p (a b) -> p a b", a=8)),
                    reads=[("pb", 0)], writes=[("xT", s, half)])

        def a_proj(tt):
            s = tt % 2
            base = 2 + 3 * (tt % 2)

            def f(e):
                r = None
                for kc in range(16):
                    for nb in range(3):
                        r = e.matmul(pb(base + nb), lhsT=XT[s][:, kc, :], rhs=WP[:, kc, nb * 512:(nb + 1) * 512],
                                     start=(kc == 0), stop=(kc == 15))
                return r
            sc.add("pe", f, reads=[("xT", s, 0), ("xT", s, 1)] + [("WP", kc) for kc in range(16)],
                   writes=[("pb", base + q_) for q_ in range(3)])

        def a_evac(tt):
            s = tt % 2
            base = 2 + 3 * (tt % 2)
            qk_ps = [pb(base), pb(base + 1)]
            for h2_ in range(2):
                sc.add("act", lambda e, h2_=h2_: e.activation(
                    out=SQ[:, h2_ * 4:(h2_ + 1) * 4, :], in_=qk_ps[h2_].rearrange("p (a b) -> p a b", a=4),
                    func=AF.Square), reads=[("pb", base + h2_)], writes=[("sq", h2_)])
            sc.add("dve", lambda e: e.tensor_reduce(out=ssqk[:, s * 8:(s + 1) * 8], in_=SQ, axis=AX.X, op=ALU.add),
                   reads=[("sq", 0), ("sq", 1)], writes=[("ssqk", s)])
            sc.add("act", lambda e: e.activation(out=rsqk[:, s * 8:(s + 1) * 8], in_=ssqk[:, s * 8:(s + 1) * 8],
                                                 func=AF.Sqrt, scale=1.0 / 128, bias=eps_c),
                   reads=[("ssqk", s), "eps"], writes=[("rsqk0", s)])
            sc.add("dve", lambda e: e.reciprocal(out=rsqk[:, s * 8:(s + 1) * 8], in_=rsqk[:, s * 8:(s + 1) * 8]),
                   reads=[("rsqk0", s)], writes=[("rsqk", s)])
            for h2_ in range(2):
                sc.add("dve", lambda e, h2_=h2_: e.tensor_tensor(
                    out=QKB[s][:, h2_ * 4:(h2_ + 1) * 4, :], in0=qk_ps[h2_].rearrange("p (a b) -> p a b", a=4),
                    in1=bc(rsqk[:, s * 8 + h2_ * 4:s * 8 + (h2_ + 1) * 4], [128, 4, 128]), op=ALU.mult),
                    reads=[("pb", base + h2_), ("rsqk", s)], writes=[("qkb", s, h2_)])
            sc.add("act", lambda e: e.copy(out=VA[:, tt, :, 0:128],
                                           in_=pb(base + 2)[:, 0:256].rearrange("p (a b) -> p a b", a=2)),
                   reads=[("pb", base + 2)], writes=[("VA", tt)])
            sc.add("act", lambda e: e.copy(out=VB[:, tt, 0:256], in_=pb(base + 2)[:, 256:512]),
                   reads=[("pb", base + 2)], writes=[("VB", tt)])

        def a_qkT(tt):
            s = tt % 2

            def f(e):
                r = None
                for j in range(8):
                    r = e.transpose(out=pbb(1)[:, j * 128:(j + 1) * 128], in_=QKB[s][:, j, :], identity=ident_b[:])
                return r
            sc.add("pe", f, reads=[("qkb", s, 0), ("qkb", s, 1), "ident_b"], writes=[("pb", 1)])
            sc.add("dve", lambda e: e.tensor_tensor(out=QKT[:, :, tt * 128:(tt + 1) * 128],
                                                    in0=pbb(1).rearrange("p (a b) -> p a b", a=8),
                                                    in1=bc(qkg_s, [128, 8, 128]), op=ALU.mult),
                   reads=[("pb", 1), "qkg"], writes=[("QKT", tt)])

        ZERO = A.view(R2 + 28672, [128, 1024], F32)
        sc.add("pool", lambda e: e.memset(ZERO, 0.0), writes=["zero"])
        part_v = part.rearrange("(a b p) n -> p a b n", p=128, b=2)

        def zero_fill(a_):
            sc.add("sync", lambda e: e.dma_start(out=part_v[:, a_, :, :], in_=ZERO.rearrange("p (b n) -> p b n", b=2)),
                   reads=["zero"], writes=[("partz", a_)], kind="dma")
        partz_all = [("partz", a_) for a_ in range(64)]
        a_load(0)
        a_load(1)
        a_norm(0)
        a_transpose(0)
        for tt in range(NT):
            a_proj(tt)
            if tt + 1 < NT:
                a_norm(tt + 1)
                a_transpose(tt + 1)
            if tt + 2 < NT:
                a_load(tt + 2)
            zero_fill(2 * tt)
            zero_fill(2 * tt + 1)
            if tt >= 1:
                a_qkT(tt - 1)
            a_evac(tt)
        a_qkT(NT - 1)
        sc.barrier()
        if stop_after == "A":
            sc.add("sync", lambda e: e.dma_start(out=out[0:128, :], in_=XIN[0]), writes=["outdummy"], kind="dma")
            sc.emit(block)
            return nc

        WO = A.view(R1, [128, 16, D], BF16)
        TS = [A.view(R2 + i * 2048, [128, 2, 256], F32) for i in range(3)]
        PT = [A.view(R2 + 6144 + i * 1024, [128, 2, 256], BF16) for i in range(3)]
        ALB = A.view(R2 + 9216, [128, 4, 256], F32)
        OB1 = A.view(R2 + 13312, [128, 256], F32)
        OBN = [A.view(R2 + 14336 + i * 1024, [128, 512], BF16) for i in range(2)]
        NAB = A.view(R2 + 16384, [128, NPAT, 128], F32)
        TNA = A.view(R2 + 29184, [128, 5, 128], F32)
        PNA = A.view(R2 + 31744, [128, 5, 128], BF16)
        LAMT = A.view(R2 + 33024, [128, 4, 128], F32)
        JB = A.view(R2 + 35072, [128, 256], F32)
        assert R2 + 36096 <= ARENA_USE
        cb_s = sm(64)
        wog_s = sm(16)
        lam_s = sm(8)
        dst = sm(64)
        nst = sm(8)

        sc.add("sync", lambda e: e.dma_start(out=ALB, in_=alib), writes=["alb"], kind="dma")
        sc.add("sync", lambda e: e.dma_start(out=cb_s, in_=cbias), writes=["cb"], kind="dma")
        sc.add("sync", lambda e: e.dma_start(out=wog_s, in_=wog), writes=["wog"], kind="dma")
        for i in range(4):
            sc.add("sync", lambda e, i=i: e.dma_start(out=LAMT[:, i, :], in_=lamv[i:i + 1, :].partition_broadcast(128)),
                   writes=[("lamt", i)], kind="dma")
        sc.add("dve", lambda e: e.tensor_tensor(out=LAMT[:, 0, :], in0=LAMT[:, 0, :], in1=LAMT[:, 1, :], op=ALU.mult),
               reads=[("lamt", 0), ("lamt", 1)], writes=["lp1"])
        sc.add("dve", lambda e: e.tensor_tensor(out=LAMT[:, 2, :], in0=LAMT[:, 2, :], in1=LAMT[:, 3, :], op=ALU.mult),
               reads=[("lamt", 2), ("lamt", 3)], writes=["lp2"])
        sc.add("dve", lambda e: e.tensor_reduce(out=lam_s[:, 0:1], in_=LAMT[:, 0, :], axis=AX.X, op=ALU.add),
               reads=["lp1"], writes=["ls1"])
        sc.add("dve", lambda e: e.tensor_reduce(out=lam_s[:, 1:2], in_=LAMT[:, 2, :], axis=AX.X, op=ALU.add),
               reads=["lp2"], writes=["ls2"])
        sc.add("act", lambda e: e.activation(out=lam_s[:, 2:4], in_=lam_s[:, 0:2], func=AF.Exp),
               reads=["ls1", "ls2"], writes=["lexp"])
        sc.add("dve", lambda e: e.tensor_tensor(out=lam_s[:, 4:5], in0=lam_s[:, 3:4], in1=lam_s[:, 2:3], op=ALU.subtract),
               reads=["lexp"], writes=["ldiff"])
        sc.add("dve", lambda e: e.tensor_scalar(out=lam_s[:, 5:6], in0=lam_s[:, 4:5], scalar1=-LAM_INIT, scalar2=None,
                                                op0=ALU.add),
               reads=["ldiff"], writes=["neglam"])

        w_out_v = w_out_p.rearrange("(kc p) n -> p kc n", p=128)
        for kc in range(16):
            sc.add("pool", lambda e, kc=kc: e.dma_start(out=WO[:, kc, :], in_=w_out_v[:, kc, :]),
                   writes=[("WOraw", kc)], kind="dma")
        for kc in range(16):
            mul2 = 1.0 if (kc % 4) < 2 else (1.0 - LAM_INIT)
            sc.add("pool", lambda e, kc=kc, mul2=mul2: e.tensor_scalar(
                out=WO[:, kc, :], in0=WO[:, kc, :], scalar1=wog_s[:, kc:kc + 1], scalar2=mul2, op0=ALU.mult, op1=ALU.mult),
                reads=[("WOraw", kc), "wog"], writes=[("WO", kc)])

        if stop_after == "B0":
            sc.add("sync", lambda e: e.dma_start(out=out[0:128, :], in_=XIN[0]), writes=["outdummy"], kind="dma")
            sc.emit(block)
            return nc
        SB = [4, 5, 6, 7]
        ACC = [0, 1, 2, 3]
        Q1, Q2, K1, K2 = 4, 5, 6, 7

        def b_score(qb, kt, n):
            bk = SB[n % 4]

            def f(e):
                e.matmul(pb(bk)[:, 0:256], lhsT=QKT[:, K1, kt * 128:(kt + 1) * 128],
                         rhs=QKT[:, Q1, qb * 256:(qb + 1) * 256], start=True, stop=True)
                return e.matmul(pb(bk)[:, 256:512], lhsT=QKT[:, K2, kt * 128:(kt + 1) * 128],
                                rhs=QKT[:, Q2, qb * 256:(qb + 1) * 256], start=True, stop=True)
            sc.add("pe", f, reads=[("QKT", kt), ("QKT", 2 * qb), ("QKT", 2 * qb + 1)], writes=[("pb", bk)])

        def b_soft(qb, kt, n):
            bk = SB[n % 4]
            delta = 2 * qb - kt
            if delta >= 1:
                ti = 0
            elif delta <= -2:
                ti = 1
            elif delta == 0:
                ti = 2
            else:
                ti = 3
            ci = delta + 32
            sc.add("dve", lambda e: e.scalar_tensor_tensor(
                out=TS[n % 3], in0=pb(bk).rearrange("p (a b) -> p a b", a=2), scalar=SCALE,
                in1=ALB[:, ti:ti + 1, :].to_broadcast([128, 2, 256]), op0=ALU.mult, op1=ALU.add),
                reads=[("pb", bk), "alb"], writes=[("ts", n % 3)])
            sc.add("act", lambda e: e.activation(out=PT[n % 3], in_=TS[n % 3], func=AF.Exp, bias=cb_s[:, ci:ci + 1]),
                   reads=[("ts", n % 3), "cb"], writes=[("pt", n % 3)])

        def b_pv(qb, kt, n):
            def f(e):
                r = None
                for i in range(2):
                    for j in range(2):
                        r = e.matmul(pb(ACC[i * 2 + j])[:, 0:258], lhsT=PT[n % 3][:, i, j * 128:(j + 1) * 128],
                                     rhs=VB[:, kt, :], start=(kt == 0), stop=(kt == NT - 1))
                return r
            sc.add("pe", f, reads=[("pt", n % 3), ("VB", kt), "VBones"], writes=[("pb", q_) for q_ in range(4)])

        def b_epilogue(qb):
            for j in range(2):
                tq = 2 * qb + j
                a1 = pb(ACC[j])
                a2 = pb(ACC[2 + j])
                st = dst[:, (tq % 4) * 8:(tq % 4) * 8 + 8]
                k = ("dst", tq % 4)
                ka1, ka2 = ("pb", ACC[j]), ("pb", ACC[2 + j])
                sc.add("dve", lambda e, a1=a1, st=st: e.reciprocal(out=st[:, 0:1], in_=a1[:, 256:257]),
                       reads=[ka1], writes=[k + (0,)])
                sc.add("dve", lambda e, a2=a2, st=st: e.reciprocal(out=st[:, 1:2], in_=a2[:, 256:257]),
                       reads=[ka2], writes=[k + (1,)])
                sc.add("dve", lambda e, st=st: e.tensor_tensor(out=st[:, 2:3], in0=st[:, 1:2], in1=lam_s[:, 5:6], op=ALU.mult),
                       reads=[k + (1,), "neglam"], writes=[k + (2,)])
                sc.add("dve", lambda e, a1=a1, st=st: e.tensor_scalar(out=OB1, in0=a1[:, 0:256], scalar1=st[:, 0:1],
                                                                      scalar2=None, op0=ALU.mult),
                       reads=[ka1, k + (0,)], writes=["ob1a"])
                sc.add("dve", lambda e, a2=a2, st=st: e.scalar_tensor_tensor(out=OB1, in0=a2[:, 0:256], scalar=st[:, 2:3],
                                                                             in1=OB1, op0=ALU.mult, op1=ALU.add),
                       reads=[ka2, "ob1a", k + (2,)], writes=["ob1"])
                sc.add("dve", lambda e: e.tensor_tensor(out=JB, in0=OB1, in1=OB1, op=ALU.mult), reads=["ob1"], writes=["jb"])
                sc.add("dve", lambda e, st=st: e.tensor_reduce(out=st[:, 3:4], in_=JB, axis=AX.X, op=ALU.add),
                       reads=["jb"], writes=[k + (3,)])
                sc.add("act", lambda e, st=st: e.activation(out=st[:, 4:5], in_=st[:, 3:4], func=AF.Ln, scale=1.0 / 256,
                                                            bias=eps_c),
                       reads=[k + (3,), "eps"], writes=[k + (4,)])
                sc.add("act", lambda e, st=st: e.activation(out=st[:, 5:6], in_=st[:, 4:5], func=AF.Exp, scale=-0.5),
                       reads=[k + (4,)], writes=[k + (5,)])
                sc.add("dve", lambda e, st=st, tq=tq: e.tensor_scalar(out=OBN[tq % 2][:, 256:512], in0=OB1, scalar1=st[:, 5:6],
                                                                      scalar2=None, op0=ALU.mult),
                       reads=["ob1", k + (5,)], writes=[("obn_b", tq % 2)])
                sc.add("sync", lambda e, tq=tq: e.dma_start(out=ag1_in[tq * 128:(tq + 1) * 128, 256:512],
                                                            in_=OBN[tq % 2][:, 256:512]),
                       reads=[("obn_b", tq % 2)], writes=[("ag1_in_b", tq)], kind="dma")

        if stop_after == "B":
            sc.add("sync", lambda e: e.dma_start(out=out[0:128, :], in_=XIN[0]), writes=["outdummy"], kind="dma")
            sc.emit(block)
            return nc
        na2 = es.enter_context(nc.sbuf_tensor("na2", [128, 3840], U8))
        TNAs = [TNA, na2[:, 0:2560].bitcast(F32).rearrange("p (a b) -> p a b", a=5)]
        PNAs = [PNA, na2[:, 2560:3840].bitcast(BF16).rearrange("p (a b) -> p a b", a=5)]
        NSAs, NSBs, NOs = [0, 2, 4], [1, 3, 5], [6, 7]

        def na_info(m):
            if m in (0, 1, 30, 31):
                sp = {0: 0, 1: 1, 30: 2, 31: 3}[m]
                p0 = 5 + sp * 5
            else:
                p0 = 0
            if m in (0, 1):
                kts = [0, 1, 2, 3, 3]
            elif m in (30, 31):
                kts = [28, 29, 30, 31, 31]
            else:
                kts = [m - 2 + i for i in range(5)]
            return p0, kts

        def na_score(hh, m):
            par3 = (hh * NT + m) % 3
            qch, kch = hh, 2 + hh
            p0, kts = na_info(m)
            nsa, nsb = NSAs[par3], NSBs[par3]

            def f(e):
                r = None
                for i in range(5):
                    o = pb(nsa)[:, i * 128:(i + 1) * 128] if i < 4 else pb(nsb)[:, 0:128]
                    r = e.matmul(o, lhsT=QKT[:, kch, kts[i] * 128:(kts[i] + 1) * 128],
                                 rhs=QKT[:, qch, m * 128:(m + 1) * 128], start=True, stop=True)
                return r
            sc.add("pe", f, reads=[("QKT", k_) for k_ in set(kts + [m])], writes=[("pb", nsa), ("pb", nsb)])

        def na_soft(hh, m):
            par = (hh * NT + m) % 2
            par3 = (hh * NT + m) % 3
            p0, kts = na_info(m)
            nsa, nsb = NSAs[par3], NSBs[par3]
            tna, pna = TNAs[par], PNAs[par]
            sc.add("dve", lambda e: e.scalar_tensor_tensor(
                out=tna[:, 0:4, :], in0=pb(nsa).rearrange("p (a b) -> p a b", a=4), scalar=SCALE,
                in1=NAB[:, p0:p0 + 4, :], op0=ALU.mult, op1=ALU.add),
                reads=[("pb", nsa), "nab"], writes=[("tna0", par)])
            sc.add("dve", lambda e: e.scalar_tensor_tensor(
                out=tna[:, 4, :], in0=pb(nsb)[:, 0:128], scalar=SCALE,
                in1=NAB[:, p0 + 4, :], op0=ALU.mult, op1=ALU.add),
                reads=[("pb", nsb), "nab"], writes=[("tna1", par)])
            sc.add("act", lambda e: e.activation(out=pna, in_=tna, func=AF.Exp),
                   reads=[("tna0", par), ("tna1", par)], writes=[("pna", par)])

        def na_out(hh, m):
            par = (hh * NT + m) % 2
            p0, kts = na_info(m)
            no = NOs[par]
            pna = PNAs[par]

            def f2(e):
                r = None
                for i in range(5):
                    r = e.matmul(pb(no)[:, 0:130], lhsT=pna[:, i, :], rhs=VA[:, kts[i], hh, :],
                                 start=(i == 0), stop=(i == 4))
                return r
            sc.add("pe", f2, reads=[("pna", par), "VAones"] + [("VA", k_) for k_ in set(kts)], writes=[("pb", no)])
            nsc = nst[:, par * 2 + hh:par * 2 + hh + 1]
            sc.add("dve", lambda e: e.reciprocal(out=nsc, in_=pb(no)[:, 128:129]), reads=[("pb", no)],
                   writes=[("nst", par, hh)])
            ob = OBN[m % 2]
            sc.add("dve", lambda e: e.tensor_scalar(out=ob[:, hh * 128:(hh + 1) * 128], in0=pb(no)[:, 0:128],
                                                    scalar1=nsc, scalar2=None, op0=ALU.mult),
                   reads=[("pb", no), ("nst", par, hh)], writes=[("obn_a", m % 2)])
            sc.add("sync", lambda e: e.dma_start(out=ag1_in[m * 128:(m + 1) * 128, hh * 128:(hh + 1) * 128],
                                                 in_=ob[:, hh * 128:(hh + 1) * 128]),
                   reads=[("obn_a", m % 2)], writes=[("ag1_in_a", hh, m)], kind="dma")

        for hh in range(2):
            sc.add("sync", lambda e, hh=hh: e.dma_start(out=NAB, in_=nab[hh]), writes=["nab"], kind="dma")
            na_score(hh, 0)
            na_score(hh, 1)
            na_soft(hh, 0)
            for m in range(NT):
                if m + 2 < NT:
                    na_score(hh, m + 2)
                if m + 1 < NT:
                    na_soft(hh, m + 1)
                na_out(hh, m)

        def ag_slab(k_):
            rd = [("ag1_in_b", t) for t in range(8 * k_, 8 * k_ + 8)] + \
                 [("ag1_in_a", h_, t) for h_ in range(2) for t in range(8 * k_, 8 * k_ + 8)]
            sc.add("pool", lambda e: e.collective_compute(
                "AllGather", ALU.bypass, replica_groups=GROUPS,
                ins=[ag1_in[1024 * k_:1024 * (k_ + 1), :].opt()], outs=[ag1_out[4096 * k_:4096 * (k_ + 1), :].opt()]),
                reads=rd, writes=[("ag1_out", k_)], kind="cc")

        seq = [(qb, kt) for qb in range(16) for kt in range(NT)]
        LOOK = 3
        for i in range(min(LOOK, len(seq))):
            b_score(seq[i][0], seq[i][1], i)
        for i, (qb, kt) in enumerate(seq):
            if i + LOOK < len(seq):
                b_score(seq[i + LOOK][0], seq[i + LOOK][1], i + LOOK)
            b_soft(qb, kt, i)
            b_pv(qb, kt, i)
            if kt == NT - 1:
                b_epilogue(qb)
                if qb % 4 == 3:
                    ag_slab(qb // 4)

        if stop_after == "C":
            sc.add("sync", lambda e: e.dma_start(out=out[0:128, :], in_=XIN[0]), writes=["outdummy"], kind="dma")
            sc.emit(block)
            return nc
        ag_reads = [("ag1_in_b", t) for t in range(NT)] + [("ag1_in_a", h_, t) for h_ in range(2) for t in range(NT)]
        if debug:
            for t_ in range(NT):
                sc.add("sync", lambda e, t_=t_: e.dma_start(out=dbg["mix"][t_ * 128:(t_ + 1) * 128, :],
                                                            in_=ag1_in[t_ * 128:(t_ + 1) * 128, :]),
                       reads=ag_reads, writes=[("dbg_mix", t_)], kind="dma")
        sc.barrier()
        if stop_after == "attn":
            sc.add("sync", lambda e: e.dma_start(out=out[0:128, :], in_=XIN[0]), writes=["outdummy"], kind="dma")
            sc.emit(block)
            return nc

        P0 = 0
        MIXT = [A.view(P0 + i * 4096, [128, 4, 512], BF16) for i in range(2)]
        MIXN = [A.view(P0 + 8192 + i * 4096, [128, 4, 512], BF16) for i in range(2)]
        MXT = [A.view(P0 + 16384 + i * 4096, [128, 16, 128], BF16) for i in range(2)]
        XO = [A.view(P0 + 24576 + i * 8192, [128, D], F32) for i in range(2)]
        X1 = [A.view(P0 + 40960 + i * 8192, [128, D], F32) for i in range(2)]
        H2F = A.view(P0 + 57344, [128, D], F32)
        H2T = A.view(P0 + 65536, [128, 16, 128], F32)
        H2B = [A.view(P0 + 73728 + i * 4096, [128, D], BF16) for i in range(2)]
        LN2R = A.view(P0 + 81920, [128, D], F32)
        WR = A.view(P0 + 90112, [128, 16, N_EXP], F32)
        JD = A.view(P0 + 95232, [128, D], BF16)
        assert P0 + 95232 + 4096 <= R1 + 65536
        JD = A.view(R2, [128, D], BF16)
        own_s = es.enter_context(nc.sbuf_tensor("own_s", [128, 8, 4], I32))
        mixi_s = es.enter_context(nc.sbuf_tensor("mixi_s", [128, 8, 4], I32))
        dstat = sm(8 * 8)
        logit = sm(8 * 16)
        affs = sm(8 * 16)

        sc.add("sync", lambda e: e.dma_start(out=own_s[:], in_=own_tok), writes=["own"], kind="dma")
        sc.add("sync", lambda e: e.dma_start(out=mixi_s[:], in_=mix_idx), writes=["mixi"], kind="dma")
        sc.add("sync", lambda e: e.dma_start(out=LN2R, in_=ln2[0:1, :].partition_broadcast(128)), writes=["ln2r"], kind="dma")
        sc.add("sync", lambda e: e.dma_start(out=WR, in_=w_router.rearrange("(kc p) n -> p kc n", p=128)),
               writes=["wr"], kind="dma")

        def d_front(i):
            s = i % 2
            st = dstat[:, i * 8:(i + 1) * 8]
            for r in range(4):
                sc.add("pool", lambda e, r=r: e.indirect_dma_start(
                    out=MIXT[s][:, r, :], out_offset=None, in_=ag1_out,
                    in_offset=bass.IndirectOffsetOnAxis(ap=mixi_s[:, i, r:r + 1], axis=0)),
                    reads=[("ag1_out", k_) for k_ in range(4)] + ["mixi"], writes=[("mixt", s, r)], kind="dma")
            sc.add("sync", lambda e: e.dma_start(out=XO[s], in_=x_own[i * 128:(i + 1) * 128, :]),
                   writes=[("xo", s)], kind="dma")
            mr = [("mixt", s, r) for r in range(4)]
            sc.add("act", lambda e: e.activation(out=JD[:, 0:1024].rearrange("p (a b) -> p a b", a=4),
                                                 in_=MIXT[s][:, :, 0:256], func=AF.Square, accum_out=st[:, 0:1]),
                   reads=mr, writes=["jd", ("dst0", i)])
            sc.add("act", lambda e: e.activation(out=st[:, 1:2], in_=st[:, 0:1], func=AF.Sqrt, scale=1.0 / 1024, bias=eps_c),
                   reads=[("dst0", i), "eps"], writes=[("dst1", i)])
            sc.add("dve", lambda e: e.reciprocal(out=st[:, 2:3], in_=st[:, 1:2]), reads=[("dst1", i)], writes=[("dst2", i)])
            sc.add("dve", lambda e: e.tensor_scalar(out=MIXN[s][:, :, 0:256], in0=MIXT[s][:, :, 0:256], scalar1=st[:, 2:3],
                                                    scalar2=None, op0=ALU.mult),
                   reads=mr + [("dst2", i)], writes=[("mixn_a", s)])
            sc.add("dve", lambda e: e.tensor_copy(out=MIXN[s][:, :, 256:512], in_=MIXT[s][:, :, 256:512]),
                   reads=mr, writes=[("mixn_b", s)])
            for half in range(2):
                def f(e, half=half):
                    r_ = None
                    for j in range(8):
                        kc = half * 8 + j
                        r_ = e.transpose(out=pbb(0)[:, j * 128:(j + 1) * 128],
                                         in_=MIXN[s][:, kc // 4, (kc % 4) * 128:(kc % 4 + 1) * 128], identity=ident_b[:])
                    return r_
                sc.add("pe", f, reads=[("mixn_a", s), ("mixn_b", s), "ident_b"], writes=[("pb", 0)])
                sc.add("act", lambda e, half=half: e.copy(out=MXT[s][:, half * 8:(half + 1) * 8, :],
                                                          in_=pbb(0).rearrange("p (a b) -> p a b", a=8)),
                       reads=[("pb", 0)], writes=[("mxt", s, half)])

        def d_proj(i):
            s = i % 2
            st = dstat[:, i * 8:(i + 1) * 8]
            x1r = [("x1", s, nb) for nb in range(4)]
            for nb in range(4):
                bk = 1 + (nb % 2)

                def f(e, nb=nb, bk=bk):
                    r_ = None
                    for kc in range(16):
                        r_ = e.matmul(pb(bk), lhsT=MXT[s][:, kc, :], rhs=WO[:, kc, nb * 512:(nb + 1) * 512],
                                      start=(kc == 0), stop=(kc == 15))
                    return r_
                sc.add("pe", f, reads=[("mxt", s, 0), ("mxt", s, 1)] + [("WO", kc) for kc in range(16)],
                       writes=[("pb", bk)])
                sc.add("dve", lambda e, nb=nb, bk=bk: e.tensor_tensor(out=X1[s][:, nb * 512:(nb + 1) * 512], in0=pb(bk),
                                                                      in1=XO[s][:, nb * 512:(nb + 1) * 512], op=ALU.add),
                       reads=[("pb", bk), ("xo", s)], writes=[("x1", s, nb)])
            for db in range(4):
                sc.add("pool", lambda e, db=db: e.indirect_dma_start(
                    out=part, out_offset=bass.IndirectOffsetOnAxis(ap=own_s[:, i, db:db + 1], axis=0),
                    in_=X1[s][:, db * 512:(db + 1) * 512], in_offset=None),
                    reads=x1r + ["own"] + partz_all, writes=[("part_x1", i, db)], kind="dma")
            if debug:
                sc.add("sync", lambda e: e.dma_start(out=dbg["x1"][i * 128:(i + 1) * 128, :], in_=X1[s]),
                       reads=x1r, writes=[("dbgx1", i)], kind="dma")

        def d_back(i):
            s = i % 2
            st = dstat[:, i * 8:(i + 1) * 8]
            x1r = [("x1", s, nb) for nb in range(4)]
            sc.add("act", lambda e: e.activation(out=JD, in_=X1[s], func=AF.Square, accum_out=st[:, 3:4]),
                   reads=x1r, writes=["jd", ("dst3", i)])
            sc.add("act", lambda e: e.activation(out=st[:, 4:5], in_=st[:, 3:4], func=AF.Sqrt, scale=1.0 / D, bias=eps_c),
                   reads=[("dst3", i), "eps"], writes=[("dst4", i)])
            sc.add("dve", lambda e: e.reciprocal(out=st[:, 5:6], in_=st[:, 4:5]), reads=[("dst4", i)], writes=[("dst5", i)])
            sc.add("dve", lambda e: e.scalar_tensor_tensor(out=H2F, in0=X1[s], scalar=st[:, 5:6], in1=LN2R,
                                                           op0=ALU.mult, op1=ALU.mult),
                   reads=x1r + [("dst5", i), "ln2r"], writes=["h2f"])
            sc.add("act", lambda e: e.copy(out=H2B[s], in_=H2F), reads=["h2f"], writes=[("h2b", s)])
            sc.add("sync", lambda e: e.dma_start(out=h2_in[i * 128:(i + 1) * 128, :], in_=H2B[s]),
                   reads=[("h2b", s)], writes=[("h2_in", i)], kind="dma")
            for q4 in range(4):
                def f(e, q4=q4):
                    r_ = None
                    for j in range(4):
                        kc = q4 * 4 + j
                        r_ = e.transpose(out=pb(3 + (q4 % 2))[:, j * 128:(j + 1) * 128], in_=H2F[:, kc * 128:(kc + 1) * 128],
                                         identity=ident_f[:])
                    return r_
                sc.add("pe", f, reads=["h2f", "ident_f"], writes=[("pb", 3 + (q4 % 2))])
                sc.add("act", lambda e, q4=q4: e.copy(out=H2T[:, q4 * 4:(q4 + 1) * 4, :],
                                                      in_=pb(3 + (q4 % 2)).rearrange("p (a b) -> p a b", a=4)),
                       reads=[("pb", 3 + (q4 % 2))], writes=[("h2t", q4)])

            def fl(e):
                r_ = None
                for kc in range(16):
                    r_ = e.matmul(pb(5)[:, 0:N_EXP], lhsT=H2T[:, kc, :], rhs=WR[:, kc, :], start=(kc == 0), stop=(kc == 15))
                return r_
            sc.add("pe", fl, reads=[("h2t", q4) for q4 in range(4)] + ["wr"], writes=[("pb", 5)])
            lg = logit[:, i * 16:(i + 1) * 16]
            af = affs[:, i * 16:(i + 1) * 16]
            sc.add("dve", lambda e: e.tensor_reduce(out=st[:, 6:7], in_=pb(5)[:, 0:N_EXP], axis=AX.X, op=ALU.max),
                   reads=[("pb", 5)], writes=[("dst6", i)])
            sc.add("dve", lambda e: e.tensor_scalar(out=lg, in0=pb(5)[:, 0:N_EXP], scalar1=st[:, 6:7], scalar2=None,
                                                    op0=ALU.subtract),
                   reads=[("pb", 5), ("dst6", i)], writes=[("lg", i)])
            sc.add("act", lambda e: e.activation(out=lg, in_=lg, func=AF.Exp, accum_out=st[:, 7:8]),
                   reads=[("lg", i)], writes=[("lge", i), ("dst7", i)])
            sc.add("dve", lambda e: e.reciprocal(out=st[:, 7:8], in_=st[:, 7:8]), reads=[("dst7", i)], writes=[("dst7r", i)])
            sc.add("dve", lambda e: e.tensor_scalar(out=af, in0=lg, scalar1=st[:, 7:8], scalar2=None, op0=ALU.mult),
                   reads=[("lge", i), ("dst7r", i)], writes=[("aff", i)])
            sc.add("sync", lambda e: e.dma_start(out=aff_in[i * 128:(i + 1) * 128, :], in_=af),
                   reads=[("aff", i)], writes=[("aff_in", i)], kind="dma")

        def d_ag(i):
            if i % 2 == 1:
                j_ = i // 2
                sc.add("pool", lambda e, j_=j_: e.collective_compute(
                    "AllGather", ALU.bypass, replica_groups=GROUPS,
                    ins=[h2_in[256 * j_:256 * (j_ + 1), :].opt()], outs=[h2_all[1024 * j_:1024 * (j_ + 1), :].opt()]),
                    reads=[("h2_in", 2 * j_), ("h2_in", 2 * j_ + 1)], writes=[("h2_all", j_)], kind="cc")

        d_front(0)
        for i in range(8):
            if i + 1 < 8:
                d_front(i + 1)
            d_proj(i)
            if i >= 1:
                d_back(i - 1)
                d_ag(i - 1)
        d_back(7)
        d_ag(7)
        sc.add("pool", lambda e: e.collective_compute("AllGather", ALU.bypass, replica_groups=GROUPS,
                                                      ins=[aff_in.opt()], outs=[aff_all.opt()]),
               reads=[("aff_in", i) for i in range(8)], writes=["aff_all"], kind="cc")
        if debug:
            sc.add("sync", lambda e: e.dma_start(out=dbg["aff"], in_=aff_all), reads=["aff_all"], writes=["dbg_aff"], kind="dma")
        sc.barrier(include_cc=False)
        if stop_after == "router":
            sc.add("sync", lambda e: e.dma_start(out=out[0:128, :], in_=X1[0]), writes=["outdummy"], kind="dma")
            sc.emit(block)
            return nc

        E0 = 0
        XSG = A.view(E0, [128, 4, D], BF16)
        XST = A.view(E0 + 16384, [128, 16, 512], BF16)
        HT = A.view(E0 + 32768, [128, NFC, 512], BF16)
        YT = [A.view(E0 + 55296 + i * 2048, [128, 512], F32) for i in range(4)]
        ST_ = [A.view(E0 + 63488 + i * 1024, [128, 512], BF16) for i in range(4)]
        SG = A.view(E0 + 67584, [128, 512], F32)
        SG2 = A.view(E0 + 69632, [128, 512], F32)
        NGU = 5
        WG = [A.view(E0 + 71680 + i * 8192, [128, 16, 128], BF16) for i in range(NGU)]
        WU = [A.view(E0 + 71680 + i * 8192 + 4096, [128, 16, 128], BF16) for i in range(NGU)]
        WDO = E0 + 71680 + NGU * 8192
        WD = [A.view(WDO + i * 22528, [128, NFC, 512], BF16) for i in range(2)]
        A16 = A.view(WDO + 45056, [128, NT, N_EXP], F32)
        SELT = A.view(WDO + 47104, [128, 4, N_EXP], F32)
        PRD = A.view(WDO + 47360, [128, NT, N_EXP], F32)
        A4 = A.view(WDO + 49408, [128, 4, NT], F32)
        CMP = A.view(WDO + 49920, [128, 4, NT], F32)
        AP3 = A.view(WDO + 50432, [128, 4, NT, 6], BF16)
        RES = A.view(WDO + 54400, [128, 4, NT], F32)
        UTRI = A.view(WDO + 52224, [128, 128], F32)
        LT32 = A.view(WDO + 52736, [128, NT], F32)
        MSK = A.view(WDO + 53376, [128, 4, NT], F32)
        POS = A.view(WDO + 53888, [128, 4, NT], F32)
        FCV = A.view(WDO + 54912, [128, NT], F32)
        ONESF = A.view(WDO + 55040, [128, 4], F32)
        CSB = A.view(WDO + 55296, [128, 4, 128], F32)
        assert WDO + 57344 <= ARENA_USE, WDO + 57344
        thr = sm(4)
        cand = sm(4)
        cntp = es.enter_context(nc.sbuf_tensor("cntp", [128, 4], BF16))
        ge = sm(4)
        idxf = sm(80)
        pselc = sm(128)
        gts = sm(16)
        idx_i = es.enter_context(nc.sbuf_tensor("idx_i", [128, 4, 20], I32))
        pidx = es.enter_context(nc.sbuf_tensor("pidx", [128, NT], F32))

        gu_n = [0]

        def load_gu(e_, fc):
            k = gu_n[0] % NGU
            gu_n[0] += 1
            gv = wg_e[e_].rearrange("(kc p) n -> p kc n", p=128)
            uv = wu_e[e_].rearrange("(kc p) n -> p kc n", p=128)
            sc.add("pool", lambda e: e.dma_start(out=WG[k], in_=gv[:, :, fc * 128:(fc + 1) * 128]),
                   writes=[("wg", k)], kind="dma")
            sc.add("pool", lambda e: e.dma_start(out=WU[k], in_=uv[:, :, fc * 128:(fc + 1) * 128]),
                   writes=[("wu", k)], kind="dma")
            return k

        wd_n = [0]

        def load_wd(e_, db):
            k = wd_n[0] % 2
            wd_n[0] += 1
            dv = wd_e[e_].rearrange("(fc p) n -> p fc n", p=128)
            for h_ in range(2):
                sc.add("pool", lambda e, h_=h_: e.dma_start(out=WD[k][:, h_ * 11:(h_ + 1) * 11, :],
                                                            in_=dv[:, h_ * 11:(h_ + 1) * 11, db * 512:(db + 1) * 512]),
                       writes=[("wd", k, h_)], kind="dma")
            return k

        sc.add("sync", lambda e: e.dma_start(out=A16, in_=aff_all.rearrange("(c p) j -> p c j", p=128)),
               reads=["aff_all"], writes=["a16"], kind="dma")
        sc.add("sync", lambda e: e.dma_start(out=SELT, in_=sel), writes=["selt"], kind="dma")
        for e_ in range(4):
            sc.add("dve", lambda e, e_=e_: e.tensor_tensor(out=PRD, in0=A16, in1=SELT[:, e_:e_ + 1, :].to_broadcast([128, NT, N_EXP]),
                                                           op=ALU.mult),
                   reads=["a16", "selt"], writes=["prd"])
            sc.add("dve", lambda e, e_=e_: e.tensor_reduce(out=A4[:, e_, :], in_=PRD, axis=AX.X, op=ALU.add),
                   reads=["prd"], writes=[("a4", e_)])
        a4r = [("a4", e_) for e_ in range(4)]
        sc.add("dve", lambda e: e.tensor_scalar(out=UTRI, in0=iota_f[:, 0:128], scalar1=iota_p[:, 0:1], scalar2=None,
                                                op0=ALU.is_gt), reads=["iota_f", "iota_p"], writes=["utri"])
        sc.add("dve", lambda e: e.tensor_scalar(out=LT32, in0=iota_f[:, 0:NT], scalar1=iota_p[:, 0:1], scalar2=None,
                                                op0=ALU.is_gt), reads=["iota_f", "iota_p"], writes=["lt32"])
        sc.add("dve", lambda e: e.tensor_copy(out=pidx[:], in_=iota_p[:, 0:1].to_broadcast([128, NT])),
               reads=["iota_p"], writes=["pidx"])
        sc.add("dve", lambda e: e.tensor_copy(out=AP3[:, :, :, 0], in_=iota_f[:, 0:NT].unsqueeze(1).to_broadcast([128, 4, NT])),
               reads=["iota_f"], writes=["ap3_0"])
        sc.add("sync", lambda e: e.dma_start(out=FCV, in_=fcv), writes=["fcv"], kind="dma")
        sc.add("pool", lambda e: e.memset(ONESF, 1.0), writes=["ones_f"])
        sc.add("dve", lambda e: e.tensor_copy(out=AP3[:, :, :, 1], in_=FCV.unsqueeze(1).to_broadcast([128, 4, NT])),
               reads=["fcv"], writes=["ap3_1"])
        sc.add("dve", lambda e: e.tensor_copy(out=AP3[:, :, :, 2], in_=pidx[:].unsqueeze(1).to_broadcast([128, 4, NT])),
               reads=["pidx"], writes=["ap3_2"])
        sc.add("dve", lambda e: e.tensor_copy(out=AP3[:, :, :, 3], in_=A4), reads=a4r, writes=["ap3_3"])
        sc.add("dve", lambda e: e.tensor_tensor(out=RES, in0=A4, in1=AP3[:, :, :, 3], op=ALU.subtract),
               reads=a4r + ["ap3_3"], writes=["res1"])
        sc.add("dve", lambda e: e.tensor_copy(out=AP3[:, :, :, 4], in_=RES), reads=["res1"], writes=["ap3_4"])
        sc.add("dve", lambda e: e.tensor_tensor(out=RES, in0=RES, in1=AP3[:, :, :, 4], op=ALU.subtract),
               reads=["res1", "ap3_4"], writes=["res2"])
        sc.add("dve", lambda e: e.tensor_copy(out=AP3[:, :, :, 5], in_=RES), reads=["res2"], writes=["ap3_5"])
        ap3r = ["ap3_0", "ap3_1", "ap3_2", "ap3_3", "ap3_4", "ap3_5"]

        sc.add("dve", lambda e: e.memset(thr, 0.0), writes=["thr"])
        for it in range(1, BISECT_ITERS + 1):
            step = 2.0 ** (-it)
            sc.add("dve", lambda e, step=step: e.tensor_scalar(out=cand, in0=thr, scalar1=step, scalar2=None, op0=ALU.add),
                   reads=["thr"], writes=["cand"])
            sc.add("dve", lambda e: e.tensor_tensor(out=CMP, in0=A4, in1=bc(cand, [128, 4, NT]), op=ALU.is_gt),
                   reads=a4r + ["cand"], writes=["cmp"])
            def fcnt(e):
                with nc.allow_low_precision(reason="per-partition counts <= 32 are exact in bf16"):
                    return e.tensor_reduce(out=cntp[:], in_=CMP, axis=AX.X, op=ALU.add)
            sc.add("dve", fcnt, reads=["cmp"], writes=["cntp"])
            sc.add("pe", lambda e: e.matmul(pb(7)[:, 0:4], lhsT=ones_b, rhs=cntp[:], start=True, stop=True),
                   reads=["cntp", "ones_b"], writes=[("pb", 7)])
            sc.add("dve", lambda e: e.tensor_scalar(out=ge, in0=pb(7)[:, 0:4], scalar1=float(CAP) - 0.5, scalar2=None,
                                                    op0=ALU.is_gt), reads=[("pb", 7)], writes=["ge"])
            sc.add("dve", lambda e, step=step: e.scalar_tensor_tensor(out=thr, in0=ge, scalar=step, in1=thr,
                                                                      op0=ALU.mult, op1=ALU.add),
                   reads=["ge", "thr"], writes=["thr"])
        sc.add("dve", lambda e: e.tensor_tensor(out=MSK, in0=A4, in1=bc(thr, [128, 4, NT]), op=ALU.is_gt),
               reads=a4r + ["thr"], writes=["msk"])

        for e_ in range(4):
            sc.add("pe", lambda e, e_=e_: e.matmul(pb(6)[0:NT, e_:e_ + 1], lhsT=MSK[:, e_, :], rhs=ONESF[:, 0:1],
                                                   start=True, stop=True),
                   reads=["msk", "ones_f"], writes=[("pb", 6)])
        for e_ in range(4):
            sc.add("dve", lambda e, e_=e_: e.tensor_copy(out=CSB[0:NT, e_, :], in_=pb(6)[0:NT, e_:e_ + 1].to_broadcast([NT, 128])),
                   reads=[("pb", 6)], writes=[("csb", e_)])
        for e_ in range(4):
            def fpos(e, e_=e_):
                e.matmul(pb(7)[:, e_ * NT:(e_ + 1) * NT], lhsT=UTRI, rhs=MSK[:, e_, :], start=True, stop=False)
                return e.matmul(pb(7)[:, e_ * NT:(e_ + 1) * NT], lhsT=CSB[0:NT, e_, :], rhs=LT32[0:NT, :], start=False, stop=True)
            sc.add("pe", fpos, reads=["msk", "utri", "lt32", ("csb", e_)], writes=[("pb", 7)])
        sc.add("dve", lambda e: e.scalar_tensor_tensor(out=POS, in0=pb(7)[:, 0:4 * NT].rearrange("p (a b) -> p a b", a=4),
                                                       scalar=1.0, in1=MSK, op0=ALU.add, op1=ALU.mult),
               reads=[("pb", 7), "msk"], writes=["pos0"])
        sc.add("dve", lambda e: e.tensor_scalar(out=POS, in0=POS, scalar1=-1.0, scalar2=None, op0=ALU.add),
               reads=["pos0"], writes=["pos"])

        gu_list = [(e_, fc) for e_ in range(4) for fc in range(NFC)]
        wd_list = [(e_, db) for e_ in range(4) for db in range(4)]
        gu_loaded, wd_loaded = [], []

        def gu_prefetch(upto):
            while len(gu_loaded) < min(upto, len(gu_list)):
                gu_loaded.append(load_gu(*gu_list[len(gu_loaded)]))

        def wd_prefetch(upto):
            while len(wd_loaded) < min(upto, len(wd_list)):
                wd_loaded.append(load_wd(*wd_list[len(wd_loaded)]))

        gu_prefetch(NGU - 1)
        wd_prefetch(1)
        prev_scatter = []
        h2r = [("h2_all", j_) for j_ in range(4)]
        def e_select(e_):
            for c in range(NT):
                stile = ST_[c % 4]
                sc.add("dve", lambda e, c=c, stile=stile: e.tensor_scalar(out=stile, in0=iota_f[:], scalar1=POS[:, e_, c:c + 1],
                                                                          scalar2=None, op0=ALU.is_equal),
                       reads=["pos", "iota_f"], writes=[("stile", c % 4)])

                def fsel(e, c=c, stile=stile):
                    r_ = None
                    if c == 0:
                        e.matmul(pb(6)[:, 0:32], lhsT=ident_b[:], rhs=zero_b[:], start=True, stop=False)
                    for sg in range(4):
                        r_ = e.matmul(pb(6)[:, sg * 8:sg * 8 + 6], lhsT=stile[:, sg * 128:(sg + 1) * 128],
                                      rhs=AP3[:, e_, c, :], start=False, stop=(c == NT - 1))
                    return r_
                sc.add("pe", fsel, reads=[("stile", c % 4), "zero_b", "ident_b"] + ap3r, writes=[("pb", 6)])
            ik = ("idx", e_)
            psc = pselc[:, e_ * 32:(e_ + 1) * 32]
            sc.add("dve", lambda e, psc=psc: e.tensor_copy(out=psc, in_=pb(6)[:, 0:32]), reads=[("pb", 6)], writes=[("psc", e_)])
            psv = psc.rearrange("p (a b) -> p a b", a=4)
            fb = e_ * 20
            sc.add("dve", lambda e, psv=psv, fb=fb: e.scalar_tensor_tensor(out=idxf[:, fb:fb + 4], in0=psv[:, :, 0], scalar=128.0,
                                                                           in1=psv[:, :, 2], op0=ALU.mult, op1=ALU.add),
                   reads=[("psc", e_)], writes=[ik + (0,)])
            for db in range(1, 4):
                sc.add("dve", lambda e, fb=fb, db=db: e.tensor_scalar(out=idxf[:, fb + 4 * db:fb + 4 * db + 4], in0=idxf[:, fb:fb + 4],
                                                                      scalar1=float(S * db), scalar2=None, op0=ALU.add),
                       reads=[ik + (0,)], writes=[ik + (0, db)])
            sc.add("dve", lambda e, psv=psv, fb=fb: e.scalar_tensor_tensor(out=idxf[:, fb + 16:fb + 20], in0=psv[:, :, 1], scalar=128.0,
                                                                           in1=psv[:, :, 2], op0=ALU.mult, op1=ALU.add),
                   reads=[("psc", e_)], writes=[ik + (1,)])
            sc.add("dve", lambda e, fb=fb: e.tensor_copy(out=idx_i[:, e_, :], in_=idxf[:, fb:fb + 20]),
                   reads=[ik + (0,), ik + (1,)] + [ik + (0, db) for db in range(1, 4)], writes=[ik])
            sc.add("dve", lambda e, psv=psv: e.tensor_reduce(out=gts[:, e_ * 4:(e_ + 1) * 4], in_=psv[:, :, 3:6], axis=AX.X, op=ALU.add),
                   reads=[("psc", e_)], writes=[("gate", e_)])
            if debug:
                sc.add("sync", lambda e: e.dma_start(out=dbg["idx"][:, e_ * 4:(e_ + 1) * 4], in_=idx_i[:, e_, 0:4]),
                       reads=[ik], writes=[("dbgidx", e_)], kind="dma")
                sc.add("sync", lambda e: e.dma_start(out=dbg["gate"][:, e_ * 4:(e_ + 1) * 4], in_=gts[:, e_ * 4:(e_ + 1) * 4]),
                       reads=[("gate", e_)], writes=[("dbggate", e_)], kind="dma")

        def e_gather(e_):
            ik = ("idx", e_)
            xstr = [("xst", sg, half) for sg in range(4) for half in range(2)]
            htr = [("ht", fc) for fc in range(NFC)]
            for sg in range(4):
                sc.add("pool", lambda e, sg=sg: e.indirect_dma_start(
                    out=XSG[:, sg, :], out_offset=None, in_=h2_all,
                    in_offset=bass.IndirectOffsetOnAxis(ap=idx_i[:, e_, 16 + sg:17 + sg], axis=0)),
                    reads=h2r + [ik], writes=[("xsg", sg)], kind="dma")

        def e_transpose(e_):
            ik = ("idx", e_)
            xstr = [("xst", sg, half) for sg in range(4) for half in range(2)]
            htr = [("ht", fc) for fc in range(NFC)]
            for sg in range(4):
                for half in range(2):
                    bk = 7

                    def ftr(e, sg=sg, half=half, bk=bk):
                        r_ = None
                        for j in range(8):
                            kc = half * 8 + j
                            r_ = e.transpose(out=pbb(bk)[:, j * 128:(j + 1) * 128], in_=XSG[:, sg, kc * 128:(kc + 1) * 128],
                                             identity=ident_b[:])
                        return r_
                    sc.add("pe", ftr, reads=[("xsg", sg), "ident_b"], writes=[("pb", bk)])
                    ce = "act" if half == 0 else "dve"
                    if ce == "act":
                        sc.add("act", lambda e, sg=sg, half=half, bk=bk: e.copy(
                            out=XST[:, half * 8:(half + 1) * 8, sg * 128:(sg + 1) * 128],
                            in_=pbb(bk).rearrange("p (a b) -> p a b", a=8)),
                            reads=[("pb", bk)], writes=[("xst", sg, half)])
                    else:
                        sc.add("dve", lambda e, sg=sg, half=half, bk=bk: e.tensor_copy(
                            out=XST[:, half * 8:(half + 1) * 8, sg * 128:(sg + 1) * 128],
                            in_=pbb(bk).rearrange("p (a b) -> p a b", a=8)),
                            reads=[("pb", bk)], writes=[("xst", sg, half)])
            xstr = [("xst", sg, half) for sg in range(4) for half in range(2)]

        def e_gateup(e_):
            ik = ("idx", e_)
            xstr = [("xst", sg, half) for sg in range(4) for half in range(2)]
            htr = [("ht", fc) for fc in range(NFC)]
            for fc in range(NFC):
                if fc == 6 and e_ + 1 < 4:
                    e_select(e_ + 1)
                if fc == 14 and e_ + 1 < 4:
                    e_gather(e_ + 1)
                n_ = e_ * NFC + fc
                gu_prefetch(n_ + NGU)
                k = gu_loaded[n_]
                pg, pu = 2 + 2 * (fc % 2), 3 + 2 * (fc % 2)

                def fgu(e, k=k, pg=pg, pu=pu):
                    r_ = None
                    for kc in range(16):
                        r_ = e.matmul(pb(pg), lhsT=WG[k][:, kc, :], rhs=XST[:, kc, :], start=(kc == 0), stop=(kc == 15))
                    for kc in range(16):
                        r_ = e.matmul(pb(pu), lhsT=WU[k][:, kc, :], rhs=XST[:, kc, :], start=(kc == 0), stop=(kc == 15))
                    return r_
                sc.add("pe", fgu, reads=xstr + [("wg", k), ("wu", k)], writes=[("pb", pg), ("pb", pu)])
                sgt = SG if fc % 2 == 0 else SG2
                sc.add("act", lambda e, pg=pg, sgt=sgt: e.activation(out=sgt, in_=pb(pg), func=AF.Silu),
                       reads=[("pb", pg)], writes=[("sg", fc % 2)])
                sc.add("dve", lambda e, pu=pu, sgt=sgt, fc=fc: e.tensor_tensor(out=HT[:, fc, :], in0=sgt, in1=pb(pu), op=ALU.mult),
                       reads=[("sg", fc % 2), ("pb", pu)], writes=[("ht", fc)])
            htr = [("ht", fc) for fc in range(NFC)]

        def e_down(e_, prev_scatter):
            ik = ("idx", e_)
            xstr = [("xst", sg, half) for sg in range(4) for half in range(2)]
            htr = [("ht", fc) for fc in range(NFC)]
            scat = []
            for db in range(4):
                nd = e_ * 4 + db
                wd_prefetch(nd + 2)
                kd = wd_loaded[nd]
                for sg in range(4):
                    m_ = db * 4 + sg
                    bk = m_ % 2

                    def fdn(e, sg=sg, kd=kd, bk=bk):
                        r_ = None
                        for fc in range(NFC):
                            r_ = e.matmul(pb(bk), lhsT=HT[:, fc, sg * 128:(sg + 1) * 128], rhs=WD[kd][:, fc, :],
                                          start=(fc == 0), stop=(fc == NFC - 1))
                        return r_
                    sc.add("pe", fdn, reads=htr + [("wd", kd, 0), ("wd", kd, 1)], writes=[("pb", bk)])
                    yt = YT[m_ % 4]
                    if m_ % 2 == 0:
                        sc.add("act", lambda e, bk=bk, yt=yt, sg=sg: e.activation(out=yt, in_=pb(bk), func=AF.Copy,
                                                                                  scale=gts[:, e_ * 4 + sg:e_ * 4 + sg + 1]),
                               reads=[("pb", bk), ("gate", e_)], writes=[("yt", m_ % 4)])
                    else:
                        sc.add("dve", lambda e, bk=bk, yt=yt, sg=sg: e.tensor_scalar(out=yt, in0=pb(bk),
                                                                                     scalar1=gts[:, e_ * 4 + sg:e_ * 4 + sg + 1],
                                                                                     scalar2=None, op0=ALU.mult),
                               reads=[("pb", bk), ("gate", e_)], writes=[("yt", m_ % 4)])
                    scat.append(sc.add("pool", lambda e, db=db, sg=sg, yt=yt: e.indirect_dma_start(
                        out=part,
                        out_offset=bass.IndirectOffsetOnAxis(ap=idx_i[:, e_, db * 4 + sg:db * 4 + sg + 1], axis=0),
                        in_=yt, in_offset=None, compute_op=ALU.add),
                        reads=[("yt", m_ % 4), ik] + partz_all + [("part_x1", i, db_) for i in range(8) for db_ in range(4)],
                        writes=[("part_sc", e_, db, sg)], kind="dma", extra=prev_scatter))
            return scat

        e_select(0)
        e_gather(0)
        e_transpose(0)
        for e_ in range(4):
            e_gateup(e_)
            if e_ + 1 < 4:
                e_transpose(e_ + 1)
            prev_scatter = e_down(e_, prev_scatter)
        all_sc = [("part_sc", e_, db, sg) for e_ in range(4) for db in range(4) for sg in range(4)]
        sc.add("pool", lambda e: e.collective_compute("ReduceScatter", ALU.add, replica_groups=GROUPS,
                                                      ins=[part.opt()], outs=[rs_out.opt()]),
               reads=all_sc + partz_all + [("part_x1", i, db_) for i in range(8) for db_ in range(4)], writes=["rs_out"], kind="cc")
        for i in range(8):
            sc.add("sync" if i % 2 == 0 else "pool",
                   lambda e, i=i: e.dma_start(out=out[i * 512:(i + 1) * 512, :], in_=rs_out[i * 512:(i + 1) * 512, :]),
                   reads=["rs_out"], writes=[("out", i)], kind="dma")
        sc.emit(block)
    return nc


def _na_bias_tables(rpb_h):
    ki = np.arange(128)[:, None]
    qi = np.arange(128)[None, :]

    def pat(m, kt):
        qr = 2 * m + qi // GRID_W
        qc = qi % GRID_W
        kr = 2 * kt + ki // GRID_W
        kc = ki % GRID_W
        rs = np.clip(qr - 4, 0, 56)
        cs = np.clip(qc - 8, 0, GRID_W - 16)
        valid = (kr >= rs) & (kr < rs + 8) & (kc >= cs) & (kc < cs + 16)
        ro = np.clip(kr - qr + 7, 0, 14)
        co = np.clip(kc - qc, -15, 15) + 15
        return np.where(valid, rpb_h[ro, co], np.float32(NEG)).astype(np.float32)

    full_mask = np.full((128, 128), NEG, np.float32)
    pats = [pat(10, 10 + d) for d in (-2, -1, 0, 1, 2)]
    for m in (0, 1):
        pats += [pat(m, kt) for kt in (0, 1, 2, 3)] + [full_mask]
    for m in (30, 31):
        pats += [pat(m, kt) for kt in (28, 29, 30, 31)] + [full_mask]
    return np.ascontiguousarray(np.stack(pats, axis=1))


def _alibi_tables(g):
    slope = np.float32(2.0 ** (-8.0 * (g + 1) / 4))
    ki = np.arange(128, dtype=np.float32)[:, None]
    qi = np.arange(256, dtype=np.float32)[None, :]
    t = np.stack([-slope * (qi - ki), slope * (qi - ki), -slope * np.abs(qi - ki), -slope * np.abs(qi - ki - 128.0)],
                 axis=1).astype(np.float32)
    cb = np.zeros((128, 64), np.float32)
    for delta in range(-31, 31):
        if delta >= 1:
            cb[:, delta + 32] = -slope * 128.0 * delta
        elif delta <= -2:
            cb[:, delta + 32] = slope * 128.0 * delta
    return np.ascontiguousarray(t), cb


def _prep_inputs(inp):
    f = lambda a: np.ascontiguousarray(np.asarray(a, dtype=np.float32))
    x = f(inp["x"])
    w_in = f(inp["w_in"])[0]
    w_out = f(inp["w_out"])[0]
    on_a = f(inp["on_a"])[0]
    subln = f(inp["subln_b"])[0]
    rpb = f(inp["rpb_a"])[0]
    wg, wu, wd = np.asarray(inp["w_gate"])[0], np.asarray(inp["w_up"])[0], np.asarray(inp["w_down"])[0]
    ln1T = np.ascontiguousarray(f(inp["ln1_g"])[0].reshape(16, 128).T)
    qkg = np.ascontiguousarray(np.stack([f(inp["qn_a"])[0]] * 2 + [f(inp["kn_a"])[0]] * 2 + [f(inp["qn_b"])[0]] * 2
                                        + [f(inp["kn_b"])[0]] * 2, axis=1))
    lamv = np.ascontiguousarray(np.stack([f(inp["lam_q1"])[0], f(inp["lam_k1"])[0], f(inp["lam_q2"])[0], f(inp["lam_k2"])[0]]))
    ln2 = f(inp["ln2_g"])
    w_router = f(inp["w_router"])[0]
    maps = []
    p = np.arange(128)
    cc_ = np.arange(NT)
    fcv = np.ascontiguousarray(np.broadcast_to((8 * ((cc_ % 8) // 2) + 2 * (cc_ // 8) + (cc_ % 2)).astype(np.float32), (128, NT)))
    for c in range(8):
        b, g = c // 4, c % 4
        cols = np.concatenate([
            np.arange(256 * g, 256 * g + 256), 1024 + np.arange(256 * g, 256 * g + 256),
            3072 + np.arange(128 * g, 128 * g + 128), 3584 + np.arange(128 * g, 128 * g + 128),
            4096 + np.arange(128 * g, 128 * g + 128), 4608 + np.arange(128 * g, 128 * g + 128),
            2048 + np.arange(256 * g, 256 * g + 256), 5120 + np.arange(256 * g, 256 * g + 256)])
        rows, wog_cols = [], []
        for r in range(4):
            rows += [np.arange(256 * r, 256 * r + 256), 1024 + np.arange(256 * r, 256 * r + 256)]
            wog_cols += [on_a[256 * r:256 * r + 128], on_a[256 * r + 128:256 * r + 256], subln[0:128], subln[128:256]]
        rows = np.concatenate(rows)
        alib, cb = _alibi_tables(g)
        sel = np.zeros((128, 4, N_EXP), np.float32)
        for e_ in range(4):
            sel[:, e_, 4 * g + e_] = 1.0
        own = (1024 * g + np.arange(8)[None, :] * 128 + p[:, None]).astype(np.int32)
        mixi = (4096 * g + np.arange(4)[None, None, :] * 1024 + (np.arange(8)[None, :] * 128 + p[:, None])[:, :, None]).astype(np.int32)
        maps.append({
            "x_b": x[b], "x_own": np.ascontiguousarray(x[b, 1024 * g:1024 * (g + 1)]),
            "w_in_g": np.ascontiguousarray(w_in[:, cols]), "ln1T": ln1T, "qkg": qkg,
            "nab": np.ascontiguousarray(np.stack([_na_bias_tables(rpb[2 * g]), _na_bias_tables(rpb[2 * g + 1])])),
            "alib": alib, "cbias": cb, "lamv": lamv,
            "w_out_p": np.ascontiguousarray(w_out[rows]), "wog": np.ascontiguousarray(np.stack(wog_cols, axis=1)),
            "ln2": ln2, "w_router": w_router, "sel": sel, "own_tok": np.ascontiguousarray((own[:, :, None] + S * np.arange(4)[None, None, :]).astype(np.int32)),
            "mix_idx": np.ascontiguousarray(mixi), "fcv": fcv,
            "wg_e": np.ascontiguousarray(wg[4 * g:4 * g + 4], dtype=np.float32),
            "wu_e": np.ascontiguousarray(wu[4 * g:4 * g + 4], dtype=np.float32),
            "wd_e": np.ascontiguousarray(wd[4 * g:4 * g + 4], dtype=np.float32),
        })
    return maps


def kernel(**inputs):
    maps = _prep_inputs(inputs)
    nc = build_nc()
    res = run_bass_kernel_spmd(nc, maps, core_ids=list(range(8)))
    out = np.empty((2, S, D), np.float32)
    for c in range(8):
        b, g = c // 4, c % 4
        out[b, :, 512 * g:512 * (g + 1)] = np.asarray(res.results[c]["out"], dtype=np.float32)
    return out
```
